# Optimizing a Trainium2 kernel written in Bass

```python
import jax
import jax.numpy as jnp
from jax import lax
import numpy as np

D_MODEL = 2048
BATCH = 2
SEQ = 4096
DEPTH = 1

D_CONV = D_MODEL // 2
D_RWKV = D_MODEL - D_CONV
D_MIX = D_CONV + D_RWKV
HEAD_SIZE = 64
N_RWKV_HEADS = D_RWKV // HEAD_SIZE
CONV_WIDTH = 31
DECAY_LORA = max(32, int(round(1.8 * D_RWKV ** 0.5 / 32)) * 32)
AAA_LORA = max(32, int(round(1.8 * D_RWKV ** 0.5 / 32)) * 32)
GATE_LORA = max(32, int(round(0.6 * D_RWKV ** 0.8 / 32)) * 32)
RWKV_COL_SIZES = (D_RWKV, D_RWKV, D_RWKV, DECAY_LORA, DECAY_LORA, AAA_LORA, AAA_LORA, GATE_LORA)
D_RWKV_COLS = sum(RWKV_COL_SIZES)
RWKV_SPLITS = tuple(int(s) for s in np.cumsum(RWKV_COL_SIZES)[:-1])
D_GLU = 2 * D_CONV
D_IN_PROJ = D_GLU + D_RWKV_COLS
N_GROUPS = 4
EXPERTS_PER_GROUP = 8
N_EXPERTS = N_GROUPS * EXPERTS_PER_GROUP
TOP_K = 2
D_EXPERT = D_MODEL // 4
MOE_BLOCK = 128
N_MOD = 6
RMS_EPS = 1e-6
LN_EPS = 1e-5
GN_EPS = 64e-5

kernel_name = "hymba_conformer_rwkv7_hmoe_block"


def rms_norm(x, g):
    xf = x.astype(jnp.float32)
    y = xf * lax.rsqrt(jnp.mean(xf * xf, axis=-1, keepdims=True) + RMS_EPS)
    return (y * g).astype(x.dtype)


def layer_norm(z, g, b, eps):
    zf = z.astype(jnp.float32)
    mu = jnp.mean(zf, axis=-1, keepdims=True)
    var = jnp.mean(jnp.square(zf - mu), axis=-1, keepdims=True)
    return ((zf - mu) * lax.rsqrt(var + eps) * g + b).astype(z.dtype)


def modulate(h, shift, scale):
    return h * (1 + scale[:, None, :]) + shift[:, None, :]


def conformer_conv(u, conv_w, conv_b, ln_g, ln_b):
    val, gate = jnp.split(u, 2, axis=-1)
    z = val * jax.nn.sigmoid(gate)
    z = lax.conv_general_dilated(
        z, conv_w.astype(z.dtype), window_strides=(1,),
        padding=[(CONV_WIDTH // 2, CONV_WIDTH // 2)],
        dimension_numbers=("NWC", "WIO", "NWC"),
        feature_group_count=D_CONV) + conv_b
    z = layer_norm(z, ln_g, ln_b, LN_EPS)
    return jax.nn.silu(z)


def token_shift(z, mu_prev, mu_next):
    zp = jnp.pad(z, ((0, 0), (1, 0), (0, 0)))[:, :-1]
    zn = jnp.pad(z, ((0, 0), (0, 1), (0, 0)))[:, 1:]
    return z + mu_prev * (zp - z) + mu_next * (zn - z)


def wkv7_bidir_scan(r, w_f, w_b, k_f, k_b, v, a_vec, b_f, b_b):
    def to_dirs(t_f, t_b):
        t = jnp.stack([t_f, jnp.flip(t_b, axis=1)], axis=0)
        return jnp.moveaxis(t, 2, 0)

    xs = (to_dirs(r, r), to_dirs(w_f, w_b), to_dirs(k_f, k_b),
          to_dirs(v, v), to_dirs(a_vec, a_vec), to_dirs(b_f, b_b))

    def step(state, inp):
        r_t, w_t, k_t, v_t, a_t, b_t = inp
        sa = jnp.einsum("dbhij,dbhj->dbhi", state, a_t)
        state = (state * w_t[..., None, :] + sa[..., :, None] * b_t[..., None, :]
                 + v_t[..., :, None] * k_t[..., None, :])
        y = jnp.einsum("dbhij,dbhj->dbhi", state, r_t)
        return state, y

    bsz = r.shape[0]
    state0 = jnp.zeros((2, bsz, N_RWKV_HEADS, HEAD_SIZE, HEAD_SIZE), jnp.float32)
    _, ys = lax.scan(step, state0, xs)
    ys = jnp.moveaxis(ys, 0, 2)
    return ys[0] + jnp.flip(ys[1], axis=1)


def rwkv7_bidir_mix(z, mu_prev, mu_next, w0_f, w2_f, w0_b, w2_b, a0_f, a2_f, a0_b, a2_b,
                    g2, k_k, k_a, r_k, lnx_g, lnx_b):
    f32 = jnp.float32
    bsz, seq, _ = z.shape
    z = token_shift(z, mu_prev, mu_next)
    r, k, v, wd_f, wd_b, ad_f, ad_b, gd = jnp.split(z, RWKV_SPLITS, axis=-1)

    def heads(t):
        return t.reshape(bsz, seq, N_RWKV_HEADS, HEAD_SIZE)

    def decay(wd, w0, w2):
        w = (w0 + jnp.tanh(wd) @ w2).astype(f32)
        w = -jax.nn.softplus(-w) - 0.5
        return jnp.exp(-jnp.exp(w))

    dec_f = decay(wd_f, w0_f, w2_f)
    dec_b = decay(wd_b, w0_b, w2_b)
    a_f = jax.nn.sigmoid((a0_f + ad_f @ a2_f).astype(f32))
    a_b = jax.nn.sigmoid((a0_b + ad_b @ a2_b).astype(f32))
    g = jax.nn.sigmoid(gd) @ g2

    kk = heads((k * k_k).astype(f32))
    kk = kk / jnp.maximum(jnp.sqrt(jnp.sum(kk * kk, axis=-1, keepdims=True)), 1e-12)
    kf = k.astype(f32)
    k_f = kf * (1 + (a_f - 1) * k_a)
    k_b = kf * (1 + (a_b - 1) * k_a)
    rh = heads(r.astype(f32))
    vh = heads(v.astype(f32))

    y = wkv7_bidir_scan(rh, heads(dec_f), heads(dec_b), heads(k_f), heads(k_b), vh,
                        -kk, kk * heads(a_f), kk * heads(a_b))
    y = layer_norm(y, lnx_g.reshape(N_RWKV_HEADS, HEAD_SIZE),
                   lnx_b.reshape(N_RWKV_HEADS, HEAD_SIZE), GN_EPS)
    bonus = jnp.sum(rh * heads(k_f + k_b) * r_k.reshape(N_RWKV_HEADS, HEAD_SIZE),
                    axis=-1, keepdims=True) * vh
    out = (y + bonus).reshape(bsz, seq, D_RWKV) * g.astype(f32)
    return out.astype(z.dtype)


def hier_moe(h, w_rg, b_rg, w_re, b_re, w_gate, w_up, w_down):
    bsz, seq, d = h.shape
    n_tok = bsz * seq
    t = h.reshape(n_tok, d)
    g_logits = (t @ w_rg).astype(jnp.float32) + b_rg
    g_prob = jax.nn.softmax(g_logits, axis=-1)
    g_idx = jnp.argmax(g_logits, axis=-1)
    p_group = jnp.take_along_axis(g_prob, g_idx[:, None], axis=1)[:, 0]
    e_logits = ((t @ w_re).astype(jnp.float32) + b_re).reshape(n_tok, N_GROUPS, EXPERTS_PER_GROUP)
    e_sel = jnp.take_along_axis(e_logits, g_idx[:, None, None], axis=1)[:, 0]
    e_prob = jax.nn.softmax(e_sel, axis=-1)
    top_p, top_i = lax.top_k(e_prob, TOP_K)
    top_p = top_p / jnp.sum(top_p, axis=-1, keepdims=True)
    weights = p_group[:, None] * top_p
    expert_id = g_idx[:, None] * EXPERTS_PER_GROUP + top_i

    n_asg = n_tok * TOP_K
    flat_e = expert_id.reshape(-1)
    flat_tok = jnp.repeat(jnp.arange(n_tok, dtype=jnp.int32), TOP_K)
    flat_w = weights.reshape(-1)
    order = jnp.argsort(flat_e)
    e_sorted = flat_e[order]
    counts = jnp.bincount(flat_e, length=N_EXPERTS)
    padded = (counts + MOE_BLOCK - 1) // MOE_BLOCK * MOE_BLOCK
    pad_end = jnp.cumsum(padded)
    pad_start = pad_end - padded
    seg_start = jnp.cumsum(counts) - counts
    dest = pad_start[e_sorted] + (jnp.arange(n_asg) - seg_start[e_sorted])
    n_blocks = -(-n_asg // MOE_BLOCK) + N_EXPERTS
    n_rows = n_blocks * MOE_BLOCK
    tok_pad = jnp.zeros((n_rows,), jnp.int32).at[dest].set(flat_tok[order])
    w_pad = jnp.zeros((n_rows,), jnp.float32).at[dest].set(flat_w[order])
    block_start = jnp.arange(n_blocks) * MOE_BLOCK
    block_e = jnp.minimum(jnp.sum(pad_end[None, :] <= block_start[:, None], axis=1), N_EXPERTS - 1)

    xs = t[tok_pad].reshape(n_blocks, MOE_BLOCK, d)

    def block_ffn(args):
        xb, e = args
        return (jax.nn.silu(xb @ w_gate[e]) * (xb @ w_up[e])) @ w_down[e]

    ys = lax.map(block_ffn, (xs, block_e)).reshape(n_rows, d)
    ys = ys * w_pad[:, None].astype(ys.dtype)
    out = jax.ops.segment_sum(ys, tok_pad, num_segments=n_tok)
    return out.reshape(bsz, seq, d)


def setup_inputs(seed: int = 0) -> dict:
    key = jax.random.key(seed)
    ks = iter(jax.random.split(key, 40))
    f32 = jnp.float32
    L, D = DEPTH, D_MODEL

    def nrm(shape, s):
        return jax.random.normal(next(ks), shape, f32) * s

    def unif(shape, lo, hi):
        return jax.random.uniform(next(ks), shape, f32, lo, hi)

    return {
        "x": nrm((BATCH, SEQ, D), 1.0),
        "c": nrm((BATCH, D), 1.0),
        "w_ada": nrm((L, D, N_MOD * D), 0.5 * D ** -0.5),
        "b_ada": nrm((L, N_MOD * D), 0.02),
        "norm1_g": 1.0 + nrm((L, D), 0.02),
        "w_in": nrm((L, D, D_IN_PROJ), D ** -0.5),
        "conv_w": nrm((L, CONV_WIDTH, 1, D_CONV), CONV_WIDTH ** -0.5),
        "conv_b": nrm((L, D_CONV), 0.02),
        "conv_ln_g": 1.0 + nrm((L, D_CONV), 0.02),
        "conv_ln_b": nrm((L, D_CONV), 0.02),
        "mu_prev": unif((L, D_RWKV_COLS), 0.0, 0.5),
        "mu_next": unif((L, D_RWKV_COLS), 0.0, 0.5),
        "w0_f": unif((L, D_RWKV), -6.0, -1.0),
        "w2_f": nrm((L, DECAY_LORA, D_RWKV), 0.1 * DECAY_LORA ** -0.5),
        "w0_b": unif((L, D_RWKV), -6.0, -1.0),
        "w2_b": nrm((L, DECAY_LORA, D_RWKV), 0.1 * DECAY_LORA ** -0.5),
        "a0_f": nrm((L, D_RWKV), 0.5),
        "a2_f": nrm((L, AAA_LORA, D_RWKV), 0.3 * AAA_LORA ** -0.5),
        "a0_b": nrm((L, D_RWKV), 0.5),
        "a2_b": nrm((L, AAA_LORA, D_RWKV), 0.3 * AAA_LORA ** -0.5),
        "g2": nrm((L, GATE_LORA, D_RWKV), GATE_LORA ** -0.5),
        "k_k": 0.85 + nrm((L, D_RWKV), 0.05),
        "k_a": 1.0 + nrm((L, D_RWKV), 0.05),
        "r_k": nrm((L, D_RWKV), 0.1),
        "lnx_g": 1.0 + nrm((L, D_RWKV), 0.02),
        "lnx_b": nrm((L, D_RWKV), 0.02),
        "w_out": nrm((L, D_MIX, D), D_MIX ** -0.5),
        "norm2_g": 1.0 + nrm((L, D), 0.02),
        "w_rg": nrm((L, D, N_GROUPS), D ** -0.5),
        "b_rg": nrm((L, N_GROUPS), 0.01),
        "w_re": nrm((L, D, N_EXPERTS), D ** -0.5),
        "b_re": nrm((L, N_EXPERTS), 0.01),
        "w_gate": nrm((L, N_EXPERTS, D, D_EXPERT), D ** -0.5),
        "w_up": nrm((L, N_EXPERTS, D, D_EXPERT), D ** -0.5),
        "w_down": nrm((L, N_EXPERTS, D_EXPERT, D), D_EXPERT ** -0.5),
        "normf_g": 1.0 + nrm((D,), 0.02),
    }


def reference(x, c, w_ada, b_ada, norm1_g, w_in, conv_w, conv_b, conv_ln_g, conv_ln_b,
              mu_prev, mu_next, w0_f, w2_f, w0_b, w2_b, a0_f, a2_f, a0_b, a2_b, g2,
              k_k, k_a, r_k, lnx_g, lnx_b, w_out, norm2_g, w_rg, b_rg, w_re, b_re,
              w_gate, w_up, w_down, normf_g):
    bsz = x.shape[0]
    cs = jax.nn.silu(c)
    h = x
    for l in range(DEPTH):
        mod = (cs @ w_ada[l] + b_ada[l]).reshape(bsz, N_MOD, D_MODEL)
        sh1, sc1, gt1 = mod[:, 0], mod[:, 1], mod[:, 2]
        sh2, sc2, gt2 = mod[:, 3], mod[:, 4], mod[:, 5]

        hn = modulate(rms_norm(h, norm1_g[l]), sh1, sc1)
        proj = hn @ w_in[l]
        y_conv = conformer_conv(proj[..., :D_GLU], conv_w[l], conv_b[l],
                                conv_ln_g[l], conv_ln_b[l])
        y_rwkv = rwkv7_bidir_mix(proj[..., D_GLU:], mu_prev[l], mu_next[l],
                                 w0_f[l], w2_f[l], w0_b[l], w2_b[l],
                                 a0_f[l], a2_f[l], a0_b[l], a2_b[l], g2[l],
                                 k_k[l], k_a[l], r_k[l], lnx_g[l], lnx_b[l])
        y = jnp.concatenate([y_conv, y_rwkv], axis=-1) @ w_out[l]
        h = h + gt1[:, None, :] * y

        hn2 = modulate(rms_norm(h, norm2_g[l]), sh2, sc2)
        h = h + gt2[:, None, :] * hier_moe(hn2, w_rg[l], b_rg[l], w_re[l], b_re[l],
                                          w_gate[l], w_up[l], w_down[l])
    return rms_norm(h, normf_g)
```

```python
import numpy as np
from contextlib import ExitStack
import concourse.bass as bass
import concourse.mybir as mybir
from concourse.bass_utils import run_bass_kernel_spmd

F32 = mybir.dt.float32
BF16 = mybir.dt.bfloat16
AF = mybir.ActivationFunctionType
ALU = mybir.AluOpType
AX = mybir.AxisListType

NCORES = 8
D = 2048
SEQ = 4096
TOK = 1024
HALO = 64
TH = TOK + 2 * HALO
DC = 1024
DR = 1024
NCOL = 5536
NCC = 44
NRC = 28
KW = 31
NE = 32
DE = 512
CH = 128
RMS_EPS = 1e-6
LN_EPS = 1e-5
GN_EPS = 64e-5


class Ev:
    __slots__ = ("sem", "val")

    def __init__(self, sem, val=None):
        self.sem = sem
        self.val = val


class Tok:
    __slots__ = ("w", "r", "name")

    def __init__(self, name=""):
        self.w = None
        self.r = []
        self.name = name


class DSem:
    def __init__(self, sem, batch):
        self.sem = sem
        self.total = 0
        self.batch = batch
        self.ev = Ev(sem, 0) if batch else None


class Sched:
    ENG = ("pe", "act", "dve", "pool", "sp")

    def __init__(self, nc, stack, n_dma_sems=72):
        self.nc = nc
        self.sem = {e: stack.enter_context(nc.semaphore("s_" + e)) for e in self.ENG}
        self.count = {e: 0 for e in self.ENG}
        self.ops = {e: [] for e in self.ENG}
        self.pending = {e: [] for e in self.ENG}
        self.free_dsems = [stack.enter_context(nc.semaphore("d%d" % i)) for i in range(n_dma_sems)]
        self.dsems = {}
        self.out_evs = []
        self.waited = {e: {} for e in self.ENG}
        self.nops = 0

    def dsem(self, key, batch=False):
        if key not in self.dsems:
            self.dsems[key] = DSem(self.free_dsems.pop(), batch)
        return self.dsems[key]

    def _collect(self, reads, writes):
        waits = []
        for t in reads:
            if t.w is not None:
                waits.append(t.w)
        for t in writes:
            if t.w is not None:
                waits.append(t.w)
            waits.extend(t.r)
        return waits

    def op(self, eng, fn, reads=(), writes=(), signal=True):
        waits = self._collect(reads, writes)
        ev = Ev(self.sem[eng])
        if signal:
            self.count[eng] += 1
            ev.val = self.count[eng]
            for p in self.pending[eng]:
                p.val = ev.val
            self.pending[eng] = []
        else:
            self.pending[eng].append(ev)
        self.ops[eng].append((waits, fn, (self.sem[eng], 1) if signal else None))
        for t in reads:
            if len(t.r) > 6:
                t.r = t.r[-6:] + [e for e in t.r[:-6] if e.val is None]
            t.r.append(ev)
        for t in writes:
            t.w = ev
            t.r = []
        self.nops += 1
        return ev

    def dma(self, q, out, in_, key, reads=(), writes=(), batch=False, is_out=False, **kw):
        ds = self.dsem(key, batch)
        waits = self._collect(reads, writes)
        ds.total += 16
        if ds.batch:
            ev = ds.ev
            ev.val = ds.total
        else:
            ev = Ev(ds.sem, ds.total)

        def fn(e, out=out, in_=in_, kw=kw):
            return e.dma_start(out=out, in_=in_, **kw)

        self.ops[q].append((waits, fn, (ds.sem, 16)))
        for t in reads:
            t.r.append(ev)
        for t in writes:
            t.w = ev
            t.r = []
        if is_out:
            self.out_evs.append(ev)
        self.nops += 1
        return ev

    def barrier(self):
        evs = []
        for e in self.ENG:
            assert not self.pending[e], "engine %s has non-signalled tail" % e
            if self.count[e]:
                evs.append(Ev(self.sem[e], self.count[e]))
        for ds in self.dsems.values():
            if ds.total:
                evs.append(Ev(ds.sem, ds.total))
        for e in self.ENG:
            self.ops[e].append((list(evs), None, None))

    def emit(self, final=False):
        nc = self.nc
        if final:
            self.ops["sp"].append((list(self.out_evs), None, None))
        for e in self.ENG:
            assert not self.pending[e], "engine %s ends with non-signaling op" % e
        with nc.Block() as block:
            def mk(ename):
                def body(eng):
                    waited = self.waited[ename]
                    for waits, fn, inc in self.ops[ename]:
                        need = {}
                        for ev in waits:
                            assert ev.val is not None
                            if ename == "pe" and ev.sem is self.sem["pe"]:
                                continue
                            k = id(ev.sem)
                            if need.get(k, (None, 0))[1] < ev.val:
                                need[k] = (ev.sem, ev.val)
                        for k, (s, v) in need.items():
                            if waited.get(k, 0) >= v:
                                continue
                            waited[k] = v
                            eng.wait_ge(s, v)
                        if fn is not None:
                            ins = fn(eng)
                            if inc is not None:
                                ins.then_inc(inc[0], inc[1])
                    self.ops[ename] = []
                return body
            block.tensor(mk("pe"))
            block.scalar(mk("act"))
            block.vector(mk("dve"))
            block.gpsimd(mk("pool"))
            block.sync(mk("sp"))


class Ring:
    def __init__(self, aps, name):
        self.aps = aps
        self.toks = [Tok("%s%d" % (name, i)) for i in range(len(aps))]
        self.i = 0

    def next(self):
        k = self.i % len(self.aps)
        self.i += 1
        return self.aps[k], self.toks[k], k


def build(stop=99, debug=False, dbg_pc=1, dbg_d=0, dbg_tb=0):
    nc = bass.Bass("TRN2", target_bir_lowering=False)
    dram_in = {}

    def din(name, shape, dt=F32):
        dram_in[name] = nc.dram_tensor(name, list(shape), dt, kind="ExternalInput").ap()
        return dram_in[name]

    xf = din("xf", [SEQ, D])
    xh = din("xh", [TH, D])
    hmask_d = din("hmask", [128, TH])
    qflag_d = din("qflag", [128, 4])
    ccol_d = din("c_col", [128, 16])
    wada_d = din("w_ada_r", [96, 128, 16, 128])
    bada_d = din("b_ada_r", [128, 96])
    gn_d = din("gnorm", [128, 3, 16])
    win_d = din("w_in_r", [NCC, 128, 16, 128])
    ident_d = din("ident", [128, 128])
    out_d = nc.dram_tensor("out", [TOK, D], F32, kind="ExternalOutput").ap()

    PT = nc.dram_tensor("PT", [NRC * 128, SEQ], F32, kind="ExternalOutput" if debug == 2 else "Internal").ap()
    PH = nc.dram_tensor("PH", [NCC * 128, TH], F32, kind="ExternalOutput" if debug == 2 else "Internal").ap()
    dbg = {}
    if debug:
        dbg["modv"] = nc.dram_tensor("dbg_modv", [128, 96], F32, kind="ExternalOutput").ap()
        dbg["xT"] = nc.dram_tensor("dbg_xT", [128, 16, TOK], F32, kind="ExternalOutput").ap()
    dbg_y = nc.dram_tensor("dbg_y", [128, 16, TOK], BF16, kind="ExternalOutput").ap() if debug else None

    with ExitStack() as st:
        S = Sched(nc, st)

        uid = [0]

        def sb(stack, name, shape, dt=F32):
            uid[0] += 1
            return stack.enter_context(nc.sbuf_tensor("sb%d_%s" % (uid[0], name), list(shape), dt))

        def ps(stack, name, shape, dt=F32):
            uid[0] += 1
            return stack.enter_context(nc.psum_tensor("ps%d_%s" % (uid[0], name), list(shape), dt))

        ident = sb(st, "ident", [128, 128]); t_ident = Tok("ident")
        identb = sb(st, "identb", [128, 128], BF16); t_identb = Tok("identb")
        modv = sb(st, "modv", [128, 96]); t_modv = Tok("modv")
        gnv = sb(st, "gnv", [128, 3, 16]); t_gnv = Tok("gnv")
        a1 = sb(st, "a1", [128, 16]); t_a1 = Tok("a1")
        a2 = sb(st, "a2", [128, 16]); t_a2 = Tok("a2")
        qflag = sb(st, "qflag", [128, 4]); t_qflag = Tok("qflag")

        stage_tbl = {}
        if debug == 3:
            for nm_ in ("r", "k", "lw", "lr", "kk", "kd", "bb", "cum", "pref", "Y0s", "QT"):
                stage_tbl[nm_] = ([128, 512], F32)
            for nm_ in ("At", "Rt", "Bt", "Kt", "Bh", "Kh", "Vb"):
                stage_tbl[nm_] = ([128, 512], BF16)
            for nm_ in ("Atok", "Vtok", "AkT0", "ArbT0", "ArkT0", "TT0", "AkT1", "ArbT1", "ArkT1", "TT1", "N1", "NT1", "Gtok", "U0tok"):
                stage_tbl[nm_] = ([128, 4, 128], BF16)
            stage_tbl["WC"] = ([128, 4], F32); stage_tbl["Zloc"] = ([128, 4, 64], F32); stage_tbl["PhiT"] = ([128, 4, 64], F32); stage_tbl["Zin"] = ([128, 64], F32)
        stage_slots = {k_: (sb(st, "stage_" + k_, v_[0], v_[1]), Tok()) for k_, v_ in stage_tbl.items()}
        S.dma("sp", ident[:], ident_d, "const", writes=[t_ident], batch=True)
        S.dma("pool", identb[:], ident_d, "constc", writes=[t_identb], batch=True)
        S.dma("sp", gnv[:], gn_d, "const", writes=[t_gnv], batch=True)
        S.dma("sp", qflag[:], qflag_d, "const", writes=[t_qflag], batch=True)

        with ExitStack() as ph:
            ccol = sb(ph, "ccol", [128, 16]); t_ccol = Tok("ccol")
            cs = sb(ph, "cs", [128, 16]); t_cs = Tok("cs")
            bada = sb(ph, "bada", [128, 96]); t_bada = Tok("bada")
            slabs = [sb(ph, "wa%d" % i, [128, 16, 128]) for i in range(6)]
            ring = Ring(slabs, "wa")
            pmod = ps(ph, "pmod", [128, 96]); t_pmod = Tok("pmod")
            S.dma("sp", ccol[:], ccol_d, "const", writes=[t_ccol], batch=True)
            S.dma("sp", bada[:], bada_d, "const", writes=[t_bada], batch=True)
            S.op("act", lambda e: e.activation(cs[:], ccol[:], AF.Silu), reads=[t_ccol], writes=[t_cs])
            for cc in range(96):
                slab, tk, k = ring.next()
                S.dma("sp" if cc % 2 == 0 else "act", slab[:], wada_d[cc], "wa%d" % k, writes=[tk])
                for kk in range(16):
                    S.op("pe", lambda e, slab=slab, kk=kk, cc=cc: e.matmul(
                        pmod[:, cc:cc + 1], slab[:, kk, :], cs[:, kk:kk + 1], start=(kk == 0), stop=(kk == 15)),
                        reads=[tk, t_cs], writes=[t_pmod], signal=(kk == 15))
            S.op("dve", lambda e: e.tensor_tensor(modv[:], pmod[:], bada[:], ALU.add),
                 reads=[t_pmod, t_bada], writes=[t_modv])
            S.op("dve", lambda e: e.scalar_tensor_tensor(a1[:], modv[:, 16:32], 1.0, gnv[:, 0, :], ALU.add, ALU.mult),
                 reads=[t_modv, t_gnv], writes=[t_a1])
            S.op("dve", lambda e: e.scalar_tensor_tensor(a2[:], modv[:, 64:80], 1.0, gnv[:, 1, :], ALU.add, ALU.mult),
                 reads=[t_modv, t_gnv], writes=[t_a2])
            if debug:
                S.dma("sp", dbg["modv"], modv[:], "dbg", reads=[t_modv], batch=True, is_out=True)
            S.barrier()
            S.emit()
        if stop <= 0:
            return _finish(nc, S, st, dram_in)

        def transpose_pass(ph, tag, src, ntile, with_norm, dst_fn, t_dst):
            xts = [sb(ph, tag + "xt%d" % i, [128, D]) for i in range(4)]
            xring = Ring(xts, tag + "xt")
            sq = sb(ph, tag + "sq", [128, D]); t_sq = Tok("sq")
            ss = [sb(ph, tag + "ss%d" % i, [128, 1]) for i in range(4)]
            dg = [sb(ph, tag + "dg%d" % i, [128, 128]) for i in range(4)]
            t_ss = [Tok() for _ in range(4)]; t_dg = [Tok() for _ in range(4)]
            ptr = [ps(ph, tag + "ptr%d" % i, [128, 256]) for i in range(4)]
            pring = Ring(ptr, tag + "ptr")
            ev_i = 0
            g = 0
            ti = 0
            while ti < ntile:
                n_in = min(2, ntile - ti)
                tiles = []
                for j in range(n_in):
                    xt, tx, k = xring.next()
                    S.dma("sp" if (ti + j) % 2 == 0 else "act", xt[:], src[(ti + j) * 128:(ti + j + 1) * 128, :],
                          "xt%d" % k, writes=[tx])
                    if with_norm:
                        S.op("act", lambda e, xt=xt, k=k: e.activation(sq[:], xt[:], AF.Square, accum_out=ss[k][:]),
                             reads=[tx], writes=[t_sq, t_ss[k]])
                        S.op("dve", lambda e, k=k: e.tensor_scalar(ss[k][:], ss[k][:], 1.0 / D, RMS_EPS, ALU.mult, ALU.add),
                             reads=[t_ss[k]], writes=[t_ss[k]])
                        S.op("act", lambda e, k=k: e.activation(ss[k][:], ss[k][:], AF.Sqrt),
                             reads=[t_ss[k]], writes=[t_ss[k]])
                        S.op("dve", lambda e, k=k: e.reciprocal(ss[k][:], ss[k][:]),
                             reads=[t_ss[k]], writes=[t_ss[k]])
                        S.op("dve", lambda e, k=k: e.tensor_scalar(dg[k][:], ident[:], ss[k][:], None, ALU.mult),
                             reads=[t_ss[k], t_ident], writes=[t_dg[k]])
                    tiles.append((xt, tx, k))
                for fc in range(16):
                    pt, tp, _ = pring.next()
                    for j, (xt, tx, k) in enumerate(tiles):
                        rhs = dg[k] if with_norm else ident
                        rt = t_dg[k] if with_norm else t_ident
                        S.op("pe", lambda e, pt=pt, xt=xt, rhs=rhs, j=j, fc=fc: e.matmul(
                            pt[:, j * 128:(j + 1) * 128], xt[:, fc * 128:(fc + 1) * 128], rhs[:], start=True, stop=True),
                            reads=[tx, rt], writes=[tp], signal=(j == n_in - 1))
                    eng = "act" if ev_i % 2 == 0 else "dve"
                    ev_i += 1
                    dst = dst_fn(fc, ti * 128, n_in * 128)
                    src_ps = pt[:, 0:n_in * 128]
                    if with_norm:
                        if eng == "act":
                            S.op("act", lambda e, src_ps=src_ps, dst=dst, fc=fc: e.activation(
                                dst, src_ps, AF.Identity, bias=modv[:, fc:fc + 1], scale=a1[:, fc:fc + 1]),
                                reads=[tp, t_modv, t_a1], writes=[t_dst])
                        else:
                            S.op("dve", lambda e, src_ps=src_ps, dst=dst, fc=fc: e.tensor_scalar(
                                dst, src_ps, a1[:, fc:fc + 1], modv[:, fc:fc + 1], ALU.mult, ALU.add),
                                reads=[tp, t_modv, t_a1], writes=[t_dst])
                    else:
                        if eng == "act":
                            S.op("act", lambda e, src_ps=src_ps, dst=dst: e.copy(dst, src_ps), reads=[tp], writes=[t_dst])
                        else:
                            S.op("dve", lambda e, src_ps=src_ps, dst=dst: e.tensor_copy(dst, src_ps), reads=[tp], writes=[t_dst])
                ti += n_in

        def gemm_to_dram(ph, tag, hnT, t_hnT, ntok, chunks, dram, col0=0, mask=None):
            slabs = [sb(ph, tag + "w%d" % i, [128, 16, 128], BF16) for i in range(3)]
            wring = Ring(slabs, tag + "w")
            stg = [sb(ph, tag + "stg%d" % i, [128, ntok]) for i in range(2)]
            sring = Ring(stg, tag + "stg")
            pg = [ps(ph, tag + "pg%d" % i, [128, 512]) for i in range(4)]
            pring = Ring(pg, tag + "pg")
            groups = []
            o = 0
            while o < ntok:
                n = min(512, ntok - o)
                groups.append((o, n))
                o += n
            ev_i = 0
            for gc, rc in chunks:
                slab, tw, k = wring.next()
                S.dma("pool", slab[:], win_d[gc], "w%d" % k, writes=[tw])
                sg, tsg, k2 = sring.next()
                for (o, n) in groups:
                    pt, tp, _ = pring.next()
                    for kk in range(16):
                        S.op("pe", lambda e, pt=pt, slab=slab, kk=kk, o=o, n=n: e.matmul(
                            pt[:, 0:n], slab[:, kk, :], hnT[:, kk, o:o + n], start=(kk == 0), stop=(kk == 15)),
                            reads=[tw, t_hnT], writes=[tp], signal=(kk == 15))
                    if mask is not None:
                        S.op("dve", lambda e, pt=pt, sg=sg, o=o, n=n: e.tensor_tensor(sg[:, o:o + n], pt[:, 0:n], hmask[:, o:o + n], ALU.mult),
                             reads=[tp, t_hmask], writes=[tsg])
                    else:
                        eng = "act" if ev_i % 2 == 0 else "dve"
                        ev_i += 1
                        if eng == "act":
                            S.op("act", lambda e, pt=pt, sg=sg, o=o, n=n: e.copy(sg[:, o:o + n], pt[:, 0:n]), reads=[tp], writes=[tsg])
                        else:
                            S.op("dve", lambda e, pt=pt, sg=sg, o=o, n=n: e.tensor_copy(sg[:, o:o + n], pt[:, 0:n]), reads=[tp], writes=[tsg])
                S.dma("sp", dram[rc * 128:(rc + 1) * 128, col0:col0 + ntok], sg[:], "st%d" % k2, reads=[tsg], is_out=(debug == 2))

        HALF = SEQ // 2
        for half in range(2):
            with ExitStack() as ph:
                hnT = sb(ph, "hnT%d" % half, [128, 16, HALF], BF16); t_hnT = Tok("hnT")
                with ExitStack() as ph2:
                    transpose_pass(ph2, "a%d" % half, xf[half * HALF:(half + 1) * HALF, :], HALF // 128, True,
                                   lambda fc, o, n, hnT=hnT: hnT[:, fc, o:o + n], t_hnT)
                    S.barrier(); S.emit()
                with ExitStack() as ph2:
                    gemm_to_dram(ph2, "a%d" % half, hnT, t_hnT, HALF, [(16 + rc, rc) for rc in range(NRC)], PT, col0=half * HALF)
                    S.barrier(); S.emit()
        if stop <= 1:
            return _finish(nc, S, st, dram_in)
        with ExitStack() as ph:
            hnT = sb(ph, "hnTh", [128, 16, TH], BF16); t_hnT = Tok("hnTh")
            hmask = sb(ph, "hmask", [128, TH]); t_hmask = Tok("hmask")
            S.dma("sp", hmask[:], hmask_d, "hm", writes=[t_hmask])
            with ExitStack() as ph2:
                transpose_pass(ph2, "b", xh, TH // 128, True, lambda fc, o, n: hnT[:, fc, o:o + n], t_hnT)
                S.barrier(); S.emit()
            with ExitStack() as ph2:
                gemm_to_dram(ph2, "b", hnT, t_hnT, TH, [(gc, gc) for gc in range(NCC)], PH, mask=True)
                S.barrier(); S.emit()
        if stop <= 2:
            return _finish(nc, S, st, dram_in)

        class Buf:
            def __init__(self, t, k=None):
                self.t = t
                self.k = k if k is not None else Tok()

        def B(stack, name, shape, dt=F32):
            return Buf(sb(stack, name, shape, dt))

        def PB(stack, name, shape):
            return Buf(ps(stack, name, shape))

        def _ks(bs):
            return [b.k for b in bs]

        def V(fn, r=(), w=()):
            return S.op("dve", fn, reads=_ks(r), writes=_ks(w))

        def A(fn, r=(), w=()):
            return S.op("act", fn, reads=_ks(r), writes=_ks(w))

        def P(fn, r=(), w=()):
            return S.op("pool", fn, reads=_ks(r), writes=_ks(w))

        def M(fn, r=(), w=(), sig=True):
            return S.op("pe", fn, reads=_ks(r), writes=_ks(w), signal=sig)

        def LD(q, dst, src, key, w, batch=False):
            return S.dma(q, dst, src, key, writes=_ks(w), batch=batch)

        dumps = {}

        def DUMP(name, ap, shape, dt=F32):
            if debug != 3:
                return
            dten = nc.dram_tensor("dmp_" + name, list(shape), dt, kind="ExternalOutput").ap()
            dumps[name] = dten
            return dten

        staged = []

        def dump_buf(name, b_, shape, dt=F32, view=None):
            if debug != 3:
                return
            dten = DUMP(name, None, shape, dt)
            slot = Buf(stage_slots[name][0], stage_slots[name][1])
            P(lambda e: e.tensor_copy(slot.t[:], b_.t[:]), [b_], [slot])
            staged.append((dten, slot))

        def flush_dumps():
            for dten, slot in staged:
                S.dma("sp", dten, slot.t[:], "dmp", reads=[slot.k], batch=True, is_out=True)
            staged[:] = []

        Bident = Buf(ident, t_ident); Bidentb = Buf(identb, t_identb); Bmodv = Buf(modv, t_modv)
        Bgnv = Buf(gnv, t_gnv); Ba2 = Buf(a2, t_a2); Bqflag = Buf(qflag, t_qflag)

        cmask = B(st, "cmask", [128, 8, 128]); rst = B(st, "rst", [128, 512]); blk64 = B(st, "blk64", [128, 128])
        ones = B(st, "ones", [128, 128]); ident2 = B(st, "ident2", [128, 64])
        mu = B(st, "mu", [128, 2, NCC]); c0 = B(st, "c0", [128, NCC]); rv = B(st, "rv", [128, 9, 8]); omka = B(st, "omka", [128, 8])
        w2 = B(st, "w2", [128, DR], BF16); a2w = B(st, "a2w", [128, DR], BF16)
        g2a = B(st, "g2a", [128, DR], BF16); g2b = B(st, "g2b", [32, DR], BF16)
        convw = B(st, "convw", [128, 8, KW]); convv = B(st, "convv", [128, 3, 8])
        st_y = ExitStack()
        yTr = B(st_y, "yTr", [128, 8, TOK], BF16)
        for (b_, d_) in ((cmask, din("cmask", [128, 8, 128])), (rst, din("rst", [128, 512])), (blk64, din("blk64", [128, 128])),
                         (ones, din("ones", [128, 128])), (ident2, din("ident2", [128, 64])), (mu, din("mu", [128, 2, NCC])),
                         (rv, din("rv", [128, 9, 8])), (convw, din("convw", [128, 8, KW])), (convv, din("convv", [128, 3, 8]))):
            LD("sp", b_.t[:], d_, "const", [b_], batch=True)
        for (b_, d_) in ((w2, din("w2", [128, DR])), (a2w, din("a2w", [128, DR])), (g2a, din("g2a", [128, DR])), (g2b, din("g2b", [32, DR]))):
            LD("pool", b_.t[:], d_, "constc", [b_], batch=True)
        V(lambda e: e.tensor_tensor(c0.t[:], mu.t[:, 0, :], mu.t[:, 1, :], ALU.add), [mu], [c0])
        V(lambda e: e.tensor_scalar(c0.t[:], c0.t[:], -1.0, 1.0, ALU.mult, ALU.add), [c0], [c0])
        V(lambda e: e.tensor_scalar(omka.t[:], rv.t[:, 5, :], -1.0, 1.0, ALU.mult, ALU.add), [rv], [omka])

        def load_shift(ph_, dst, dram, gc, rows, col0, n, lo_edge, hi_edge, raw, nrows=128, key="raw"):
            a = 0 if lo_edge else 1
            bnd = 0 if hi_edge else 1
            if lo_edge or hi_edge:
                P(lambda e: e.memset(raw.t[:nrows, 0:n + 2], 0.0), [], [raw])
            LD("sp", raw.t[:nrows, 1 - a:n + 1 + bnd], dram[rows:rows + nrows, col0 - a:col0 + n + bnd], key, [raw])
            V(lambda e: e.tensor_scalar(dst.t[:nrows, 0:n], raw.t[:nrows, 1:n + 1], c0.t[:nrows, gc:gc + 1], None, ALU.mult), [raw, c0], [dst])
            V(lambda e: e.scalar_tensor_tensor(dst.t[:nrows, 0:n], raw.t[:nrows, 0:n], mu.t[:nrows, 0, gc:gc + 1], dst.t[:nrows, 0:n], ALU.mult, ALU.add), [raw, mu, dst], [dst])
            V(lambda e: e.scalar_tensor_tensor(dst.t[:nrows, 0:n], raw.t[:nrows, 2:n + 2], mu.t[:nrows, 1, gc:gc + 1], dst.t[:nrows, 0:n], ALU.mult, ALU.add), [raw, mu, dst], [dst])

        def issue_load(raw, dram, rows, col0, n, lo_edge, hi_edge, key):
            a = 0 if lo_edge else 1
            bnd = 0 if hi_edge else 1
            if lo_edge or hi_edge:
                P(lambda e: e.memset(raw.t[:, 0:n + 2], 0.0), [], [raw])
            LD("sp", raw.t[:, 1 - a:n + 1 + bnd], dram[rows:rows + 128, col0 - a:col0 + n + bnd], key, [raw])

        def apply_shift(dst, raw, gc, n):
            V(lambda e: e.tensor_scalar(dst.t[:, 0:n], raw.t[:, 1:n + 1], c0.t[:, gc:gc + 1], None, ALU.mult), [raw, c0], [dst])
            V(lambda e: e.scalar_tensor_tensor(dst.t[:, 0:n], raw.t[:, 0:n], mu.t[:, 0, gc:gc + 1], dst.t[:, 0:n], ALU.mult, ALU.add), [raw, mu, dst], [dst])
            V(lambda e: e.scalar_tensor_tensor(dst.t[:, 0:n], raw.t[:, 2:n + 2], mu.t[:, 1, gc:gc + 1], dst.t[:, 0:n], ALU.mult, ALU.add), [raw, mu, dst], [dst])

        NB = SEQ // 512
        with ExitStack() as ph:
            TWD = nc.dram_tensor("TWD", [128, SEQ], BF16, kind="Internal").ap(); ADD = nc.dram_tensor("ADD", [128, SEQ], BF16, kind="Internal").ap()
            ado = B(ph, "ado", [128, TOK], BF16); sga = B(ph, "sga", [128, TOK], BF16); sgb = B(ph, "sgb", [32, TOK], BF16)
            YD = [nc.dram_tensor("YF", [128, SEQ], F32, kind="Internal").ap(), nc.dram_tensor("YB", [128, SEQ], F32, kind="Internal").ap()]
            with ExitStack() as p2:
                raw = B(p2, "raw0", [128, 1026]); tmp = B(p2, "tmp0", [128, 1024]); tb1 = B(p2, "tb1", [128, 1024], BF16); tb2 = B(p2, "tb2", [128, 1024], BF16)
                for blk in range(SEQ // 1024):
                    lo, hi = blk == 0, blk == SEQ // 1024 - 1
                    load_shift(p2, tmp, PT, 40, 24 * 128, blk * 1024, 1024, lo, hi, raw)
                    A(lambda e: e.activation(tb1.t[:], tmp.t[:], AF.Tanh), [tmp], [tb1])
                    S.dma("sp", TWD[:, blk * 1024:(blk + 1) * 1024], tb1.t[:], "twd", reads=[tb1.k])
                    load_shift(p2, tmp, PT, 41, 25 * 128, blk * 1024, 1024, lo, hi, raw)
                    A(lambda e: e.copy(tb2.t[:], tmp.t[:]), [tmp], [tb2])
                    S.dma("sp", ADD[:, blk * 1024:(blk + 1) * 1024], tb2.t[:], "add", reads=[tb2.k])
                load_shift(p2, tmp, PH, 41, 41 * 128, HALO, TOK, False, False, raw)
                A(lambda e: e.copy(ado.t[:], tmp.t[:]), [tmp], [ado])
                load_shift(p2, tmp, PH, 42, 42 * 128, HALO, TOK, False, False, raw)
                A(lambda e: e.activation(sga.t[:], tmp.t[:], AF.Sigmoid), [tmp], [sga])
                load_shift(p2, tmp, PH, 43, 43 * 128, HALO, TOK, False, False, raw, nrows=32)
                A(lambda e: e.activation(sgb.t[:], tmp.t[0:32, :], AF.Sigmoid), [tmp], [sgb])
                S.barrier(); S.emit()

            for pc in ([dbg_pc] if debug == 3 else range(8)):
                cs_ = slice(pc * 128, (pc + 1) * 128)
                with ExitStack() as p2:
                    WS_F32 = ("r", "k", "lw", "lr", "kk", "t1", "t2", "pref", "cum", "E", "bb", "kd")
                    WS_BF = ("At", "Rt", "Bt", "Kt", "Bh", "Kh", "Vb")
                    WS_TOK = ("Atok", "Bhtok", "Khtok", "Vtok", "Gtok", "AkVs", "U0tok")

                    def mk_ws(tag):
                        W = {"raws": [[B(p2, "raw%s%d%s" % (x_, s_, tag), [128, 514]) for x_ in "rkv"] for s_ in range(2)],
                             "tws": [B(p2, "twb%d%s" % (s_, tag), [128, 512], BF16) for s_ in range(2)],
                             "ads": [B(p2, "adb%d%s" % (s_, tag), [128, 512], BF16) for s_ in range(2)]}
                        for n_ in WS_F32:
                            W[n_] = B(p2, n_ + tag, [128, 512])
                        for n_ in WS_BF:
                            W[n_] = B(p2, n_ + tag, [128, 512], BF16)
                        for n_ in WS_TOK:
                            W[n_] = B(p2, n_ + tag, [128, 4, 128], BF16)
                        W["hb"] = [{n_: B(p2, "%s%d%s" % (n_, h, tag), [128, 4, 128], BF16) for n_ in
                                    ("AkT", "ArbT", "ArkT", "TT", "N", "NT", "Na", "NTa", "Nb", "NTb", "Tt")} for h in range(2)]
                        W["PhiT"] = B(p2, "PhiT" + tag, [128, 4, 64]); W["Zloc"] = B(p2, "Zloc" + tag, [128, 4, 64])
                        W["Z"] = B(p2, "Z" + tag, [128, 64]); W["WC"] = B(p2, "WC" + tag, [128, 4])
                        W["py"] = PB(p2, "py" + tag, [128, 512]); W["pz"] = PB(p2, "pz" + tag, [128, 64])
                        return W

                    WSP = [mk_ws("f"), mk_ws("b")]
                    pr = [PB(p2, "pr%d" % i, [128, 512]) for i in range(4)]
                    pring = Ring(pr, "pr")

                    def pnext():
                        k = pring.i % len(pr)
                        pring.i += 1
                        return pr[k]

                    v3 = lambda b_: b_.t[:].rearrange("p (c t) -> p c t", t=128)

                    def issue_block_loads(d, tb, W, s_):
                        sfx = "fb"[d]
                        lo, hi = tb == 0, tb == NB - 1
                        c0_ = tb * 512
                        ts_ = slice(c0_, c0_ + 512)
                        LD("act", W["tws"][s_].t[:], TWD[:, ts_], "twl%d%s" % (s_, sfx), [W["tws"][s_]])
                        LD("act", W["ads"][s_].t[:], ADD[:, ts_], "adl%d%s" % (s_, sfx), [W["ads"][s_]])
                        issue_load(W["raws"][s_][0], PT, pc * 128, c0_, 512, lo, hi, "rawr%d%s" % (s_, sfx))
                        issue_load(W["raws"][s_][1], PT, (8 + pc) * 128, c0_, 512, lo, hi, "rawk%d%s" % (s_, sfx))
                        issue_load(W["raws"][s_][2], PT, (16 + pc) * 128, c0_, 512, lo, hi, "rawv%d%s" % (s_, sfx))

                    def block_gen(d, tb, W, idx, nxt_tb):
                        s_ = idx % 2
                        if idx == 0:
                            issue_block_loads(d, tb, W, s_)
                        if nxt_tb is not None:
                            issue_block_loads(d, nxt_tb, W, 1 - s_)
                        tw, adf = W["tws"][s_], W["ads"][s_]
                        r_, k_, lw, lr, kk = W["r"], W["k"], W["lw"], W["lr"], W["kk"]
                        t1, t2, pref, cum, E, bb, kd = W["t1"], W["t2"], W["pref"], W["cum"], W["E"], W["bb"], W["kd"]
                        v_ = t2; QT = E; Y0s = t1
                        At, Rt, Bt, Kt, Bh, Kh, Vb = W["At"], W["Rt"], W["Bt"], W["Kt"], W["Bh"], W["Kh"], W["Vb"]
                        Atok, Bhtok, Khtok, Vtok, Gtok, AkVs, U0tok = [W[n_] for n_ in WS_TOK]
                        hb = W["hb"]; PhiT, Zloc, Z, WC, py, pz = W["PhiT"], W["Zloc"], W["Z"], W["WC"], W["py"], W["pz"]
                        hs_w = slice(d * 64, d * 64 + 64)
                        mN, mI, mNT = (0, 1, 2) if d == 0 else (2, 3, 0)
                        sfx = "fb"[d]
                        lo, hi = tb == 0, tb == NB - 1
                        c0_ = tb * 512
                        ts_ = slice(c0_, c0_ + 512)
                        apply_shift(r_, W["raws"][s_][0], 16 + pc, 512)
                        apply_shift(k_, W["raws"][s_][1], 24 + pc, 512)
                        apply_shift(v_, W["raws"][s_][2], 32 + pc, 512)
                        P(lambda e: e.tensor_copy(Vb.t[:], v_.t[:]), [v_], [Vb])
                        yield
                        pt = pnext()
                        M(lambda e, pt=pt, hs_w=hs_w: e.matmul(pt.t[:], w2.t[hs_w, cs_], tw.t[hs_w, :], start=True, stop=True), [w2, tw], [pt])
                        A(lambda e, pt=pt, d=d: e.activation(lw.t[:], pt.t[:], AF.Sigmoid, bias=rv.t[:, d, pc:pc + 1]), [pt, rv], [lw])
                        V(lambda e: e.tensor_scalar(lw.t[:], lw.t[:], -0.6065306597126334, None, ALU.mult), [lw], [lw])
                        pt = pnext()
                        M(lambda e, pt=pt, hs_w=hs_w: e.matmul(pt.t[:], a2w.t[hs_w, cs_], adf.t[hs_w, :], start=True, stop=True), [a2w, adf], [pt])
                        A(lambda e, pt=pt, d=d: e.activation(lr.t[:], pt.t[:], AF.Sigmoid, bias=rv.t[:, 2 + d, pc:pc + 1]), [pt, rv], [lr])
                        yield
                        V(lambda e: e.tensor_scalar(kk.t[:], k_.t[:], rv.t[:, 4, pc:pc + 1], None, ALU.mult), [k_, rv], [kk])
                        P(lambda e: e.tensor_tensor(t1.t[:], kk.t[:], kk.t[:], ALU.mult), [kk], [t1])
                        pt = pnext()
                        M(lambda e, pt=pt: e.matmul(pt.t[:], blk64.t[:], t1.t[:], start=True, stop=True), [blk64, t1], [pt])
                        A(lambda e, pt=pt: e.activation(t2.t[:], pt.t[:], AF.Sqrt), [pt], [t2])
                        V(lambda e: e.tensor_scalar(t2.t[:], t2.t[:], 1e-12, None, ALU.max), [t2], [t2])
                        V(lambda e: e.reciprocal(t2.t[:], t2.t[:]), [t2], [t2])
                        P(lambda e: e.tensor_tensor(kk.t[:], kk.t[:], t2.t[:], ALU.mult), [kk, t2], [kk])
                        yield
                        V(lambda e: e.tensor_scalar(t1.t[:], lr.t[:], rv.t[:, 5, pc:pc + 1], omka.t[:, pc:pc + 1], ALU.mult, ALU.add), [lr, rv, omka], [t1])
                        P(lambda e: e.tensor_tensor(kd.t[:], k_.t[:], t1.t[:], ALU.mult), [k_, t1], [kd])
                        P(lambda e: e.tensor_tensor(bb.t[:], kk.t[:], lr.t[:], ALU.mult), [kk, lr], [bb])
                        yield
                        V(lambda e: e.tensor_tensor_scan(pref.t[:], rst.t[:], lw.t[:], 0.0, ALU.mult, ALU.add), [rst, lw], [pref])
                        tot_bc = v3(pref)[:, :, 127:128].to_broadcast([128, 4, 128])
                        if d == 0:
                            P(lambda e: e.tensor_copy(cum.t[:], pref.t[:]), [pref], [cum])
                        else:
                            V(lambda e: e.tensor_tensor(cum.t[:], lw.t[:], pref.t[:], ALU.subtract), [lw, pref], [cum])
                            V(lambda e: e.tensor_tensor(v3(cum), v3(cum), tot_bc, ALU.add), [cum, pref], [cum])
                        A(lambda e: e.activation(WC.t[:], v3(pref)[:, :, 127], AF.Exp), [pref], [WC])
                        yield
                        A(lambda e: e.activation(E.t[:], cum.t[:], AF.Exp, scale=-1.0), [cum], [E])
                        V(lambda e: e.tensor_tensor(Bt.t[:], bb.t[:], E.t[:], ALU.mult), [bb, E], [Bt])
                        P(lambda e: e.tensor_tensor(Kt.t[:], kd.t[:], E.t[:], ALU.mult), [kd, E], [Kt])
                        yield
                        V(lambda e: e.tensor_tensor(v3(t1), tot_bc, v3(cum), ALU.subtract), [pref, cum], [t1])
                        A(lambda e: e.activation(E.t[:], t1.t[:], AF.Exp), [t1], [E])
                        V(lambda e: e.tensor_tensor(Bh.t[:], bb.t[:], E.t[:], ALU.mult), [bb, E], [Bh])
                        P(lambda e: e.tensor_tensor(Kh.t[:], kd.t[:], E.t[:], ALU.mult), [kd, E], [Kh])
                        yield
                        A(lambda e: e.activation(E.t[:], cum.t[:], AF.Exp), [cum], [E])
                        V(lambda e: e.tensor_tensor(Rt.t[:], r_.t[:], E.t[:], ALU.mult), [r_, E], [Rt])
                        V(lambda e: e.tensor_tensor(t1.t[:], cum.t[:], lw.t[:], ALU.subtract), [cum, lw], [t1])
                        A(lambda e: e.activation(E.t[:], t1.t[:], AF.Exp), [t1], [E])
                        V(lambda e: e.scalar_tensor_tensor(At.t[:], kk.t[:], -1.0, E.t[:], ALU.mult, ALU.mult), [kk, E], [At])
                        if debug == 3 and d == dbg_d and tb == dbg_tb:
                            for nm_, b__, dt_ in (("r", r_, F32), ("k", k_, F32), ("lw", lw, F32), ("lr", lr, F32), ("kk", kk, F32), ("kd", kd, F32), ("bb", bb, F32),
                                                  ("cum", cum, F32), ("pref", pref, F32), ("At", At, BF16), ("Rt", Rt, BF16), ("Bt", Bt, BF16), ("Kt", Kt, BF16),
                                                  ("Bh", Bh, BF16), ("Kh", Kh, BF16), ("Vb", Vb, BF16)):
                                dump_buf(nm_, b__, [128, 512], dt_)
                            dump_buf("WC", WC, [128, 4])
                        yield
                        for (src_, dst_) in ((At, Atok), (Bh, Bhtok), (Kh, Khtok), (Vb, Vtok)):
                            pt = pnext()
                            for c in range(4):
                                M(lambda e, pt=pt, src_=src_, c=c: e.matmul(pt.t[:, c * 128:(c + 1) * 128], src_.t[:, c * 128:(c + 1) * 128], identb[:], start=True, stop=True),
                                  [src_, Bidentb], [pt], sig=(c == 3))
                            A(lambda e, pt=pt, dst_=dst_: e.copy(dst_.t[:].rearrange("p c t -> p (c t)"), pt.t[:]), [pt], [dst_])
                        yield
                        for h in range(2):
                            hs = slice(h * 64, h * 64 + 64)
                            H = hb[h]
                            for (nm, lh, rh, mk) in (("N", Bt, At, mN), ("NT", At, Bt, mNT), ("AkT", Kt, At, mN), ("ArbT", Bt, Rt, mI), ("ArkT", Kt, Rt, mI)):
                                pt = pnext()
                                for c in range(4):
                                    M(lambda e, pt=pt, lh=lh, rh=rh, c=c, hs=hs: e.matmul(pt.t[:, c * 128:(c + 1) * 128], lh.t[hs, c * 128:(c + 1) * 128], rh.t[hs, c * 128:(c + 1) * 128], start=True, stop=True),
                                      [lh, rh], [pt], sig=(c == 3))
                                V(lambda e, pt=pt, nm=nm, mk=mk, H=H: e.tensor_tensor(H[nm].t[:], pt.t[:].rearrange("p (c t) -> p c t", t=128), cmask.t[:, mk:mk + 1, :].to_broadcast([128, 4, 128]), ALU.mult),
                                  [pt, cmask], [H[nm]])
                        yield
                        c4 = lambda b_: b_.t[:].rearrange("p c t -> p (c t)")
                        mk4 = lambda m_: cmask.t[:, m_:m_ + 1, :].to_broadcast([128, 4, 128])
                        idb = identb[:].rearrange("p (o t) -> p o t", o=1).to_broadcast([128, 4, 128])

                        def mm4(lhs, rhs):
                            pt_ = pnext()
                            for c in range(4):
                                M(lambda e, pt_=pt_, lhs=lhs, rhs=rhs, c=c: e.matmul(pt_.t[:, c * 128:(c + 1) * 128], lhs.t[:, c, :], rhs.t[:, c, :], start=True, stop=True),
                                  [lhs, rhs], [pt_], sig=(c == 3))
                            return pt_

                        def cp4(dst, pt_, eng):
                            if eng == "act":
                                A(lambda e, dst=dst, pt_=pt_: e.copy(c4(dst), pt_.t[:]), [pt_], [dst])
                            else:
                                V(lambda e, dst=dst, pt_=pt_: e.tensor_copy(c4(dst), pt_.t[:]), [pt_], [dst])

                        def acc4(dst, pt_):
                            V(lambda e, dst=dst, pt_=pt_: e.tensor_tensor(c4(dst), pt_.t[:], c4(dst), ALU.add), [pt_, dst], [dst])

                        def inv_gen(H):
                            TTb, Ttb = H["TT"], H["Tt"]
                            Nk, NTk = H["Na"], H["NTa"]
                            P(lambda e, Nk=Nk: e.tensor_tensor(Nk.t[:], H["N"].t[:], mk4(4), ALU.mult), [H["N"], cmask], [Nk])
                            P(lambda e, NTk=NTk: e.tensor_tensor(NTk.t[:], H["NT"].t[:], mk4(4), ALU.mult), [H["NT"], cmask], [NTk])
                            V(lambda e, Nk=Nk: e.tensor_tensor(TTb.t[:], Nk.t[:], idb, ALU.add), [Nk, Bidentb], [TTb])
                            V(lambda e, NTk=NTk: e.tensor_tensor(Ttb.t[:], NTk.t[:], idb, ALU.add), [NTk, Bidentb], [Ttb])
                            yield
                            for lev in range(3):
                                N2, NT2 = (H["Nb"], H["NTb"]) if lev % 2 == 0 else (H["Na"], H["NTa"])
                                p1 = mm4(Nk, NTk)
                                p2 = mm4(NTk, Nk)
                                yield
                                cp4(NT2, p1, "act"); cp4(N2, p2, "act")
                                yield
                                p3 = mm4(NT2, TTb); p4 = mm4(N2, Ttb)
                                yield
                                acc4(TTb, p3); acc4(Ttb, p4)
                                yield
                                Nk, NTk = N2, NT2
                            for mi, mk_ in enumerate((5, 6, 7)):
                                O_, Ot_, X_, Xt_ = H["Na"], H["NTa"], H["Nb"], H["NTb"]
                                last = (mi == 2)
                                P(lambda e, O_=O_, mk_=mk_: e.tensor_tensor(O_.t[:], H["N"].t[:], mk4(mk_), ALU.mult), [H["N"], cmask], [O_])
                                P(lambda e, Ot_=Ot_, mk_=mk_: e.tensor_tensor(Ot_.t[:], H["NT"].t[:], mk4(mk_), ALU.mult), [H["NT"], cmask], [Ot_])
                                yield
                                px = mm4(Ot_, TTb)
                                pxt = mm4(O_, Ttb) if not last else None
                                yield
                                cp4(X_, px, "act")
                                if not last:
                                    cp4(Xt_, pxt, "act")
                                yield
                                pa = mm4(Ttb, X_)
                                pb = mm4(TTb, Xt_) if not last else None
                                yield
                                acc4(TTb, pa)
                                if not last:
                                    acc4(Ttb, pb)
                                yield

                        gens = [inv_gen(hb[0]), inv_gen(hb[1])]
                        while gens:
                            for g_ in list(gens):
                                try:
                                    next(g_)
                                except StopIteration:
                                    gens.remove(g_)
                        if debug == 3 and d == dbg_d and tb == dbg_tb:
                            dump_buf("Atok", Atok, [128, 4, 128], BF16); dump_buf("Vtok", Vtok, [128, 4, 128], BF16)
                            for h in range(2):
                                for nm_ in ("AkT", "ArbT", "ArkT", "TT"):
                                    dump_buf("%s%d" % (nm_, h), hb[h][nm_], [128, 4, 128], BF16)
                            dump_buf("N1", hb[1]["N"], [128, 4, 128], BF16); dump_buf("NT1", hb[1]["NT"], [128, 4, 128], BF16)
                        yield
                        pt = pnext()
                        for h in range(2):
                            for c in range(4):
                                M(lambda e, pt=pt, h=h, c=c: e.matmul(pt.t[:, c * 128 + h * 64:c * 128 + h * 64 + 64], hb[h]["TT"].t[:, c, :], Atok.t[:, c, h * 64:h * 64 + 64], start=True, stop=True),
                                  [hb[h]["TT"], Atok], [pt], sig=(h == 1 and c == 3))
                        A(lambda e, pt=pt: e.copy(Gtok.t[:].rearrange("p c t -> p (c t)"), pt.t[:]), [pt], [Gtok])
                        yield
                        pt = pnext()
                        for h in range(2):
                            for c in range(4):
                                M(lambda e, pt=pt, h=h, c=c: e.matmul(pt.t[:, c * 128 + h * 64:c * 128 + h * 64 + 64], hb[h]["AkT"].t[:, c, :], Vtok.t[:, c, h * 64:h * 64 + 64], start=True, stop=True),
                                  [hb[h]["AkT"], Vtok], [pt], sig=(h == 1 and c == 3))
                        V(lambda e, pt=pt: e.tensor_copy(AkVs.t[:].rearrange("p c t -> p (c t)"), pt.t[:]), [pt], [AkVs])
                        yield
                        pt = pnext()
                        for h in range(2):
                            for c in range(4):
                                M(lambda e, pt=pt, h=h, c=c: e.matmul(pt.t[:, c * 128 + h * 64:c * 128 + h * 64 + 64], hb[h]["TT"].t[:, c, :], AkVs.t[:, c, h * 64:h * 64 + 64], start=True, stop=True),
                                  [hb[h]["TT"], AkVs], [pt], sig=(h == 1 and c == 3))
                        A(lambda e, pt=pt: e.copy(U0tok.t[:].rearrange("p c t -> p (c t)"), pt.t[:]), [pt], [U0tok])
                        yield
                        ptY = pnext()
                        for h in range(2):
                            hs = slice(h * 64, h * 64 + 64)
                            for c in range(4):
                                M(lambda e, h=h, c=c, hs=hs, ptY=ptY: e.matmul(ptY.t[hs, c * 128:(c + 1) * 128], U0tok.t[:, c, hs], hb[h]["ArbT"].t[:, c, :], start=True, stop=False), [U0tok, hb[h]["ArbT"]], [ptY], sig=False)
                                M(lambda e, h=h, c=c, hs=hs, ptY=ptY: e.matmul(ptY.t[hs, c * 128:(c + 1) * 128], Vtok.t[:, c, hs], hb[h]["ArkT"].t[:, c, :], start=False, stop=True), [Vtok, hb[h]["ArkT"]], [ptY], sig=(h == 1 and c == 3))
                        A(lambda e, ptY=ptY: e.copy(Y0s.t[:], ptY.t[:]), [ptY], [Y0s])
                        ptZ = pnext()
                        for h in range(2):
                            hs = slice(h * 64, h * 64 + 64)
                            for c in range(4):
                                M(lambda e, c=c, hs=hs, ptZ=ptZ: e.matmul(ptZ.t[hs, c * 64:(c + 1) * 64], Bhtok.t[:, c, hs], U0tok.t[:, c, hs], start=True, stop=False), [Bhtok, U0tok], [ptZ], sig=False)
                                M(lambda e, c=c, hs=hs, ptZ=ptZ: e.matmul(ptZ.t[hs, c * 64:(c + 1) * 64], Khtok.t[:, c, hs], Vtok.t[:, c, hs], start=False, stop=True), [Khtok, Vtok], [ptZ], sig=(h == 1 and c == 3))
                        V(lambda e, ptZ=ptZ: e.tensor_copy(Zloc.t[:].rearrange("p c j -> p (c j)"), ptZ.t[:, 0:256]), [ptZ], [Zloc])
                        ptP = pnext()
                        for h in range(2):
                            hs = slice(h * 64, h * 64 + 64)
                            for c in range(4):
                                M(lambda e, c=c, hs=hs, ptP=ptP: e.matmul(ptP.t[hs, c * 64:(c + 1) * 64], Gtok.t[:, c, hs], Bhtok.t[:, c, hs], start=True, stop=True), [Gtok, Bhtok], [ptP], sig=(h == 1 and c == 3))
                        for c in range(4):
                            V(lambda e, c=c, ptP=ptP: e.scalar_tensor_tensor(PhiT.t[:, c, :], ident2.t[:], WC.t[:, c:c + 1], ptP.t[:, c * 64:(c + 1) * 64], ALU.mult, ALU.add), [ident2, WC, ptP], [PhiT])
                        ptQ = pnext()
                        for h in range(2):
                            hs = slice(h * 64, h * 64 + 64)
                            for c in range(4):
                                M(lambda e, h=h, c=c, hs=hs, ptQ=ptQ: e.matmul(ptQ.t[hs, c * 128:(c + 1) * 128], Gtok.t[:, c, hs], hb[h]["ArbT"].t[:, c, :], start=True, stop=True), [Gtok, hb[h]["ArbT"]], [ptQ], sig=(h == 1 and c == 3))
                        V(lambda e, ptQ=ptQ: e.tensor_tensor(QT.t[:], ptQ.t[:], Rt.t[:], ALU.add), [ptQ, Rt], [QT])
                        if debug == 3 and d == dbg_d and tb == dbg_tb:
                            dump_buf("Gtok", Gtok, [128, 4, 128], BF16); dump_buf("U0tok", U0tok, [128, 4, 128], BF16)
                            dump_buf("Y0s", Y0s, [128, 512]); dump_buf("Zloc", Zloc, [128, 4, 64]); dump_buf("PhiT", PhiT, [128, 4, 64]); dump_buf("QT", QT, [128, 512])
                            dump_buf("Zin", Z, [128, 64])
                        yield
                        corder = range(4) if d == 0 else range(3, -1, -1)
                        for ci, c in enumerate(corder):
                            for h in range(2):
                                hs = slice(h * 64, h * 64 + 64)
                                M(lambda e, c=c, hs=hs: e.matmul(py.t[hs, c * 128:(c + 1) * 128], Z.t[hs, :], QT.t[hs, c * 128:(c + 1) * 128], start=True, stop=True), [Z, QT], [py], sig=False)
                                M(lambda e, c=c, hs=hs: e.matmul(pz.t[hs, :], PhiT.t[hs, c, :], Z.t[hs, :], start=True, stop=True), [PhiT, Z], [pz], sig=(h == 1))
                            V(lambda e, c=c: e.tensor_tensor(Z.t[:], pz.t[:], Zloc.t[:, c, :], ALU.add), [pz, Zloc], [Z])
                        V(lambda e: e.tensor_tensor(Y0s.t[:], py.t[:], Y0s.t[:], ALU.add), [py, Y0s], [Y0s])
                        S.dma("sp", YD[d][:, ts_], Y0s.t[:], "yst" + sfx, reads=[Y0s.k])
                        yield

                    for d in range(2):
                        P(lambda e, d=d: e.memset(WSP[d]["Z"].t[:], 0.0), [], [WSP[d]["Z"]])
                    ords = [list(range(NB)), list(range(NB - 1, -1, -1))]
                    seqs = [iter([block_gen(d_, tb, WSP[d_], i_, (ords[d_][i_ + 1] if i_ + 1 < NB else None)) for i_, tb in enumerate(ords[d_])]) for d_ in range(2)]
                    cur = [next(seqs[0]), next(seqs[1])]
                    while any(c_ is not None for c_ in cur):
                        for di in range(2):
                            if cur[di] is None:
                                continue
                            try:
                                next(cur[di])
                            except StopIteration:
                                cur[di] = next(seqs[di], None)
                    if debug == 3:
                        flush_dumps()

                    S.barrier(); S.emit()
                with ExitStack() as p2:
                    raw = B(p2, "rawo", [128, TOK + 2])
                    ro = B(p2, "ro", [128, TOK]); ko = B(p2, "ko", [128, TOK]); vo = B(p2, "vo", [128, TOK])
                    ys = B(p2, "ys", [128, TOK]); u1 = B(p2, "u1", [128, TOK]); u2 = B(p2, "u2", [128, TOK]); u3 = B(p2, "u3", [128, TOK])
                    po = [PB(p2, "po%d" % i, [128, 512]) for i in range(6)]
                    load_shift(p2, ro, PH, 16 + pc, (16 + pc) * 128, HALO, TOK, False, False, raw)
                    load_shift(p2, ko, PH, 24 + pc, (24 + pc) * 128, HALO, TOK, False, False, raw)
                    load_shift(p2, vo, PH, 32 + pc, (32 + pc) * 128, HALO, TOK, False, False, raw)
                    yl = [B(p2, "yl0", [128, TOK]), B(p2, "yl1", [128, TOK])]
                    for q_ in range(4):
                        for d in range(2):
                            LD("sp", yl[d].t[:], YD[d][:, q_ * TOK:(q_ + 1) * TOK], "yl%d" % d, [yl[d]])
                            if q_ == 0 and d == 0:
                                V(lambda e, d=d, q_=q_: e.tensor_scalar(ys.t[:], yl[d].t[:], qflag[:, q_:q_ + 1], None, ALU.mult), [yl[d], Bqflag], [ys])
                            else:
                                V(lambda e, d=d, q_=q_: e.scalar_tensor_tensor(ys.t[:], yl[d].t[:], qflag[:, q_:q_ + 1], ys.t[:], ALU.mult, ALU.add), [yl[d], Bqflag, ys], [ys])
                    for half in range(2):
                        hsl = slice(half * 512, half * 512 + 512)
                        for d in range(2):
                            M(lambda e, d=d, hsl=hsl, half=half: e.matmul(po[d].t[:], a2w.t[d * 64:d * 64 + 64, cs_], ado.t[d * 64:d * 64 + 64, hsl], start=True, stop=True), [a2w, ado], [po[d]])
                        A(lambda e, hsl=hsl: e.activation(u1.t[:, hsl], po[0].t[:], AF.Sigmoid, bias=rv.t[:, 2, pc:pc + 1]), [po[0], rv], [u1])
                        A(lambda e, hsl=hsl: e.activation(u2.t[:, hsl], po[1].t[:], AF.Sigmoid, bias=rv.t[:, 3, pc:pc + 1]), [po[1], rv], [u2])
                    V(lambda e: e.tensor_tensor(u1.t[:], u1.t[:], u2.t[:], ALU.add), [u1, u2], [u1])
                    V(lambda e: e.tensor_scalar(u2.t[:], omka.t[:, pc:pc + 1].to_broadcast([128, TOK]), 2.0, None, ALU.mult), [omka], [u2])
                    V(lambda e: e.scalar_tensor_tensor(u1.t[:], u1.t[:], rv.t[:, 5, pc:pc + 1], u2.t[:], ALU.mult, ALU.add), [u1, rv, u2], [u1])
                    V(lambda e: e.tensor_tensor(u1.t[:], u1.t[:], ko.t[:], ALU.mult), [u1, ko], [u1])
                    V(lambda e: e.scalar_tensor_tensor(u1.t[:], u1.t[:], rv.t[:, 6, pc:pc + 1], ro.t[:], ALU.mult, ALU.mult), [u1, rv, ro], [u1])
                    A(lambda e: e.activation(u2.t[:], ys.t[:], AF.Square), [ys], [u2])
                    for half in range(2):
                        hsl = slice(half * 512, half * 512 + 512)
                        M(lambda e, hsl=hsl, half=half: e.matmul(po[0 + half].t[:], blk64.t[:], ys.t[:, hsl], start=True, stop=True), [blk64, ys], [po[0 + half]])
                        M(lambda e, hsl=hsl, half=half: e.matmul(po[2 + half].t[:], blk64.t[:], u2.t[:, hsl], start=True, stop=True), [blk64, u2], [po[2 + half]])
                        M(lambda e, hsl=hsl, half=half: e.matmul(po[4 + half].t[:], blk64.t[:], u1.t[:, hsl], start=True, stop=True), [blk64, u1], [po[4 + half]])
                    for half in range(2):
                        hsl = slice(half * 512, half * 512 + 512)
                        A(lambda e, hsl=hsl, half=half: e.mul(u3.t[:, hsl], po[0 + half].t[:], 1.0 / 64), [po[0 + half]], [u3])
                        V(lambda e, hsl=hsl, half=half: e.tensor_tensor(u1.t[:, hsl], po[4 + half].t[:], vo.t[:, hsl], ALU.mult), [po[4 + half], vo], [u1])
                        V(lambda e, hsl=hsl: e.tensor_tensor(ro.t[:, hsl], u3.t[:, hsl], u3.t[:, hsl], ALU.mult), [u3], [ro])
                        V(lambda e, hsl=hsl, half=half: e.scalar_tensor_tensor(u2.t[:, hsl], po[2 + half].t[:], 1.0 / 64, ro.t[:, hsl], ALU.mult, ALU.subtract), [po[2 + half], ro], [u2])
                    V(lambda e: e.tensor_scalar(u2.t[:], u2.t[:], GN_EPS, None, ALU.add), [u2], [u2])
                    A(lambda e: e.activation(u2.t[:], u2.t[:], AF.Sqrt), [u2], [u2])
                    V(lambda e: e.reciprocal(u2.t[:], u2.t[:]), [u2], [u2])
                    V(lambda e: e.tensor_tensor(ys.t[:], ys.t[:], u3.t[:], ALU.subtract), [ys, u3], [ys])
                    V(lambda e: e.tensor_tensor(ys.t[:], ys.t[:], u2.t[:], ALU.mult), [ys, u2], [ys])
                    V(lambda e: e.tensor_scalar(ys.t[:], ys.t[:], rv.t[:, 7, pc:pc + 1], rv.t[:, 8, pc:pc + 1], ALU.mult, ALU.add), [ys, rv], [ys])
                    V(lambda e: e.tensor_tensor(ys.t[:], ys.t[:], u1.t[:], ALU.add), [ys, u1], [ys])
                    for half in range(2):
                        hsl = slice(half * 512, half * 512 + 512)
                        M(lambda e, hsl=hsl, half=half: e.matmul(po[half].t[:], g2a.t[:, cs_], sga.t[:, hsl], start=True, stop=False), [g2a, sga], [po[half]], sig=False)
                        M(lambda e, hsl=hsl, half=half: e.matmul(po[half].t[:], g2b.t[0:32, cs_], sgb.t[0:32, hsl], start=False, stop=True), [g2b, sgb], [po[half]])
                        V(lambda e, hsl=hsl, half=half: e.tensor_tensor(yTr.t[:, pc, hsl], po[half].t[:], ys.t[:, hsl], ALU.mult), [po[half], ys], [yTr])
                    S.barrier(); S.emit()
        if stop <= 3:
            r_nc = _finish(nc, S, st, dram_in)
            st_y.close()
            return r_nc

        st_y2 = ExitStack()
        yTc = B(st_y2, "yTc", [128, 8, TOK], BF16)
        with ExitStack() as ph:
            W = TOK + KW - 1
            zc = B(ph, "zc", [128, 8, TOK]); val = B(ph, "val", [128, W]); gate = B(ph, "gate", [128, W]); z = B(ph, "z", [128, W])
            sq = B(ph, "csq", [128, TOK]); mean = B(ph, "cmean", [128, TOK]); rstd = B(ph, "crstd", [128, TOK]); tmpc = B(ph, "ctmp", [128, TOK])
            pS = [PB(ph, "pS%d" % i, [128, 512]) for i in range(2)]; pQ = [PB(ph, "pQ%d" % i, [128, 512]) for i in range(2)]
            c_lo = HALO - KW // 2
            for cc in range(8):
                LD("sp", val.t[:], PH[cc * 128:(cc + 1) * 128, c_lo:c_lo + W], "cv", [val])
                LD("act", gate.t[:], PH[(8 + cc) * 128:(9 + cc) * 128, c_lo:c_lo + W], "cg", [gate])
                A(lambda e: e.activation(gate.t[:], gate.t[:], AF.Sigmoid), [gate], [gate])
                V(lambda e: e.tensor_tensor(z.t[:], val.t[:], gate.t[:], ALU.mult), [val, gate], [z])
                V(lambda e, cc=cc: e.tensor_scalar(zc.t[:, cc, :], z.t[:, 0:TOK], convw.t[:, cc, 0:1], convv.t[:, 0, cc:cc + 1], ALU.mult, ALU.add), [z, convw, convv], [zc])
                for k in range(1, KW):
                    V(lambda e, cc=cc, k=k: e.scalar_tensor_tensor(zc.t[:, cc, :], z.t[:, k:k + TOK], convw.t[:, cc, k:k + 1], zc.t[:, cc, :], ALU.mult, ALU.add), [z, convw, zc], [zc])
                A(lambda e, cc=cc: e.activation(sq.t[:], zc.t[:, cc, :], AF.Square), [zc], [sq])
                for half in range(2):
                    hsl = slice(half * 512, half * 512 + 512)
                    M(lambda e, cc=cc, hsl=hsl, half=half: e.matmul(pS[half].t[:], ones.t[:], zc.t[:, cc, hsl], start=(cc == 0), stop=(cc == 7)), [ones, zc], [pS[half]])
                    M(lambda e, cc=cc, hsl=hsl, half=half: e.matmul(pQ[half].t[:], ones.t[:], sq.t[:, hsl], start=(cc == 0), stop=(cc == 7)), [ones, sq], [pQ[half]])
            for half in range(2):
                hsl = slice(half * 512, half * 512 + 512)
                A(lambda e, hsl=hsl, half=half: e.mul(mean.t[:, hsl], pS[half].t[:], 1.0 / DC), [pS[half]], [mean])
                V(lambda e, hsl=hsl: e.tensor_tensor(tmpc.t[:, hsl], mean.t[:, hsl], mean.t[:, hsl], ALU.mult), [mean], [tmpc])
                V(lambda e, hsl=hsl, half=half: e.scalar_tensor_tensor(rstd.t[:, hsl], pQ[half].t[:], 1.0 / DC, tmpc.t[:, hsl], ALU.mult, ALU.subtract), [pQ[half], tmpc], [rstd])
            V(lambda e: e.tensor_scalar(rstd.t[:], rstd.t[:], LN_EPS, None, ALU.add), [rstd], [rstd])
            A(lambda e: e.activation(rstd.t[:], rstd.t[:], AF.Sqrt), [rstd], [rstd])
            V(lambda e: e.reciprocal(rstd.t[:], rstd.t[:]), [rstd], [rstd])
            for cc in range(8):
                V(lambda e, cc=cc: e.tensor_tensor(tmpc.t[:], zc.t[:, cc, :], mean.t[:], ALU.subtract), [zc, mean], [tmpc])
                V(lambda e: e.tensor_tensor(tmpc.t[:], tmpc.t[:], rstd.t[:], ALU.mult), [tmpc, rstd], [tmpc])
                A(lambda e, cc=cc: e.activation(yTc.t[:, cc, :], tmpc.t[:], AF.Silu, bias=convv.t[:, 2, cc:cc + 1], scale=convv.t[:, 1, cc:cc + 1]), [tmpc, convv], [yTc])
            S.barrier(); S.emit()
        if stop <= 4:
            r_nc = _finish(nc, S, st, dram_in)
            st_y2.close(); st_y.close()
            return r_nc

        wout_d = din("w_out_r", [16, 128, 16, 128])
        st_x = ExitStack()
        xT = sb(st_x, "xT", [128, 16, TOK]); t_xT = Tok("xT")
        BxT = Buf(xT, t_xT)
        with ExitStack() as ph2:
            transpose_pass(ph2, "c", xh[HALO:HALO + TOK, :], TOK // 128, False, lambda fc, o, n: xT[:, fc, o:o + n], t_xT)
            S.barrier(); S.emit()
        with ExitStack() as ph:
            slabs = [B(ph, "wo%d" % i, [128, 16, 128], BF16) for i in range(3)]
            pp = [PB(ph, "ppo%d" % i, [128, 512]) for i in range(4)]
            pi = 0
            for oc in range(16):
                sl = slabs[oc % 3]
                LD("pool", sl.t[:], wout_d[oc], "w%d" % (oc % 3), [sl])
                for half in range(2):
                    hsl = slice(half * 512, half * 512 + 512)
                    pt = pp[pi % 4]; pi += 1
                    for kk in range(16):
                        M(lambda e, pt=pt, sl=sl, kk=kk, hsl=hsl: e.matmul(pt.t[:], sl.t[:, kk, :], (yTc.t[:, kk, hsl] if kk < 8 else yTr.t[:, kk - 8, hsl]), start=(kk == 0), stop=(kk == 15)), [sl, yTc, yTr], [pt], sig=(kk == 15))
                    V(lambda e, pt=pt, oc=oc, hsl=hsl: e.scalar_tensor_tensor(xT[:, oc, hsl], pt.t[:], modv[:, 32 + oc:33 + oc], xT[:, oc, hsl], ALU.mult, ALU.add), [pt, Bmodv, BxT], [BxT])
            S.barrier(); S.emit()
        if stop <= 5:
            r_nc = _finish(nc, S, st, dram_in, dbg_fn=lambda: S.dma("sp", dbg["xT"], xT[:], "dbg", reads=[t_xT], batch=True, is_out=True) if debug else None)
            st_x.close(); st_y2.close(); st_y.close()
            return r_nc

        wr_d = din("wr", [128, 16, 36]); br_d = din("br", [128, 36])
        wg_d = din("wg_r", [NE, 4, 128, 16, 128]); wu_d = din("wu_r", [NE, 4, 128, 16, 128]); wd_d = din("wd_r", [NE, 4, 128, 4, 512])
        WT = nc.dram_tensor("WT", [NE, TOK], F32, kind="Internal").ap()
        with ExitStack() as ph:
            hn2T = B(ph, "hn2T", [128, 16, TOK], BF16)
            with ExitStack() as p2:
                wr = B(p2, "wr", [128, 16, 36]); br = B(p2, "br", [128, 36])
                LD("sp", wr.t[:], wr_d, "const", [wr], batch=True); LD("sp", br.t[:], br_d, "const", [br], batch=True)
                sq = B(p2, "nsq", [128, TOK]); rs2 = B(p2, "rs2", [128, TOK]); hf = B(p2, "hf", [128, TOK])
                pS = [PB(p2, "nS%d" % i, [128, 512]) for i in range(2)]; pL = [PB(p2, "nL%d" % i, [128, 512]) for i in range(2)]
                pT = PB(p2, "nT", [128, 512]); pW = [PB(p2, "nW%d" % i, [128, 512]) for i in range(2)]
                for fc in range(16):
                    A(lambda e, fc=fc: e.activation(sq.t[:], xT[:, fc, :], AF.Square), [BxT], [sq])
                    for half in range(2):
                        hsl = slice(half * 512, half * 512 + 512)
                        M(lambda e, fc=fc, hsl=hsl, half=half: e.matmul(pS[half].t[:], ones.t[:], sq.t[:, hsl], start=(fc == 0), stop=(fc == 15)), [ones, sq], [pS[half]])
                for half in range(2):
                    hsl = slice(half * 512, half * 512 + 512)
                    V(lambda e, hsl=hsl, half=half: e.tensor_scalar(rs2.t[:, hsl], pS[half].t[:], 1.0 / D, RMS_EPS, ALU.mult, ALU.add), [pS[half]], [rs2])
                A(lambda e: e.activation(rs2.t[:], rs2.t[:], AF.Sqrt), [rs2], [rs2])
                V(lambda e: e.reciprocal(rs2.t[:], rs2.t[:]), [rs2], [rs2])
                for fc in range(16):
                    V(lambda e, fc=fc: e.tensor_tensor(hf.t[:], xT[:, fc, :], rs2.t[:], ALU.mult), [BxT, rs2], [hf])
                    V(lambda e, fc=fc: e.tensor_scalar(hf.t[:], hf.t[:], a2[:, fc:fc + 1], modv[:, 48 + fc:49 + fc], ALU.mult, ALU.add), [hf, Ba2, Bmodv], [hf])
                    A(lambda e, fc=fc: e.copy(hn2T.t[:, fc, :], hf.t[:]), [hf], [hn2T])
                    for half in range(2):
                        hsl = slice(half * 512, half * 512 + 512)
                        M(lambda e, fc=fc, hsl=hsl, half=half: e.matmul(pL[half].t[0:36, :], wr.t[:, fc, :], hf.t[:, hsl], start=(fc == 0), stop=(fc == 15)), [wr, hf], [pL[half]])
                LT = B(p2, "LT", [36, TOK]); L = B(p2, "L", [128, 8, 36])
                for half in range(2):
                    A(lambda e, half=half: e.copy(LT.t[:, half * 512:(half + 1) * 512], pL[half].t[0:36, :]), [pL[half]], [LT])
                for t_ in range(8):
                    M(lambda e, t_=t_: e.matmul(pT.t[:, t_ * 36:(t_ + 1) * 36], LT.t[:, t_ * 128:(t_ + 1) * 128], ident[0:36, 0:36], start=True, stop=True), [LT, Bident], [pT], sig=(t_ == 7))
                V(lambda e: e.tensor_tensor(L.t[:], pT.t[:, 0:288].rearrange("p (t c) -> p t c", c=36), br.t[:].rearrange("p (o c) -> p o c", o=1).to_broadcast([128, 8, 36]), ALU.add), [pT, br], [L])
                gl = L.t[:, :, 0:4]
                el = L.t[:, :, 4:36]
                gmax = B(p2, "gmax", [128, 8]); gmask = B(p2, "gmask", [128, 8, 4]); gex = B(p2, "gex", [128, 8, 4]); pg = B(p2, "pg", [128, 8])
                elm = B(p2, "elm", [128, 8, 32]); m1 = B(p2, "m1", [128, 8]); m2 = B(p2, "m2", [128, 8]); k1 = B(p2, "k1", [128, 8, 32]); k2 = B(p2, "k2", [128, 8, 32])
                w1 = B(p2, "w1", [128, 8]); w2_ = B(p2, "w2_", [128, 8]); wt = B(p2, "wt", [128, 8, 32])
                bc8 = lambda b_, n: b_.t[:].rearrange("p (t o) -> p t o", o=1).to_broadcast([128, 8, n])
                V(lambda e: e.tensor_reduce(gmax.t[:], gl, AX.X, ALU.max), [L], [gmax])
                V(lambda e: e.tensor_tensor(gmask.t[:], gl, bc8(gmax, 4), ALU.is_equal), [L, gmax], [gmask])
                V(lambda e: e.tensor_tensor(gex.t[:], gl, bc8(gmax, 4), ALU.subtract), [L, gmax], [gex])
                A(lambda e: e.activation(gex.t[:], gex.t[:], AF.Exp), [gex], [gex])
                V(lambda e: e.tensor_reduce(pg.t[:], gex.t[:], AX.X, ALU.add), [gex], [pg])
                V(lambda e: e.reciprocal(pg.t[:], pg.t[:]), [pg], [pg])
                V(lambda e: e.tensor_scalar(gmask.t[:], gmask.t[:], 1e30, -1e30, ALU.mult, ALU.add), [gmask], [gmask])
                V(lambda e: e.tensor_tensor(elm.t[:].rearrange("p t (g x) -> p t g x", x=8), el.rearrange("p t (g x) -> p t g x", x=8),
                                            gmask.t[:].rearrange("p t (g o) -> p t g o", o=1).to_broadcast([128, 8, 4, 8]), ALU.add), [L, gmask], [elm])
                V(lambda e: e.tensor_reduce(m1.t[:], elm.t[:], AX.X, ALU.max), [elm], [m1])
                V(lambda e: e.tensor_tensor(k1.t[:], elm.t[:], bc8(m1, 32), ALU.is_equal), [elm, m1], [k1])
                V(lambda e: e.scalar_tensor_tensor(elm.t[:], k1.t[:], -1e30, elm.t[:], ALU.mult, ALU.add), [k1, elm], [elm])
                V(lambda e: e.tensor_reduce(m2.t[:], elm.t[:], AX.X, ALU.max), [elm], [m2])
                V(lambda e: e.tensor_tensor(k2.t[:], elm.t[:], bc8(m2, 32), ALU.is_equal), [elm, m2], [k2])
                V(lambda e: e.tensor_tensor(w1.t[:], m1.t[:], m2.t[:], ALU.subtract), [m1, m2], [w1])
                A(lambda e: e.activation(w2_.t[:], w1.t[:], AF.Sigmoid, scale=-1.0), [w1], [w2_])
                A(lambda e: e.activation(w1.t[:], w1.t[:], AF.Sigmoid), [w1], [w1])
                V(lambda e: e.tensor_tensor(w1.t[:], w1.t[:], pg.t[:], ALU.mult), [w1, pg], [w1])
                V(lambda e: e.tensor_tensor(w2_.t[:], w2_.t[:], pg.t[:], ALU.mult), [w2_, pg], [w2_])
                V(lambda e: e.tensor_tensor(wt.t[:], k1.t[:], bc8(w1, 32), ALU.mult), [k1, w1], [wt])
                V(lambda e: e.tensor_tensor(k2.t[:], k2.t[:], bc8(w2_, 32), ALU.mult), [k2, w2_], [k2])
                V(lambda e: e.tensor_tensor(wt.t[:], wt.t[:], k2.t[:], ALU.add), [wt, k2], [wt])
                wtT = B(p2, "wtT", [32, TOK])
                for t_ in range(8):
                    M(lambda e, t_=t_: e.matmul(pW[t_ // 4].t[0:32, (t_ % 4) * 128:(t_ % 4 + 1) * 128], wt.t[:, t_, :], ident[:], start=True, stop=True), [wt, Bident], [pW[t_ // 4]], sig=(t_ % 4 == 3))
                for half in range(2):
                    A(lambda e, half=half: e.copy(wtT.t[:, half * 512:(half + 1) * 512], pW[half].t[0:32, :]), [pW[half]], [wtT])
                S.dma("sp", WT, wtT.t[:], "wt", reads=[wtT.k])
                S.barrier(); S.emit()
            with ExitStack() as p2:
                gu = [B(p2, "gu%d" % i, [128, 16, 128], BF16) for i in range(4)]
                gu = [Buf(g0.t[:], g0.k) for g0 in gu]
                for j4 in range(4):
                    gu.append(Buf(yTr.t[:, 2 * j4:2 * j4 + 2, :].rearrange("p a (b c) -> p (a b) c", c=128)))
                for j4 in range(2, 4):
                    gu.append(Buf(yTc.t[:, 2 * j4:2 * j4 + 2, :].rearrange("p a (b c) -> p (a b) c", c=128)))
                NGU = len(gu)
                wdn = [B(p2, "wdn%d" % i, [128, 4, 512], BF16) for i in range(4)]
                act0 = B(p2, "act", [128, 4, TOK], BF16)
                acts = [Buf(act0.t[:], act0.k), Buf(yTc.t[:, 0:4, :])]
                wb = [B(p2, "wb%d" % i, [128, TOK]) for i in range(2)]
                sl_ = [B(p2, "sl%d" % i, [128, 512]) for i in range(2)]
                pG = [PB(p2, "pG%d" % i, [128, 512]) for i in range(2)]; pU = [PB(p2, "pU%d" % i, [128, 512]) for i in range(2)]
                pD = [PB(p2, "pD%d" % i, [128, 512]) for i in range(4)]
                cnt = {"gi": 0, "di": 0, "si": 0}

                def gate_up(ex):
                    act = acts[ex % 2]
                    wbe = wb[ex % 2]
                    LD("sp", wbe.t[:], WT[ex:ex + 1, :].to_broadcast([128, TOK]), "wb%d" % (ex % 2), [wbe])
                    for dc in range(4):
                        g_ = gu[cnt["gi"] % NGU]; LD("pool", g_.t, wg_d[ex, dc], "gu%d" % (cnt["gi"] % NGU), [g_]); cnt["gi"] += 1
                        u_ = gu[cnt["gi"] % NGU]; LD("pool", u_.t, wu_d[ex, dc], "gu%d" % (cnt["gi"] % NGU), [u_]); cnt["gi"] += 1
                        for half in range(2):
                            hsl = slice(half * 512, half * 512 + 512)
                            for kk in range(16):
                                M(lambda e, g_=g_, kk=kk, hsl=hsl, half=half: e.matmul(pG[half].t[:], g_.t[:, kk, :], hn2T.t[:, kk, hsl], start=(kk == 0), stop=(kk == 15)), [g_, hn2T], [pG[half]], sig=(kk == 15))
                            for kk in range(16):
                                M(lambda e, u_=u_, kk=kk, hsl=hsl, half=half: e.matmul(pU[half].t[:], u_.t[:, kk, :], hn2T.t[:, kk, hsl], start=(kk == 0), stop=(kk == 15)), [u_, hn2T], [pU[half]], sig=(kk == 15))
                            s_ = sl_[cnt["si"] % 2]; cnt["si"] += 1
                            A(lambda e, s_=s_, half=half: e.activation(s_.t[:], pG[half].t[:], AF.Silu), [pG[half]], [s_])
                            P(lambda e, s_=s_, wbe=wbe, hsl=hsl: e.tensor_tensor(s_.t[:], s_.t[:], wbe.t[:, hsl], ALU.mult), [s_, wbe], [s_])
                            V(lambda e, s_=s_, dc=dc, hsl=hsl, half=half, act=act: e.tensor_tensor(act.t[:, dc, hsl], pU[half].t[:], s_.t[:], ALU.mult), [pU[half], s_], [act])

                def down(ex):
                    act = acts[ex % 2]
                    for og in range(4):
                        wd_ = wdn[cnt["di"] % 4]; LD("pool", wd_.t[:], wd_d[ex, og], "wd%d" % (cnt["di"] % 4), [wd_]); cnt["di"] += 1
                        for o4 in range(4):
                            oc = og * 4 + o4
                            for half in range(2):
                                hsl = slice(half * 512, half * 512 + 512)
                                pt = pD[(o4 * 2 + half) % 4]
                                for kk in range(4):
                                    M(lambda e, pt=pt, wd_=wd_, kk=kk, o4=o4, hsl=hsl, act=act: e.matmul(pt.t[:], wd_.t[:, kk, o4 * 128:(o4 + 1) * 128], act.t[:, kk, hsl], start=(kk == 0), stop=(kk == 3)), [wd_, act], [pt], sig=(kk == 3))
                                V(lambda e, pt=pt, oc=oc, hsl=hsl: e.scalar_tensor_tensor(xT[:, oc, hsl], pt.t[:], modv[:, 80 + oc:81 + oc], xT[:, oc, hsl], ALU.mult, ALU.add), [pt, Bmodv, BxT], [BxT])

                for ex in range(NE):
                    gate_up(ex)
                    if ex >= 1:
                        down(ex - 1)
                down(NE - 1)
                S.barrier(); S.emit()
        if stop <= 6:
            r_nc = _finish(nc, S, st, dram_in, dbg_fn=lambda: S.dma("sp", dbg["xT"], xT[:], "dbg", reads=[t_xT], batch=True, is_out=True) if debug else None)
            st_x.close(); st_y2.close(); st_y.close()
            return r_nc

        with ExitStack() as ph:
            dgf = B(ph, "dgf", [128, 16, 128]); sqs = [B(ph, "fsq%d" % i, [128, 128]) for i in range(2)]
            rt = B(ph, "frt", [128, 8]); ot = [B(ph, "ot%d" % i, [128, D]) for i in range(2)]
            pss = PB(ph, "pss", [128, 8]); pf = [PB(ph, "pf%d" % i, [128, 512]) for i in range(4)]
            for fc in range(16):
                V(lambda e, fc=fc: e.tensor_scalar(dgf.t[:, fc, :], ident[:], gnv[:, 2, fc:fc + 1], None, ALU.mult), [Bident, Bgnv], [dgf])
            qi = 0
            for t_ in range(8):
                for fc in range(16):
                    q_ = sqs[qi % 2]; qi += 1
                    A(lambda e, q_=q_, fc=fc, t_=t_: e.activation(q_.t[:], xT[:, fc, t_ * 128:(t_ + 1) * 128], AF.Square), [BxT], [q_])
                    M(lambda e, q_=q_, fc=fc, t_=t_: e.matmul(pss.t[:, t_:t_ + 1], q_.t[:], ones.t[:, 0:1], start=(fc == 0), stop=(fc == 15)), [q_, ones], [pss])
            V(lambda e: e.tensor_scalar(rt.t[:], pss.t[:], 1.0 / D, RMS_EPS, ALU.mult, ALU.add), [pss], [rt])
            A(lambda e: e.activation(rt.t[:], rt.t[:], AF.Sqrt), [rt], [rt])
            V(lambda e: e.reciprocal(rt.t[:], rt.t[:]), [rt], [rt])
            pi = 0
            for t_ in range(8):
                o_ = ot[t_ % 2]
                for fg in range(4):
                    pt = pf[pi % 4]; pi += 1
                    for j4 in range(4):
                        fc = fg * 4 + j4
                        M(lambda e, pt=pt, fc=fc, j4=j4, t_=t_: e.matmul(pt.t[:, j4 * 128:(j4 + 1) * 128], xT[:, fc, t_ * 128:(t_ + 1) * 128], dgf.t[:, fc, :], start=True, stop=True), [BxT, dgf], [pt], sig=(j4 == 3))
                    if fg % 2 == 0:
                        V(lambda e, pt=pt, o_=o_, fg=fg, t_=t_: e.tensor_scalar(o_.t[:, fg * 512:(fg + 1) * 512], pt.t[:], rt.t[:, t_:t_ + 1], None, ALU.mult), [pt, rt], [o_])
                    else:
                        A(lambda e, pt=pt, o_=o_, fg=fg, t_=t_: e.mul(o_.t[:, fg * 512:(fg + 1) * 512], pt.t[:], rt.t[:, t_:t_ + 1]), [pt, rt], [o_])
                S.dma("sp", out_d[t_ * 128:(t_ + 1) * 128, :], o_.t[:], "out%d" % (t_ % 2), reads=[o_.k], is_out=True)
            r_nc = _finish(nc, S, st, dram_in)
        st_x.close(); st_y2.close(); st_y.close()
        return r_nc


_IN_NAMES = []


def _finish(nc, S, st, dram_in=None, dbg_fn=None):
    _IN_NAMES[:] = list(dram_in.keys())
    if dbg_fn is not None:
        dbg_fn()
    S.barrier()
    S.emit(final=True)
    return nc


def _colvec(v, n):
    return np.ascontiguousarray(np.asarray(v, np.float32).reshape(n, 128).T)


def _kslab(w, nchunk):
    K, nc_ = w.shape
    pad = nchunk * 128 - nc_
    if pad:
        w = np.concatenate([w, np.zeros((K, pad), np.float32)], axis=1)
    return np.ascontiguousarray(w.reshape(K // 128, 128, nchunk, 128).transpose(2, 1, 0, 3))


def prep_shared(inp):
    f = lambda k: np.asarray(inp[k], np.float32)
    sh = {}
    sh["w_ada_r"] = _kslab(f("w_ada")[0], 96)
    sh["b_ada_r"] = _colvec(f("b_ada")[0], 96)
    g = np.stack([f("norm1_g")[0], f("norm2_g")[0], f("normf_g")])
    sh["gnorm"] = np.ascontiguousarray(g.reshape(3, 16, 128).transpose(2, 0, 1))
    sh["w_in_r"] = _kslab(f("w_in")[0], NCC)
    sh["ident"] = np.eye(128, dtype=np.float32)
    idx = np.arange(128)
    bm = lambda L: (idx[:, None] // L) == (idx[None, :] // L)
    cm = np.stack([idx[:, None] < idx[None, :], idx[:, None] <= idx[None, :], idx[:, None] > idx[None, :], idx[:, None] >= idx[None, :],
                   bm(16), bm(32) & ~bm(16), bm(64) & ~bm(32), ~bm(64)], axis=1)
    sh["cmask"] = np.ascontiguousarray(cm.astype(np.float32))
    rst = np.ones((128, 512), np.float32); rst[:, ::128] = 0.0
    sh["rst"] = rst
    sh["blk64"] = np.kron(np.eye(2, dtype=np.float32), np.ones((64, 64), np.float32))
    sh["ones"] = np.ones((128, 128), np.float32)
    sh["ident2"] = np.ascontiguousarray(np.concatenate([np.eye(64, dtype=np.float32)] * 2, axis=0))
    mu = np.zeros((2, NCC * 128), np.float32)
    mu[0, 2048:2048 + 3488] = f("mu_prev")[0]
    mu[1, 2048:2048 + 3488] = f("mu_next")[0]
    sh["mu"] = np.ascontiguousarray(mu.reshape(2, NCC, 128).transpose(2, 0, 1))
    rvn = ["w0_f", "w0_b", "a0_f", "a0_b", "k_k", "k_a", "r_k", "lnx_g", "lnx_b"]
    sh["rv"] = np.ascontiguousarray(np.stack([f(k)[0].reshape(8, 128).T for k in rvn], axis=1))
    sh["w2"] = np.ascontiguousarray(np.concatenate([f("w2_f")[0], f("w2_b")[0]], axis=0))
    sh["a2w"] = np.ascontiguousarray(np.concatenate([f("a2_f")[0], f("a2_b")[0]], axis=0))
    sh["g2a"] = np.ascontiguousarray(f("g2")[0][:128])
    sh["g2b"] = np.ascontiguousarray(f("g2")[0][128:160])
    cw = f("conv_w")[0][:, 0, :]
    sh["convw"] = np.ascontiguousarray(cw.reshape(KW, 8, 128).transpose(2, 1, 0))
    sh["convv"] = np.ascontiguousarray(np.stack([f(k)[0].reshape(8, 128).T for k in ("conv_b", "conv_ln_g", "conv_ln_b")], axis=1))
    sh["w_out_r"] = _kslab(f("w_out")[0], 16)
    wr = np.concatenate([f("w_rg")[0], f("w_re")[0]], axis=1)
    sh["wr"] = np.ascontiguousarray(wr.reshape(16, 128, 36).transpose(1, 0, 2))
    br = np.concatenate([f("b_rg")[0], f("b_re")[0]])
    sh["br"] = np.ascontiguousarray(np.broadcast_to(br[None, :], (128, 36)))
    wg, wu, wd = f("w_gate")[0], f("w_up")[0], f("w_down")[0]
    sh["wg_r"] = np.ascontiguousarray(wg.reshape(NE, 16, 128, 4, 128).transpose(0, 3, 2, 1, 4))
    sh["wu_r"] = np.ascontiguousarray(wu.reshape(NE, 16, 128, 4, 128).transpose(0, 3, 2, 1, 4))
    sh["wd_r"] = np.ascontiguousarray(wd.reshape(NE, 4, 128, 4, 512).transpose(0, 3, 2, 1, 4))
    return sh


def prep_core(inp, i):
    b, q = i // 4, i % 4
    x = np.asarray(inp["x"], np.float32)
    m = {}
    m["xf"] = np.ascontiguousarray(x[b])
    lo = q * TOK - HALO
    xh = np.zeros((TH, D), np.float32)
    hm = np.zeros((TH,), np.float32)
    s0, s1 = max(lo, 0), min(lo + TH, SEQ)
    xh[s0 - lo:s1 - lo] = x[b, s0:s1]
    hm[s0 - lo:s1 - lo] = 1.0
    m["xh"] = xh
    m["hmask"] = np.ascontiguousarray(np.broadcast_to(hm[None, :], (128, TH)))
    qf = np.zeros((128, 4), np.float32)
    qf[:, q] = 1.0
    m["qflag"] = qf
    m["c_col"] = _colvec(np.asarray(inp["c"], np.float32)[b], 16)
    return m


_NC_CACHE = {}


def kernel(**inputs):
    if "nc" not in _NC_CACHE:
        _NC_CACHE["nc"] = build()
    nc = _NC_CACHE["nc"]
    sh = prep_shared(inputs)
    in_maps = []
    for i in range(NCORES):
        m = dict(sh)
        m.update(prep_core(inputs, i))
        in_maps.append({k: m[k] for k in _IN_NAMES})
    res = run_bass_kernel_spmd(nc, in_maps, core_ids=list(range(NCORES)))
    out = np.zeros((2, SEQ, D), np.float32)
    for i in range(NCORES):
        b, q = i // 4, i % 4
        out[b, q * TOK:(q + 1) * TOK] = res.results[i]["out"]
    return out
```

```python
import numpy as np
from contextlib import ExitStack
import concourse.bass as bass
import concourse.mybir as mybir
from concourse.bass_utils import run_bass_kernel_spmd

F32 = mybir.dt.float32
BF16 = mybir.dt.bfloat16
AF = mybir.ActivationFunctionType
ALU = mybir.AluOpType
AX = mybir.AxisListType

NCORES = 8
D = 2048
SEQ = 4096
TOK = 1024
HALO = 64
TH = TOK + 2 * HALO
DC = 1024
DR = 1024
NCOL = 5536
NCC = 44
NRC = 28
KW = 31
NE = 32
DE = 512
CH = 128
RMS_EPS = 1e-6
LN_EPS = 1e-5
GN_EPS = 64e-5


class Ev:
    __slots__ = ("sem", "val")

    def __init__(self, sem, val=None):
        self.sem = sem
        self.val = val


class Tok:
    __slots__ = ("w", "r", "name")

    def __init__(self, name=""):
        self.w = None
        self.r = []
        self.name = name


class DSem:
    def __init__(self, sem, batch):
        self.sem = sem
        self.total = 0
        self.batch = batch
        self.ev = Ev(sem, 0) if batch else None


class Sched:
    ENG = ("pe", "act", "dve", "pool", "sp")

    def __init__(self, nc, stack, n_dma_sems=72):
        self.nc = nc
        self.sem = {e: stack.enter_context(nc.semaphore("s_" + e)) for e in self.ENG}
        self.count = {e: 0 for e in self.ENG}
        self.ops = {e: [] for e in self.ENG}
        self.pending = {e: [] for e in self.ENG}
        self.free_dsems = [stack.enter_context(nc.semaphore("d%d" % i)) for i in range(n_dma_sems)]
        self.dsems = {}
        self.out_evs = []
        self.waited = {e: {} for e in self.ENG}
        self.nops = 0

    def dsem(self, key, batch=False):
        if key not in self.dsems:
            self.dsems[key] = DSem(self.free_dsems.pop(), batch)
        return self.dsems[key]

    def _collect(self, reads, writes):
        waits = []
        for t in reads:
            if t.w is not None:
                waits.append(t.w)
        for t in writes:
            if t.w is not None:
                waits.append(t.w)
            waits.extend(t.r)
        return waits

    def op(self, eng, fn, reads=(), writes=(), signal=True):
        waits = self._collect(reads, writes)
        ev = Ev(self.sem[eng])
        if signal:
            self.count[eng] += 1
            ev.val = self.count[eng]
            for p in self.pending[eng]:
                p.val = ev.val
            self.pending[eng] = []
        else:
            self.pending[eng].append(ev)
        self.ops[eng].append((waits, fn, (self.sem[eng], 1) if signal else None))
        for t in reads:
            if len(t.r) > 6:
                t.r = t.r[-6:] + [e for e in t.r[:-6] if e.val is None]
            t.r.append(ev)
        for t in writes:
            t.w = ev
            t.r = []
        self.nops += 1
        return ev

    def dma(self, q, out, in_, key, reads=(), writes=(), batch=False, is_out=False, **kw):
        ds = self.dsem(key, batch)
        waits = self._collect(reads, writes)
        ds.total += 16
        if ds.batch:
            ev = ds.ev
            ev.val = ds.total
        else:
            ev = Ev(ds.sem, ds.total)

        def fn(e, out=out, in_=in_, kw=kw):
            return e.dma_start(out=out, in_=in_, **kw)

        self.ops[q].append((waits, fn, (ds.sem, 16)))
        for t in reads:
            t.r.append(ev)
        for t in writes:
            t.w = ev
            t.r = []
        if is_out:
            self.out_evs.append(ev)
        self.nops += 1
        return ev

    def barrier(self):
        evs = []
        for e in self.ENG:
            assert not self.pending[e], "engine %s has non-signalled tail" % e
            if self.count[e]:
                evs.append(Ev(self.sem[e], self.count[e]))
        for ds in self.dsems.values():
            if ds.total:
                evs.append(Ev(ds.sem, ds.total))
        for e in self.ENG:
            self.ops[e].append((list(evs), None, None))

    def emit(self, final=False):
        nc = self.nc
        if final:
            self.ops["sp"].append((list(self.out_evs), None, None))
        for e in self.ENG:
            assert not self.pending[e], "engine %s ends with non-signaling op" % e
        with nc.Block() as block:
            def mk(ename):
                def body(eng):
                    waited = self.waited[ename]
                    for waits, fn, inc in self.ops[ename]:
                        need = {}
                        for ev in waits:
                            assert ev.val is not None
                            if ename == "pe" and ev.sem is self.sem["pe"]:
                                continue
                            k = id(ev.sem)
                            if need.get(k, (None, 0))[1] < ev.val:
                                need[k] = (ev.sem, ev.val)
                        for k, (s, v) in need.items():
                            if waited.get(k, 0) >= v:
                                continue
                            waited[k] = v
                            eng.wait_ge(s, v)
                        if fn is not None:
                            ins = fn(eng)
                            if inc is not None:
                                ins.then_inc(inc[0], inc[1])
                    self.ops[ename] = []
                return body
            block.tensor(mk("pe"))
            block.scalar(mk("act"))
            block.vector(mk("dve"))
            block.gpsimd(mk("pool"))
            block.sync(mk("sp"))


class Ring:
    def __init__(self, aps, name):
        self.aps = aps
        self.toks = [Tok("%s%d" % (name, i)) for i in range(len(aps))]
        self.i = 0

    def next(self):
        k = self.i % len(self.aps)
        self.i += 1
        return self.aps[k], self.toks[k], k


def build(stop=99, debug=False, dbg_pc=1, dbg_d=0, dbg_tb=0):
    nc = bass.Bass("TRN2", target_bir_lowering=False)
    dram_in = {}

    def din(name, shape, dt=F32):
        dram_in[name] = nc.dram_tensor(name, list(shape), dt, kind="ExternalInput").ap()
        return dram_in[name]

    xf = din("xf", [SEQ, D])
    xh = din("xh", [TH, D])
    hmask_d = din("hmask", [128, TH])
    qflag_d = din("qflag", [128, 4])
    ccol_d = din("c_col", [128, 16])
    wada_d = din("w_ada_r", [96, 128, 16, 128])
    bada_d = din("b_ada_r", [128, 96])
    gn_d = din("gnorm", [128, 3, 16])
    win_d = din("w_in_r", [NCC, 128, 16, 128])
    ident_d = din("ident", [128, 128])
    out_d = nc.dram_tensor("out", [TOK, D], F32, kind="ExternalOutput").ap()

    PT = nc.dram_tensor("PT", [NRC * 128, SEQ], F32, kind="ExternalOutput" if debug == 2 else "Internal").ap()
    PH = nc.dram_tensor("PH", [NCC * 128, TH], F32, kind="ExternalOutput" if debug == 2 else "Internal").ap()
    dbg = {}
    if debug:
        dbg["modv"] = nc.dram_tensor("dbg_modv", [128, 96], F32, kind="ExternalOutput").ap()
        dbg["xT"] = nc.dram_tensor("dbg_xT", [128, 16, TOK], F32, kind="ExternalOutput").ap()
    dbg_y = nc.dram_tensor("dbg_y", [128, 16, TOK], BF16, kind="ExternalOutput").ap() if debug else None

    with ExitStack() as st:
        S = Sched(nc, st)

        uid = [0]

        def sb(stack, name, shape, dt=F32):
            uid[0] += 1
            return stack.enter_context(nc.sbuf_tensor("sb%d_%s" % (uid[0], name), list(shape), dt))

        def ps(stack, name, shape, dt=F32):
            uid[0] += 1
            return stack.enter_context(nc.psum_tensor("ps%d_%s" % (uid[0], name), list(shape), dt))

        ident = sb(st, "ident", [128, 128]); t_ident = Tok("ident")
        identb = sb(st, "identb", [128, 128], BF16); t_identb = Tok("identb")
        modv = sb(st, "modv", [128, 96]); t_modv = Tok("modv")
        gnv = sb(st, "gnv", [128, 3, 16]); t_gnv = Tok("gnv")
        a1 = sb(st, "a1", [128, 16]); t_a1 = Tok("a1")
        a2 = sb(st, "a2", [128, 16]); t_a2 = Tok("a2")
        qflag = sb(st, "qflag", [128, 4]); t_qflag = Tok("qflag")

        stage_tbl = {}
        if debug == 3:
            for nm_ in ("r", "k", "lw", "lr", "kk", "kd", "bb", "cum", "pref", "Y0s", "QT"):
                stage_tbl[nm_] = ([128, 512], F32)
            for nm_ in ("At", "Rt", "Bt", "Kt", "Bh", "Kh", "Vb"):
                stage_tbl[nm_] = ([128, 512], BF16)
            for nm_ in ("Atok", "Vtok", "AkT0", "ArbT0", "ArkT0", "TT0", "AkT1", "ArbT1", "ArkT1", "TT1", "N1", "NT1", "Gtok", "U0tok"):
                stage_tbl[nm_] = ([128, 4, 128], BF16)
            stage_tbl["WC"] = ([128, 4], F32); stage_tbl["Zloc"] = ([128, 4, 64], F32); stage_tbl["PhiT"] = ([128, 4, 64], F32); stage_tbl["Zin"] = ([128, 64], F32)
        stage_slots = {k_: (sb(st, "stage_" + k_, v_[0], v_[1]), Tok()) for k_, v_ in stage_tbl.items()}
        S.dma("sp", ident[:], ident_d, "const", writes=[t_ident], batch=True)
        S.dma("pool", identb[:], ident_d, "constc", writes=[t_identb], batch=True)
        S.dma("sp", gnv[:], gn_d, "const", writes=[t_gnv], batch=True)
        S.dma("sp", qflag[:], qflag_d, "const", writes=[t_qflag], batch=True)

        with ExitStack() as ph:
            ccol = sb(ph, "ccol", [128, 16]); t_ccol = Tok("ccol")
            cs = sb(ph, "cs", [128, 16]); t_cs = Tok("cs")
            bada = sb(ph, "bada", [128, 96]); t_bada = Tok("bada")
            slabs = [sb(ph, "wa%d" % i, [128, 16, 128]) for i in range(6)]
            ring = Ring(slabs, "wa")
            pmod = ps(ph, "pmod", [128, 96]); t_pmod = Tok("pmod")
            S.dma("sp", ccol[:], ccol_d, "const", writes=[t_ccol], batch=True)
            S.dma("sp", bada[:], bada_d, "const", writes=[t_bada], batch=True)
            S.op("act", lambda e: e.activation(cs[:], ccol[:], AF.Silu), reads=[t_ccol], writes=[t_cs])
            for cc in range(96):
                slab, tk, k = ring.next()
                S.dma("sp" if cc % 2 == 0 else "act", slab[:], wada_d[cc], "wa%d" % k, writes=[tk])
                for kk in range(16):
                    S.op("pe", lambda e, slab=slab, kk=kk, cc=cc: e.matmul(
                        pmod[:, cc:cc + 1], slab[:, kk, :], cs[:, kk:kk + 1], start=(kk == 0), stop=(kk == 15)),
                        reads=[tk, t_cs], writes=[t_pmod], signal=(kk == 15))
            S.op("dve", lambda e: e.tensor_tensor(modv[:], pmod[:], bada[:], ALU.add),
                 reads=[t_pmod, t_bada], writes=[t_modv])
            S.op("dve", lambda e: e.scalar_tensor_tensor(a1[:], modv[:, 16:32], 1.0, gnv[:, 0, :], ALU.add, ALU.mult),
                 reads=[t_modv, t_gnv], writes=[t_a1])
            S.op("dve", lambda e: e.scalar_tensor_tensor(a2[:], modv[:, 64:80], 1.0, gnv[:, 1, :], ALU.add, ALU.mult),
                 reads=[t_modv, t_gnv], writes=[t_a2])
            if debug:
                S.dma("sp", dbg["modv"], modv[:], "dbg", reads=[t_modv], batch=True, is_out=True)
            S.barrier()
            S.emit()
        if stop <= 0:
            return _finish(nc, S, st, dram_in)

        def transpose_pass(ph, tag, src, ntile, with_norm, dst_fn, t_dst):
            xts = [sb(ph, tag + "xt%d" % i, [128, D]) for i in range(4)]
            xring = Ring(xts, tag + "xt")
            sq = sb(ph, tag + "sq", [128, D]); t_sq = Tok("sq")
            ss = [sb(ph, tag + "ss%d" % i, [128, 1]) for i in range(4)]
            dg = [sb(ph, tag + "dg%d" % i, [128, 128]) for i in range(4)]
            t_ss = [Tok() for _ in range(4)]; t_dg = [Tok() for _ in range(4)]
            ptr = [ps(ph, tag + "ptr%d" % i, [128, 256]) for i in range(4)]
            pring = Ring(ptr, tag + "ptr")
            ev_i = 0
            g = 0
            ti = 0
            while ti < ntile:
                n_in = min(2, ntile - ti)
                tiles = []
                for j in range(n_in):
                    xt, tx, k = xring.next()
                    S.dma("sp" if (ti + j) % 2 == 0 else "act", xt[:], src[(ti + j) * 128:(ti + j + 1) * 128, :],
                          "xt%d" % k, writes=[tx])
                    if with_norm:
                        S.op("act", lambda e, xt=xt, k=k: e.activation(sq[:], xt[:], AF.Square, accum_out=ss[k][:]),
                             reads=[tx], writes=[t_sq, t_ss[k]])
                        S.op("dve", lambda e, k=k: e.tensor_scalar(ss[k][:], ss[k][:], 1.0 / D, RMS_EPS, ALU.mult, ALU.add),
                             reads=[t_ss[k]], writes=[t_ss[k]])
                        S.op("act", lambda e, k=k: e.activation(ss[k][:], ss[k][:], AF.Sqrt),
                             reads=[t_ss[k]], writes=[t_ss[k]])
                        S.op("dve", lambda e, k=k: e.reciprocal(ss[k][:], ss[k][:]),
                             reads=[t_ss[k]], writes=[t_ss[k]])
                        S.op("dve", lambda e, k=k: e.tensor_scalar(dg[k][:], ident[:], ss[k][:], None, ALU.mult),
                             reads=[t_ss[k], t_ident], writes=[t_dg[k]])
                    tiles.append((xt, tx, k))
                for fc in range(16):
                    pt, tp, _ = pring.next()
                    for j, (xt, tx, k) in enumerate(tiles):
                        rhs = dg[k] if with_norm else ident
                        rt = t_dg[k] if with_norm else t_ident
                        S.op("pe", lambda e, pt=pt, xt=xt, rhs=rhs, j=j, fc=fc: e.matmul(
                            pt[:, j * 128:(j + 1) * 128], xt[:, fc * 128:(fc + 1) * 128], rhs[:], start=True, stop=True),
                            reads=[tx, rt], writes=[tp], signal=(j == n_in - 1))
                    eng = "act" if ev_i % 2 == 0 else "dve"
                    ev_i += 1
                    dst = dst_fn(fc, ti * 128, n_in * 128)
                    src_ps = pt[:, 0:n_in * 128]
                    if with_norm:
                        if eng == "act":
                            S.op("act", lambda e, src_ps=src_ps, dst=dst, fc=fc: e.activation(
                                dst, src_ps, AF.Identity, bias=modv[:, fc:fc + 1], scale=a1[:, fc:fc + 1]),
                                reads=[tp, t_modv, t_a1], writes=[t_dst])
                        else:
                            S.op("dve", lambda e, src_ps=src_ps, dst=dst, fc=fc: e.tensor_scalar(
                                dst, src_ps, a1[:, fc:fc + 1], modv[:, fc:fc + 1], ALU.mult, ALU.add),
                                reads=[tp, t_modv, t_a1], writes=[t_dst])
                    else:
                        if eng == "act":
                            S.op("act", lambda e, src_ps=src_ps, dst=dst: e.copy(dst, src_ps), reads=[tp], writes=[t_dst])
                        else:
                            S.op("dve", lambda e, src_ps=src_ps, dst=dst: e.tensor_copy(dst, src_ps), reads=[tp], writes=[t_dst])
                ti += n_in

        def gemm_to_dram(ph, tag, hnT, t_hnT, ntok, chunks, dram, col0=0, mask=None):
            slabs = [sb(ph, tag + "w%d" % i, [128, 16, 128], BF16) for i in range(3)]
            wring = Ring(slabs, tag + "w")
            stg = [sb(ph, tag + "stg%d" % i, [128, ntok]) for i in range(2)]
            sring = Ring(stg, tag + "stg")
            pg = [ps(ph, tag + "pg%d" % i, [128, 512]) for i in range(4)]
            pring = Ring(pg, tag + "pg")
            groups = []
            o = 0
            while o < ntok:
                n = min(512, ntok - o)
                groups.append((o, n))
                o += n
            ev_i = 0
            for gc, rc in chunks:
                slab, tw, k = wring.next()
                S.dma("pool", slab[:], win_d[gc], "w%d" % k, writes=[tw])
                sg, tsg, k2 = sring.next()
                for (o, n) in groups:
                    pt, tp, _ = pring.next()
                    for kk in range(16):
                        S.op("pe", lambda e, pt=pt, slab=slab, kk=kk, o=o, n=n: e.matmul(
                            pt[:, 0:n], slab[:, kk, :], hnT[:, kk, o:o + n], start=(kk == 0), stop=(kk == 15)),
                            reads=[tw, t_hnT], writes=[tp], signal=(kk == 15))
                    if mask is not None:
                        S.op("dve", lambda e, pt=pt, sg=sg, o=o, n=n: e.tensor_tensor(sg[:, o:o + n], pt[:, 0:n], hmask[:, o:o + n], ALU.mult),
                             reads=[tp, t_hmask], writes=[tsg])
                    else:
                        eng = "act" if ev_i % 2 == 0 else "dve"
                        ev_i += 1
                        if eng == "act":
                            S.op("act", lambda e, pt=pt, sg=sg, o=o, n=n: e.copy(sg[:, o:o + n], pt[:, 0:n]), reads=[tp], writes=[tsg])
                        else:
                            S.op("dve", lambda e, pt=pt, sg=sg, o=o, n=n: e.tensor_copy(sg[:, o:o + n], pt[:, 0:n]), reads=[tp], writes=[tsg])
                S.dma("sp", dram[rc * 128:(rc + 1) * 128, col0:col0 + ntok], sg[:], "st%d" % k2, reads=[tsg], is_out=(debug == 2))

        HALF = SEQ // 2
        for half in range(2):
            with ExitStack() as ph:
                hnT = sb(ph, "hnT%d" % half, [128, 16, HALF], BF16); t_hnT = Tok("hnT")
                with ExitStack() as ph2:
                    transpose_pass(ph2, "a%d" % half, xf[half * HALF:(half + 1) * HALF, :], HALF // 128, True,
                                   lambda fc, o, n, hnT=hnT: hnT[:, fc, o:o + n], t_hnT)
                    S.barrier(); S.emit()
                with ExitStack() as ph2:
                    gemm_to_dram(ph2, "a%d" % half, hnT, t_hnT, HALF, [(16 + rc, rc) for rc in range(NRC)], PT, col0=half * HALF)
                    S.barrier(); S.emit()
        if stop <= 1:
            return _finish(nc, S, st, dram_in)
        with ExitStack() as ph:
            hnT = sb(ph, "hnTh", [128, 16, TH], BF16); t_hnT = Tok("hnTh")
            hmask = sb(ph, "hmask", [128, TH]); t_hmask = Tok("hmask")
            S.dma("sp", hmask[:], hmask_d, "hm", writes=[t_hmask])
            with ExitStack() as ph2:
                transpose_pass(ph2, "b", xh, TH // 128, True, lambda fc, o, n: hnT[:, fc, o:o + n], t_hnT)
                S.barrier(); S.emit()
            with ExitStack() as ph2:
                gemm_to_dram(ph2, "b", hnT, t_hnT, TH, [(gc, gc) for gc in range(NCC)], PH, mask=True)
                S.barrier(); S.emit()
        if stop <= 2:
            return _finish(nc, S, st, dram_in)

        class Buf:
            def __init__(self, t, k=None):
                self.t = t
                self.k = k if k is not None else Tok()

        def B(stack, name, shape, dt=F32):
            return Buf(sb(stack, name, shape, dt))

        def PB(stack, name, shape):
            return Buf(ps(stack, name, shape))

        def _ks(bs):
            return [b.k for b in bs]

        def V(fn, r=(), w=()):
            return S.op("dve", fn, reads=_ks(r), writes=_ks(w))

        def A(fn, r=(), w=()):
            return S.op("act", fn, reads=_ks(r), writes=_ks(w))

        def P(fn, r=(), w=()):
            return S.op("pool", fn, reads=_ks(r), writes=_ks(w))

        def M(fn, r=(), w=(), sig=True):
            return S.op("pe", fn, reads=_ks(r), writes=_ks(w), signal=sig)

        def LD(q, dst, src, key, w, batch=False):
            return S.dma(q, dst, src, key, writes=_ks(w), batch=batch)

        dumps = {}

        def DUMP(name, ap, shape, dt=F32):
            if debug != 3:
                return
            dten = nc.dram_tensor("dmp_" + name, list(shape), dt, kind="ExternalOutput").ap()
            dumps[name] = dten
            return dten

        staged = []

        def dump_buf(name, b_, shape, dt=F32, view=None):
            if debug != 3:
                return
            dten = DUMP(name, None, shape, dt)
            slot = Buf(stage_slots[name][0], stage_slots[name][1])
            P(lambda e: e.tensor_copy(slot.t[:], b_.t[:]), [b_], [slot])
            staged.append((dten, slot))

        def flush_dumps():
            for dten, slot in staged:
                S.dma("sp", dten, slot.t[:], "dmp", reads=[slot.k], batch=True, is_out=True)
            staged[:] = []

        Bident = Buf(ident, t_ident); Bidentb = Buf(identb, t_identb); Bmodv = Buf(modv, t_modv)
        Bgnv = Buf(gnv, t_gnv); Ba2 = Buf(a2, t_a2); Bqflag = Buf(qflag, t_qflag)

        cmask = B(st, "cmask", [128, 8, 128]); rst = B(st, "rst", [128, 512]); blk64 = B(st, "blk64", [128, 128])
        ones = B(st, "ones", [128, 128]); ident2 = B(st, "ident2", [128, 64])
        mu = B(st, "mu", [128, 2, NCC]); c0 = B(st, "c0", [128, NCC]); rv = B(st, "rv", [128, 9, 8]); omka = B(st, "omka", [128, 8])
        w2 = B(st, "w2", [128, DR], BF16); a2w = B(st, "a2w", [128, DR], BF16)
        g2a = B(st, "g2a", [128, DR], BF16); g2b = B(st, "g2b", [32, DR], BF16)
        convw = B(st, "convw", [128, 8, KW]); convv = B(st, "convv", [128, 3, 8])
        st_y = ExitStack()
        yTr = B(st_y, "yTr", [128, 8, TOK], BF16)
        for (b_, d_) in ((cmask, din("cmask", [128, 8, 128])), (rst, din("rst", [128, 512])), (blk64, din("blk64", [128, 128])),
                         (ones, din("ones", [128, 128])), (ident2, din("ident2", [128, 64])), (mu, din("mu", [128, 2, NCC])),
                         (rv, din("rv", [128, 9, 8])), (convw, din("convw", [128, 8, KW])), (convv, din("convv", [128, 3, 8]))):
            LD("sp", b_.t[:], d_, "const", [b_], batch=True)
        for (b_, d_) in ((w2, din("w2", [128, DR])), (a2w, din("a2w", [128, DR])), (g2a, din("g2a", [128, DR])), (g2b, din("g2b", [32, DR]))):
            LD("pool", b_.t[:], d_, "constc", [b_], batch=True)
        V(lambda e: e.tensor_tensor(c0.t[:], mu.t[:, 0, :], mu.t[:, 1, :], ALU.add), [mu], [c0])
        V(lambda e: e.tensor_scalar(c0.t[:], c0.t[:], -1.0, 1.0, ALU.mult, ALU.add), [c0], [c0])
        V(lambda e: e.tensor_scalar(omka.t[:], rv.t[:, 5, :], -1.0, 1.0, ALU.mult, ALU.add), [rv], [omka])

        def load_shift(ph_, dst, dram, gc, rows, col0, n, lo_edge, hi_edge, raw, nrows=128, key="raw"):
            a = 0 if lo_edge else 1
            bnd = 0 if hi_edge else 1
            if lo_edge or hi_edge:
                P(lambda e: e.memset(raw.t[:nrows, 0:n + 2], 0.0), [], [raw])
            LD("sp", raw.t[:nrows, 1 - a:n + 1 + bnd], dram[rows:rows + nrows, col0 - a:col0 + n + bnd], key, [raw])
            V(lambda e: e.tensor_scalar(dst.t[:nrows, 0:n], raw.t[:nrows, 1:n + 1], c0.t[:nrows, gc:gc + 1], None, ALU.mult), [raw, c0], [dst])
            V(lambda e: e.scalar_tensor_tensor(dst.t[:nrows, 0:n], raw.t[:nrows, 0:n], mu.t[:nrows, 0, gc:gc + 1], dst.t[:nrows, 0:n], ALU.mult, ALU.add), [raw, mu, dst], [dst])
            V(lambda e: e.scalar_tensor_tensor(dst.t[:nrows, 0:n], raw.t[:nrows, 2:n + 2], mu.t[:nrows, 1, gc:gc + 1], dst.t[:nrows, 0:n], ALU.mult, ALU.add), [raw, mu, dst], [dst])

        def issue_load(raw, dram, rows, col0, n, lo_edge, hi_edge, key):
            a = 0 if lo_edge else 1
            bnd = 0 if hi_edge else 1
            if lo_edge or hi_edge:
                P(lambda e: e.memset(raw.t[:, 0:n + 2], 0.0), [], [raw])
            LD("sp", raw.t[:, 1 - a:n + 1 + bnd], dram[rows:rows + 128, col0 - a:col0 + n + bnd], key, [raw])

        def apply_shift(dst, raw, gc, n):
            V(lambda e: e.tensor_scalar(dst.t[:, 0:n], raw.t[:, 1:n + 1], c0.t[:, gc:gc + 1], None, ALU.mult), [raw, c0], [dst])
            V(lambda e: e.scalar_tensor_tensor(dst.t[:, 0:n], raw.t[:, 0:n], mu.t[:, 0, gc:gc + 1], dst.t[:, 0:n], ALU.mult, ALU.add), [raw, mu, dst], [dst])
            V(lambda e: e.scalar_tensor_tensor(dst.t[:, 0:n], raw.t[:, 2:n + 2], mu.t[:, 1, gc:gc + 1], dst.t[:, 0:n], ALU.mult, ALU.add), [raw, mu, dst], [dst])

        NB = SEQ // 512
        with ExitStack() as ph:
            TWD = nc.dram_tensor("TWD", [128, SEQ], BF16, kind="Internal").ap(); ADD = nc.dram_tensor("ADD", [128, SEQ], BF16, kind="Internal").ap()
            ado = B(ph, "ado", [128, TOK], BF16); sga = B(ph, "sga", [128, TOK], BF16); sgb = B(ph, "sgb", [32, TOK], BF16)
            YD = [nc.dram_tensor("YF", [128, SEQ], F32, kind="Internal").ap(), nc.dram_tensor("YB", [128, SEQ], F32, kind="Internal").ap()]
            with ExitStack() as p2:
                raw = B(p2, "raw0", [128, 1026]); tmp = B(p2, "tmp0", [128, 1024]); tb1 = B(p2, "tb1", [128, 1024], BF16); tb2 = B(p2, "tb2", [128, 1024], BF16)
                for blk in range(SEQ // 1024):
                    lo, hi = blk == 0, blk == SEQ // 1024 - 1
                    load_shift(p2, tmp, PT, 40, 24 * 128, blk * 1024, 1024, lo, hi, raw)
                    A(lambda e: e.activation(tb1.t[:], tmp.t[:], AF.Tanh), [tmp], [tb1])
                    S.dma("sp", TWD[:, blk * 1024:(blk + 1) * 1024], tb1.t[:], "twd", reads=[tb1.k])
                    load_shift(p2, tmp, PT, 41, 25 * 128, blk * 1024, 1024, lo, hi, raw)
                    A(lambda e: e.copy(tb2.t[:], tmp.t[:]), [tmp], [tb2])
                    S.dma("sp", ADD[:, blk * 1024:(blk + 1) * 1024], tb2.t[:], "add", reads=[tb2.k])
                load_shift(p2, tmp, PH, 41, 41 * 128, HALO, TOK, False, False, raw)
                A(lambda e: e.copy(ado.t[:], tmp.t[:]), [tmp], [ado])
                load_shift(p2, tmp, PH, 42, 42 * 128, HALO, TOK, False, False, raw)
                A(lambda e: e.activation(sga.t[:], tmp.t[:], AF.Sigmoid), [tmp], [sga])
                load_shift(p2, tmp, PH, 43, 43 * 128, HALO, TOK, False, False, raw, nrows=32)
                A(lambda e: e.activation(sgb.t[:], tmp.t[0:32, :], AF.Sigmoid), [tmp], [sgb])
                S.barrier(); S.emit()

            for pc in ([dbg_pc] if debug == 3 else range(8)):
                cs_ = slice(pc * 128, (pc + 1) * 128)
                with ExitStack() as p2:
                    WS_F32 = ("r", "k", "lw", "lr", "kk", "t1", "t2", "pref", "cum", "E", "bb", "kd")
                    WS_BF = ("At", "Rt", "Bt", "Kt", "Bh", "Kh", "Vb")
                    WS_TOK = ("Atok", "Bhtok", "Khtok", "Vtok", "Gtok", "AkVs", "U0tok")

                    def mk_ws(tag):
                        W = {"raws": [[B(p2, "raw%s%d%s" % (x_, s_, tag), [128, 514]) for x_ in "rkv"] for s_ in range(2)],
                             "tws": [B(p2, "twb%d%s" % (s_, tag), [128, 512], BF16) for s_ in range(2)],
                             "ads": [B(p2, "adb%d%s" % (s_, tag), [128, 512], BF16) for s_ in range(2)]}
                        for n_ in WS_F32:
                            W[n_] = B(p2, n_ + tag, [128, 512])
                        for n_ in WS_BF:
                            W[n_] = B(p2, n_ + tag, [128, 512], BF16)
                        for n_ in WS_TOK:
                            W[n_] = B(p2, n_ + tag, [128, 4, 128], BF16)
                        W["hb"] = [{n_: B(p2, "%s%d%s" % (n_, h, tag), [128, 4, 128], BF16) for n_ in
                                    ("AkT", "ArbT", "ArkT", "TT", "N", "NT", "Na", "NTa", "Nb", "NTb", "Tt")} for h in range(2)]
                        W["PhiT"] = B(p2, "PhiT" + tag, [128, 4, 64]); W["Zloc"] = B(p2, "Zloc" + tag, [128, 4, 64])
                        W["Z"] = B(p2, "Z" + tag, [128, 64]); W["WC"] = B(p2, "WC" + tag, [128, 4])
                        W["py"] = PB(p2, "py" + tag, [128, 512]); W["pz"] = PB(p2, "pz" + tag, [128, 64])
                        return W

                    WSP = [mk_ws("f"), mk_ws("b")]
                    pr = [PB(p2, "pr%d" % i, [128, 512]) for i in range(4)]
                    pring = Ring(pr, "pr")

                    def pnext():
                        k = pring.i % len(pr)
                        pring.i += 1
                        return pr[k]

                    v3 = lambda b_: b_.t[:].rearrange("p (c t) -> p c t", t=128)

                    def issue_block_loads(d, tb, W, s_):
                        sfx = "fb"[d]
                        lo, hi = tb == 0, tb == NB - 1
                        c0_ = tb * 512
                        ts_ = slice(c0_, c0_ + 512)
                        LD("act", W["tws"][s_].t[:], TWD[:, ts_], "twl%d%s" % (s_, sfx), [W["tws"][s_]])
                        LD("act", W["ads"][s_].t[:], ADD[:, ts_], "adl%d%s" % (s_, sfx), [W["ads"][s_]])
                        issue_load(W["raws"][s_][0], PT, pc * 128, c0_, 512, lo, hi, "rawr%d%s" % (s_, sfx))
                        issue_load(W["raws"][s_][1], PT, (8 + pc) * 128, c0_, 512, lo, hi, "rawk%d%s" % (s_, sfx))
                        issue_load(W["raws"][s_][2], PT, (16 + pc) * 128, c0_, 512, lo, hi, "rawv%d%s" % (s_, sfx))

                    def block_gen(d, tb, W, idx, nxt_tb):
                        s_ = idx % 2
                        if idx == 0:
                            issue_block_loads(d, tb, W, s_)
                        if nxt_tb is not None:
                            issue_block_loads(d, nxt_tb, W, 1 - s_)
                        tw, adf = W["tws"][s_], W["ads"][s_]
                        r_, k_, lw, lr, kk = W["r"], W["k"], W["lw"], W["lr"], W["kk"]
                        t1, t2, pref, cum, E, bb, kd = W["t1"], W["t2"], W["pref"], W["cum"], W["E"], W["bb"], W["kd"]
                        v_ = t2; QT = E; Y0s = t1
                        At, Rt, Bt, Kt, Bh, Kh, Vb = W["At"], W["Rt"], W["Bt"], W["Kt"], W["Bh"], W["Kh"], W["Vb"]
                        Atok, Bhtok, Khtok, Vtok, Gtok, AkVs, U0tok = [W[n_] for n_ in WS_TOK]
                        hb = W["hb"]; PhiT, Zloc, Z, WC, py, pz = W["PhiT"], W["Zloc"], W["Z"], W["WC"], W["py"], W["pz"]
                        hs_w = slice(d * 64, d * 64 + 64)
                        mN, mI, mNT = (0, 1, 2) if d == 0 else (2, 3, 0)
                        sfx = "fb"[d]
                        lo, hi = tb == 0, tb == NB - 1
                        c0_ = tb * 512
                        ts_ = slice(c0_, c0_ + 512)
                        apply_shift(r_, W["raws"][s_][0], 16 + pc, 512)
                        apply_shift(k_, W["raws"][s_][1], 24 + pc, 512)
                        apply_shift(v_, W["raws"][s_][2], 32 + pc, 512)
                        P(lambda e: e.tensor_copy(Vb.t[:], v_.t[:]), [v_], [Vb])
                        yield
                        pt = pnext()
                        M(lambda e, pt=pt, hs_w=hs_w: e.matmul(pt.t[:], w2.t[hs_w, cs_], tw.t[hs_w, :], start=True, stop=True), [w2, tw], [pt])
                        A(lambda e, pt=pt, d=d: e.activation(lw.t[:], pt.t[:], AF.Sigmoid, bias=rv.t[:, d, pc:pc + 1]), [pt, rv], [lw])
                        V(lambda e: e.tensor_scalar(lw.t[:], lw.t[:], -0.6065306597126334, None, ALU.mult), [lw], [lw])
                        pt = pnext()
                        M(lambda e, pt=pt, hs_w=hs_w: e.matmul(pt.t[:], a2w.t[hs_w, cs_], adf.t[hs_w, :], start=True, stop=True), [a2w, adf], [pt])
                        A(lambda e, pt=pt, d=d: e.activation(lr.t[:], pt.t[:], AF.Sigmoid, bias=rv.t[:, 2 + d, pc:pc + 1]), [pt, rv], [lr])
                        yield
                        V(lambda e: e.tensor_scalar(kk.t[:], k_.t[:], rv.t[:, 4, pc:pc + 1], None, ALU.mult), [k_, rv], [kk])
                        P(lambda e: e.tensor_tensor(t1.t[:], kk.t[:], kk.t[:], ALU.mult), [kk], [t1])
                        pt = pnext()
                        M(lambda e, pt=pt: e.matmul(pt.t[:], blk64.t[:], t1.t[:], start=True, stop=True), [blk64, t1], [pt])
                        A(lambda e, pt=pt: e.activation(t2.t[:], pt.t[:], AF.Sqrt), [pt], [t2])
                        V(lambda e: e.tensor_scalar(t2.t[:], t2.t[:], 1e-12, None, ALU.max), [t2], [t2])
                        V(lambda e: e.reciprocal(t2.t[:], t2.t[:]), [t2], [t2])
                        P(lambda e: e.tensor_tensor(kk.t[:], kk.t[:], t2.t[:], ALU.mult), [kk, t2], [kk])
                        yield
                        V(lambda e: e.tensor_scalar(t1.t[:], lr.t[:], rv.t[:, 5, pc:pc + 1], omka.t[:, pc:pc + 1], ALU.mult, ALU.add), [lr, rv, omka], [t1])
                        P(lambda e: e.tensor_tensor(kd.t[:], k_.t[:], t1.t[:], ALU.mult), [k_, t1], [kd])
                        P(lambda e: e.tensor_tensor(bb.t[:], kk.t[:], lr.t[:], ALU.mult), [kk, lr], [bb])
                        yield
                        V(lambda e: e.tensor_tensor_scan(pref.t[:], rst.t[:], lw.t[:], 0.0, ALU.mult, ALU.add), [rst, lw], [pref])
                        tot_bc = v3(pref)[:, :, 127:128].to_broadcast([128, 4, 128])
                        if d == 0:
                            P(lambda e: e.tensor_copy(cum.t[:], pref.t[:]), [pref], [cum])
                        else:
                            V(lambda e: e.tensor_tensor(cum.t[:], lw.t[:], pref.t[:], ALU.subtract), [lw, pref], [cum])
                            V(lambda e: e.tensor_tensor(v3(cum), v3(cum), tot_bc, ALU.add), [cum, pref], [cum])
                        A(lambda e: e.activation(WC.t[:], v3(pref)[:, :, 127], AF.Exp), [pref], [WC])
                        yield
                        A(lambda e: e.activation(E.t[:], cum.t[:], AF.Exp, scale=-1.0), [cum], [E])
                        V(lambda e: e.tensor_tensor(Bt.t[:], bb.t[:], E.t[:], ALU.mult), [bb, E], [Bt])
                        P(lambda e: e.tensor_tensor(Kt.t[:], kd.t[:], E.t[:], ALU.mult), [kd, E], [Kt])
                        yield
                        V(lambda e: e.tensor_tensor(v3(t1), tot_bc, v3(cum), ALU.subtract), [pref, cum], [t1])
                        A(lambda e: e.activation(E.t[:], t1.t[:], AF.Exp), [t1], [E])
                        V(lambda e: e.tensor_tensor(Bh.t[:], bb.t[:], E.t[:], ALU.mult), [bb, E], [Bh])
                        P(lambda e: e.tensor_tensor(Kh.t[:], kd.t[:], E.t[:], ALU.mult), [kd, E], [Kh])
                        yield
                        A(lambda e: e.activation(E.t[:], cum.t[:], AF.Exp), [cum], [E])
                        V(lambda e: e.tensor_tensor(Rt.t[:], r_.t[:], E.t[:], ALU.mult), [r_, E], [Rt])
                        V(lambda e: e.tensor_tensor(t1.t[:], cum.t[:], lw.t[:], ALU.subtract), [cum, lw], [t1])
                        A(lambda e: e.activation(E.t[:], t1.t[:], AF.Exp), [t1], [E])
                        V(lambda e: e.scalar_tensor_tensor(At.t[:], kk.t[:], -1.0, E.t[:], ALU.mult, ALU.mult), [kk, E], [At])
                        if debug == 3 and d == dbg_d and tb == dbg_tb:
                            for nm_, b__, dt_ in (("r", r_, F32), ("k", k_, F32), ("lw", lw, F32), ("lr", lr, F32), ("kk", kk, F32), ("kd", kd, F32), ("bb", bb, F32),
                                                  ("cum", cum, F32), ("pref", pref, F32), ("At", At, BF16), ("Rt", Rt, BF16), ("Bt", Bt, BF16), ("Kt", Kt, BF16),
                                                  ("Bh", Bh, BF16), ("Kh", Kh, BF16), ("Vb", Vb, BF16)):
                                dump_buf(nm_, b__, [128, 512], dt_)
                            dump_buf("WC", WC, [128, 4])
                        yield
                        for (src_, dst_) in ((At, Atok), (Bh, Bhtok), (Kh, Khtok), (Vb, Vtok)):
                            pt = pnext()
                            for c in range(4):
                                M(lambda e, pt=pt, src_=src_, c=c: e.matmul(pt.t[:, c * 128:(c + 1) * 128], src_.t[:, c * 128:(c + 1) * 128], identb[:], start=True, stop=True),
                                  [src_, Bidentb], [pt], sig=(c == 3))
                            A(lambda e, pt=pt, dst_=dst_: e.copy(dst_.t[:].rearrange("p c t -> p (c t)"), pt.t[:]), [pt], [dst_])
                        yield
                        for h in range(2):
                            hs = slice(h * 64, h * 64 + 64)
                            H = hb[h]
                            for (nm, lh, rh, mk) in (("N", Bt, At, mN), ("NT", At, Bt, mNT), ("AkT", Kt, At, mN), ("ArbT", Bt, Rt, mI), ("ArkT", Kt, Rt, mI)):
                                pt = pnext()
                                for c in range(4):
                                    M(lambda e, pt=pt, lh=lh, rh=rh, c=c, hs=hs: e.matmul(pt.t[:, c * 128:(c + 1) * 128], lh.t[hs, c * 128:(c + 1) * 128], rh.t[hs, c * 128:(c + 1) * 128], start=True, stop=True),
                                      [lh, rh], [pt], sig=(c == 3))
                                V(lambda e, pt=pt, nm=nm, mk=mk, H=H: e.tensor_tensor(H[nm].t[:], pt.t[:].rearrange("p (c t) -> p c t", t=128), cmask.t[:, mk:mk + 1, :].to_broadcast([128, 4, 128]), ALU.mult),
                                  [pt, cmask], [H[nm]])
                        yield
                        c4 = lambda b_: b_.t[:].rearrange("p c t -> p (c t)")
                        mk4 = lambda m_: cmask.t[:, m_:m_ + 1, :].to_broadcast([128, 4, 128])
                        idb = identb[:].rearrange("p (o t) -> p o t", o=1).to_broadcast([128, 4, 128])

                        def mm4(lhs, rhs):
                            pt_ = pnext()
                            for c in range(4):
                                M(lambda e, pt_=pt_, lhs=lhs, rhs=rhs, c=c: e.matmul(pt_.t[:, c * 128:(c + 1) * 128], lhs.t[:, c, :], rhs.t[:, c, :], start=True, stop=True),
                                  [lhs, rhs], [pt_], sig=(c == 3))
                            return pt_

                        def cp4(dst, pt_, eng):
                            if eng == "act":
                                A(lambda e, dst=dst, pt_=pt_: e.copy(c4(dst), pt_.t[:]), [pt_], [dst])
                            else:
                                V(lambda e, dst=dst, pt_=pt_: e.tensor_copy(c4(dst), pt_.t[:]), [pt_], [dst])

                        def acc4(dst, pt_):
                            V(lambda e, dst=dst, pt_=pt_: e.tensor_tensor(c4(dst), pt_.t[:], c4(dst), ALU.add), [pt_, dst], [dst])

                        def inv_gen(H):
                            TTb, Ttb = H["TT"], H["Tt"]
                            Nk, NTk = H["Na"], H["NTa"]
                            P(lambda e, Nk=Nk: e.tensor_tensor(Nk.t[:], H["N"].t[:], mk4(4), ALU.mult), [H["N"], cmask], [Nk])
                            P(lambda e, NTk=NTk: e.tensor_tensor(NTk.t[:], H["NT"].t[:], mk4(4), ALU.mult), [H["NT"], cmask], [NTk])
                            V(lambda e, Nk=Nk: e.tensor_tensor(TTb.t[:], Nk.t[:], idb, ALU.add), [Nk, Bidentb], [TTb])
                            V(lambda e, NTk=NTk: e.tensor_tensor(Ttb.t[:], NTk.t[:], idb, ALU.add), [NTk, Bidentb], [Ttb])
                            yield
                            for lev in range(3):
                                N2, NT2 = (H["Nb"], H["NTb"]) if lev % 2 == 0 else (H["Na"], H["NTa"])
                                p1 = mm4(Nk, NTk)
                                p2 = mm4(NTk, Nk)
                                yield
                                cp4(NT2, p1, "act"); cp4(N2, p2, "act")
                                yield
                                p3 = mm4(NT2, TTb); p4 = mm4(N2, Ttb)
                                yield
                                acc4(TTb, p3); acc4(Ttb, p4)
                                yield
                                Nk, NTk = N2, NT2
                            for mi, mk_ in enumerate((5, 6, 7)):
                                O_, Ot_, X_, Xt_ = H["Na"], H["NTa"], H["Nb"], H["NTb"]
                                last = (mi == 2)
                                P(lambda e, O_=O_, mk_=mk_: e.tensor_tensor(O_.t[:], H["N"].t[:], mk4(mk_), ALU.mult), [H["N"], cmask], [O_])
                                P(lambda e, Ot_=Ot_, mk_=mk_: e.tensor_tensor(Ot_.t[:], H["NT"].t[:], mk4(mk_), ALU.mult), [H["NT"], cmask], [Ot_])
                                yield
                                px = mm4(Ot_, TTb)
                                pxt = mm4(O_, Ttb) if not last else None
                                yield
                                cp4(X_, px, "act")
                                if not last:
                                    cp4(Xt_, pxt, "act")
                                yield
                                pa = mm4(Ttb, X_)
                                pb = mm4(TTb, Xt_) if not last else None
                                yield
                                acc4(TTb, pa)
                                if not last:
                                    acc4(Ttb, pb)
                                yield

                        gens = [inv_gen(hb[0]), inv_gen(hb[1])]
                        while gens:
                            for g_ in list(gens):
                                try:
                                    next(g_)
                                except StopIteration:
                                    gens.remove(g_)
                        if debug == 3 and d == dbg_d and tb == dbg_tb:
                            dump_buf("Atok", Atok, [128, 4, 128], BF16); dump_buf("Vtok", Vtok, [128, 4, 128], BF16)
                            for h in range(2):
                                for nm_ in ("AkT", "ArbT", "ArkT", "TT"):
                                    dump_buf("%s%d" % (nm_, h), hb[h][nm_], [128, 4, 128], BF16)
                            dump_buf("N1", hb[1]["N"], [128, 4, 128], BF16); dump_buf("NT1", hb[1]["NT"], [128, 4, 128], BF16)
                        yield
                        pt = pnext()
                        for h in range(2):
                            for c in range(4):
                                M(lambda e, pt=pt, h=h, c=c: e.matmul(pt.t[:, c * 128 + h * 64:c * 128 + h * 64 + 64], hb[h]["TT"].t[:, c, :], Atok.t[:, c, h * 64:h * 64 + 64], start=True, stop=True),
                                  [hb[h]["TT"], Atok], [pt], sig=(h == 1 and c == 3))
                        A(lambda e, pt=pt: e.copy(Gtok.t[:].rearrange("p c t -> p (c t)"), pt.t[:]), [pt], [Gtok])
                        yield
                        pt = pnext()
                        for h in range(2):
                            for c in range(4):
                                M(lambda e, pt=pt, h=h, c=c: e.matmul(pt.t[:, c * 128 + h * 64:c * 128 + h * 64 + 64], hb[h]["AkT"].t[:, c, :], Vtok.t[:, c, h * 64:h * 64 + 64], start=True, stop=True),
                                  [hb[h]["AkT"], Vtok], [pt], sig=(h == 1 and c == 3))
                        V(lambda e, pt=pt: e.tensor_copy(AkVs.t[:].rearrange("p c t -> p (c t)"), pt.t[:]), [pt], [AkVs])
                        yield
                        pt = pnext()
                        for h in range(2):
                            for c in range(4):
                                M(lambda e, pt=pt, h=h, c=c: e.matmul(pt.t[:, c * 128 + h * 64:c * 128 + h * 64 + 64], hb[h]["TT"].t[:, c, :], AkVs.t[:, c, h * 64:h * 64 + 64], start=True, stop=True),
                                  [hb[h]["TT"], AkVs], [pt], sig=(h == 1 and c == 3))
                        A(lambda e, pt=pt: e.copy(U0tok.t[:].rearrange("p c t -> p (c t)"), pt.t[:]), [pt], [U0tok])
                        yield
                        ptY = pnext()
                        for h in range(2):
                            hs = slice(h * 64, h * 64 + 64)
                            for c in range(4):
                                M(lambda e, h=h, c=c, hs=hs, ptY=ptY: e.matmul(ptY.t[hs, c * 128:(c + 1) * 128], U0tok.t[:, c, hs], hb[h]["ArbT"].t[:, c, :], start=True, stop=False), [U0tok, hb[h]["ArbT"]], [ptY], sig=False)
                                M(lambda e, h=h, c=c, hs=hs, ptY=ptY: e.matmul(ptY.t[hs, c * 128:(c + 1) * 128], Vtok.t[:, c, hs], hb[h]["ArkT"].t[:, c, :], start=False, stop=True), [Vtok, hb[h]["ArkT"]], [ptY], sig=(h == 1 and c == 3))
                        A(lambda e, ptY=ptY: e.copy(Y0s.t[:], ptY.t[:]), [ptY], [Y0s])
                        ptZ = pnext()
                        for h in range(2):
                            hs = slice(h * 64, h * 64 + 64)
                            for c in range(4):
                                M(lambda e, c=c, hs=hs, ptZ=ptZ: e.matmul(ptZ.t[hs, c * 64:(c + 1) * 64], Bhtok.t[:, c, hs], U0tok.t[:, c, hs], start=True, stop=False), [Bhtok, U0tok], [ptZ], sig=False)
                                M(lambda e, c=c, hs=hs, ptZ=ptZ: e.matmul(ptZ.t[hs, c * 64:(c + 1) * 64], Khtok.t[:, c, hs], Vtok.t[:, c, hs], start=False, stop=True), [Khtok, Vtok], [ptZ], sig=(h == 1 and c == 3))
                        V(lambda e, ptZ=ptZ: e.tensor_copy(Zloc.t[:].rearrange("p c j -> p (c j)"), ptZ.t[:, 0:256]), [ptZ], [Zloc])
                        ptP = pnext()
                        for h in range(2):
                            hs = slice(h * 64, h * 64 + 64)
                            for c in range(4):
                                M(lambda e, c=c, hs=hs, ptP=ptP: e.matmul(ptP.t[hs, c * 64:(c + 1) * 64], Gtok.t[:, c, hs], Bhtok.t[:, c, hs], start=True, stop=True), [Gtok, Bhtok], [ptP], sig=(h == 1 and c == 3))
                        for c in range(4):
                            V(lambda e, c=c, ptP=ptP: e.scalar_tensor_tensor(PhiT.t[:, c, :], ident2.t[:], WC.t[:, c:c + 1], ptP.t[:, c * 64:(c + 1) * 64], ALU.mult, ALU.add), [ident2, WC, ptP], [PhiT])
                        ptQ = pnext()
                        for h in range(2):
                            hs = slice(h * 64, h * 64 + 64)
                            for c in range(4):
                                M(lambda e, h=h, c=c, hs=hs, ptQ=ptQ: e.matmul(ptQ.t[hs, c * 128:(c + 1) * 128], Gtok.t[:, c, hs], hb[h]["ArbT"].t[:, c, :], start=True, stop=True), [Gtok, hb[h]["ArbT"]], [ptQ], sig=(h == 1 and c == 3))
                        V(lambda e, ptQ=ptQ: e.tensor_tensor(QT.t[:], ptQ.t[:], Rt.t[:], ALU.add), [ptQ, Rt], [QT])
                        if debug == 3 and d == dbg_d and tb == dbg_tb:
                            dump_buf("Gtok", Gtok, [128, 4, 128], BF16); dump_buf("U0tok", U0tok, [128, 4, 128], BF16)
                            dump_buf("Y0s", Y0s, [128, 512]); dump_buf("Zloc", Zloc, [128, 4, 64]); dump_buf("PhiT", PhiT, [128, 4, 64]); dump_buf("QT", QT, [128, 512])
                            dump_buf("Zin", Z, [128, 64])
                        yield
                        corder = range(4) if d == 0 else range(3, -1, -1)
                        for ci, c in enumerate(corder):
                            for h in range(2):
                                hs = slice(h * 64, h * 64 + 64)
                                M(lambda e, c=c, hs=hs: e.matmul(py.t[hs, c * 128:(c + 1) * 128], Z.t[hs, :], QT.t[hs, c * 128:(c + 1) * 128], start=True, stop=True), [Z, QT], [py], sig=False)
                                M(lambda e, c=c, hs=hs: e.matmul(pz.t[hs, :], PhiT.t[hs, c, :], Z.t[hs, :], start=True, stop=True), [PhiT, Z], [pz], sig=(h == 1))
                            V(lambda e, c=c: e.tensor_tensor(Z.t[:], pz.t[:], Zloc.t[:, c, :], ALU.add), [pz, Zloc], [Z])
                        V(lambda e: e.tensor_tensor(Y0s.t[:], py.t[:], Y0s.t[:], ALU.add), [py, Y0s], [Y0s])
                        S.dma("sp", YD[d][:, ts_], Y0s.t[:], "yst" + sfx, reads=[Y0s.k])
                        yield

                    for d in range(2):
                        P(lambda e, d=d: e.memset(WSP[d]["Z"].t[:], 0.0), [], [WSP[d]["Z"]])
                    ords = [list(range(NB)), list(range(NB - 1, -1, -1))]
                    seqs = [iter([block_gen(d_, tb, WSP[d_], i_, (ords[d_][i_ + 1] if i_ + 1 < NB else None)) for i_, tb in enumerate(ords[d_])]) for d_ in range(2)]
                    cur = [next(seqs[0]), next(seqs[1])]
                    while any(c_ is not None for c_ in cur):
                        for di in range(2):
                            if cur[di] is None:
                                continue
                            try:
                                next(cur[di])
                            except StopIteration:
                                cur[di] = next(seqs[di], None)
                    if debug == 3:
                        flush_dumps()

                    S.barrier(); S.emit()
                with ExitStack() as p2:
                    raw = B(p2, "rawo", [128, TOK + 2])
                    ro = B(p2, "ro", [128, TOK]); ko = B(p2, "ko", [128, TOK]); vo = B(p2, "vo", [128, TOK])
                    ys = B(p2, "ys", [128, TOK]); u1 = B(p2, "u1", [128, TOK]); u2 = B(p2, "u2", [128, TOK]); u3 = B(p2, "u3", [128, TOK])
                    po = [PB(p2, "po%d" % i, [128, 512]) for i in range(6)]
                    rawk_o = B(p2, "rawok", [128, TOK + 2]); rawv_o = B(p2, "rawov", [128, TOK + 2])
                    yl = [[B(p2, "yl%d_%d" % (q_, d_), [128, TOK]) for d_ in range(2)] for q_ in range(4)]
                    for q_ in range(4):
                        for d_ in range(2):
                            LD("sp" if d_ == 0 else "act", yl[q_][d_].t[:], YD[d_][:, q_ * TOK:(q_ + 1) * TOK], ("wa0", "wa1", "wa2", "wa3", "wa4", "wa5", "xt0", "xt1")[q_ * 2 + d_], [yl[q_][d_]])
                    load_shift(p2, ro, PH, 16 + pc, (16 + pc) * 128, HALO, TOK, False, False, raw, key="xt2")
                    load_shift(p2, ko, PH, 24 + pc, (24 + pc) * 128, HALO, TOK, False, False, rawk_o, key="xt3")
                    load_shift(p2, vo, PH, 32 + pc, (32 + pc) * 128, HALO, TOK, False, False, rawv_o, key="hm")
                    for q_ in range(4):
                        for d in range(2):
                            if q_ == 0 and d == 0:
                                V(lambda e, d=d, q_=q_: e.tensor_scalar(ys.t[:], yl[q_][d].t[:], qflag[:, q_:q_ + 1], None, ALU.mult), [yl[q_][d], Bqflag], [ys])
                            else:
                                V(lambda e, d=d, q_=q_: e.scalar_tensor_tensor(ys.t[:], yl[q_][d].t[:], qflag[:, q_:q_ + 1], ys.t[:], ALU.mult, ALU.add), [yl[q_][d], Bqflag, ys], [ys])
                    for half in range(2):
                        hsl = slice(half * 512, half * 512 + 512)
                        for d in range(2):
                            M(lambda e, d=d, hsl=hsl, half=half: e.matmul(po[d].t[:], a2w.t[d * 64:d * 64 + 64, cs_], ado.t[d * 64:d * 64 + 64, hsl], start=True, stop=True), [a2w, ado], [po[d]])
                        A(lambda e, hsl=hsl: e.activation(u1.t[:, hsl], po[0].t[:], AF.Sigmoid, bias=rv.t[:, 2, pc:pc + 1]), [po[0], rv], [u1])
                        A(lambda e, hsl=hsl: e.activation(u2.t[:, hsl], po[1].t[:], AF.Sigmoid, bias=rv.t[:, 3, pc:pc + 1]), [po[1], rv], [u2])
                    V(lambda e: e.tensor_tensor(u1.t[:], u1.t[:], u2.t[:], ALU.add), [u1, u2], [u1])
                    V(lambda e: e.tensor_scalar(u2.t[:], omka.t[:, pc:pc + 1].to_broadcast([128, TOK]), 2.0, None, ALU.mult), [omka], [u2])
                    V(lambda e: e.scalar_tensor_tensor(u1.t[:], u1.t[:], rv.t[:, 5, pc:pc + 1], u2.t[:], ALU.mult, ALU.add), [u1, rv, u2], [u1])
                    V(lambda e: e.tensor_tensor(u1.t[:], u1.t[:], ko.t[:], ALU.mult), [u1, ko], [u1])
                    V(lambda e: e.scalar_tensor_tensor(u1.t[:], u1.t[:], rv.t[:, 6, pc:pc + 1], ro.t[:], ALU.mult, ALU.mult), [u1, rv, ro], [u1])
                    A(lambda e: e.activation(u2.t[:], ys.t[:], AF.Square), [ys], [u2])
                    for half in range(2):
                        hsl = slice(half * 512, half * 512 + 512)
                        M(lambda e, hsl=hsl, half=half: e.matmul(po[0 + half].t[:], blk64.t[:], ys.t[:, hsl], start=True, stop=True), [blk64, ys], [po[0 + half]])
                        M(lambda e, hsl=hsl, half=half: e.matmul(po[2 + half].t[:], blk64.t[:], u2.t[:, hsl], start=True, stop=True), [blk64, u2], [po[2 + half]])
                        M(lambda e, hsl=hsl, half=half: e.matmul(po[4 + half].t[:], blk64.t[:], u1.t[:, hsl], start=True, stop=True), [blk64, u1], [po[4 + half]])
                    for half in range(2):
                        hsl = slice(half * 512, half * 512 + 512)
                        A(lambda e, hsl=hsl, half=half: e.mul(u3.t[:, hsl], po[0 + half].t[:], 1.0 / 64), [po[0 + half]], [u3])
                        V(lambda e, hsl=hsl, half=half: e.tensor_tensor(u1.t[:, hsl], po[4 + half].t[:], vo.t[:, hsl], ALU.mult), [po[4 + half], vo], [u1])
                        V(lambda e, hsl=hsl: e.tensor_tensor(ro.t[:, hsl], u3.t[:, hsl], u3.t[:, hsl], ALU.mult), [u3], [ro])
                        V(lambda e, hsl=hsl, half=half: e.scalar_tensor_tensor(u2.t[:, hsl], po[2 + half].t[:], 1.0 / 64, ro.t[:, hsl], ALU.mult, ALU.subtract), [po[2 + half], ro], [u2])
                    V(lambda e: e.tensor_scalar(u2.t[:], u2.t[:], GN_EPS, None, ALU.add), [u2], [u2])
                    A(lambda e: e.activation(u2.t[:], u2.t[:], AF.Sqrt), [u2], [u2])
                    V(lambda e: e.reciprocal(u2.t[:], u2.t[:]), [u2], [u2])
                    V(lambda e: e.tensor_tensor(ys.t[:], ys.t[:], u3.t[:], ALU.subtract), [ys, u3], [ys])
                    V(lambda e: e.tensor_tensor(ys.t[:], ys.t[:], u2.t[:], ALU.mult), [ys, u2], [ys])
                    V(lambda e: e.tensor_scalar(ys.t[:], ys.t[:], rv.t[:, 7, pc:pc + 1], rv.t[:, 8, pc:pc + 1], ALU.mult, ALU.add), [ys, rv], [ys])
                    V(lambda e: e.tensor_tensor(ys.t[:], ys.t[:], u1.t[:], ALU.add), [ys, u1], [ys])
                    for half in range(2):
                        hsl = slice(half * 512, half * 512 + 512)
                        M(lambda e, hsl=hsl, half=half: e.matmul(po[half].t[:], g2a.t[:, cs_], sga.t[:, hsl], start=True, stop=False), [g2a, sga], [po[half]], sig=False)
                        M(lambda e, hsl=hsl, half=half: e.matmul(po[half].t[:], g2b.t[0:32, cs_], sgb.t[0:32, hsl], start=False, stop=True), [g2b, sgb], [po[half]])
                        V(lambda e, hsl=hsl, half=half: e.tensor_tensor(yTr.t[:, pc, hsl], po[half].t[:], ys.t[:, hsl], ALU.mult), [po[half], ys], [yTr])
                    S.barrier(); S.emit()
        if stop <= 3:
            r_nc = _finish(nc, S, st, dram_in)
            st_y.close()
            return r_nc

        st_y2 = ExitStack()
        yTc = B(st_y2, "yTc", [128, 8, TOK], BF16)
        with ExitStack() as ph:
            W = TOK + KW - 1
            zc = B(ph, "zc", [128, 8, TOK]); val = B(ph, "val", [128, W]); gate = B(ph, "gate", [128, W]); z = B(ph, "z", [128, W])
            sq = B(ph, "csq", [128, TOK]); mean = B(ph, "cmean", [128, TOK]); rstd = B(ph, "crstd", [128, TOK]); tmpc = B(ph, "ctmp", [128, TOK])
            pS = [PB(ph, "pS%d" % i, [128, 512]) for i in range(2)]; pQ = [PB(ph, "pQ%d" % i, [128, 512]) for i in range(2)]
            c_lo = HALO - KW // 2
            for cc in range(8):
                LD("sp", val.t[:], PH[cc * 128:(cc + 1) * 128, c_lo:c_lo + W], "cv", [val])
                LD("act", gate.t[:], PH[(8 + cc) * 128:(9 + cc) * 128, c_lo:c_lo + W], "cg", [gate])
                A(lambda e: e.activation(gate.t[:], gate.t[:], AF.Sigmoid), [gate], [gate])
                V(lambda e: e.tensor_tensor(z.t[:], val.t[:], gate.t[:], ALU.mult), [val, gate], [z])
                V(lambda e, cc=cc: e.tensor_scalar(zc.t[:, cc, :], z.t[:, 0:TOK], convw.t[:, cc, 0:1], convv.t[:, 0, cc:cc + 1], ALU.mult, ALU.add), [z, convw, convv], [zc])
                for k in range(1, KW):
                    V(lambda e, cc=cc, k=k: e.scalar_tensor_tensor(zc.t[:, cc, :], z.t[:, k:k + TOK], convw.t[:, cc, k:k + 1], zc.t[:, cc, :], ALU.mult, ALU.add), [z, convw, zc], [zc])
                A(lambda e, cc=cc: e.activation(sq.t[:], zc.t[:, cc, :], AF.Square), [zc], [sq])
                for half in range(2):
                    hsl = slice(half * 512, half * 512 + 512)
                    M(lambda e, cc=cc, hsl=hsl, half=half: e.matmul(pS[half].t[:], ones.t[:], zc.t[:, cc, hsl], start=(cc == 0), stop=(cc == 7)), [ones, zc], [pS[half]])
                    M(lambda e, cc=cc, hsl=hsl, half=half: e.matmul(pQ[half].t[:], ones.t[:], sq.t[:, hsl], start=(cc == 0), stop=(cc == 7)), [ones, sq], [pQ[half]])
            for half in range(2):
                hsl = slice(half * 512, half * 512 + 512)
                A(lambda e, hsl=hsl, half=half: e.mul(mean.t[:, hsl], pS[half].t[:], 1.0 / DC), [pS[half]], [mean])
                V(lambda e, hsl=hsl: e.tensor_tensor(tmpc.t[:, hsl], mean.t[:, hsl], mean.t[:, hsl], ALU.mult), [mean], [tmpc])
                V(lambda e, hsl=hsl, half=half: e.scalar_tensor_tensor(rstd.t[:, hsl], pQ[half].t[:], 1.0 / DC, tmpc.t[:, hsl], ALU.mult, ALU.subtract), [pQ[half], tmpc], [rstd])
            V(lambda e: e.tensor_scalar(rstd.t[:], rstd.t[:], LN_EPS, None, ALU.add), [rstd], [rstd])
            A(lambda e: e.activation(rstd.t[:], rstd.t[:], AF.Sqrt), [rstd], [rstd])
            V(lambda e: e.reciprocal(rstd.t[:], rstd.t[:]), [rstd], [rstd])
            for cc in range(8):
                V(lambda e, cc=cc: e.tensor_tensor(tmpc.t[:], zc.t[:, cc, :], mean.t[:], ALU.subtract), [zc, mean], [tmpc])
                V(lambda e: e.tensor_tensor(tmpc.t[:], tmpc.t[:], rstd.t[:], ALU.mult), [tmpc, rstd], [tmpc])
                A(lambda e, cc=cc: e.activation(yTc.t[:, cc, :], tmpc.t[:], AF.Silu, bias=convv.t[:, 2, cc:cc + 1], scale=convv.t[:, 1, cc:cc + 1]), [tmpc, convv], [yTc])
            S.barrier(); S.emit()
        if stop <= 4:
            r_nc = _finish(nc, S, st, dram_in)
            st_y2.close(); st_y.close()
            return r_nc

        wout_d = din("w_out_r", [16, 128, 16, 128])
        st_x = ExitStack()
        xT = sb(st_x, "xT", [128, 16, TOK]); t_xT = Tok("xT")
        BxT = Buf(xT, t_xT)
        with ExitStack() as ph2:
            transpose_pass(ph2, "c", xh[HALO:HALO + TOK, :], TOK // 128, False, lambda fc, o, n: xT[:, fc, o:o + n], t_xT)
            S.barrier(); S.emit()
        with ExitStack() as ph:
            slabs = [B(ph, "wo%d" % i, [128, 16, 128], BF16) for i in range(3)]
            pp = [PB(ph, "ppo%d" % i, [128, 512]) for i in range(4)]
            pi = 0
            for oc in range(16):
                sl = slabs[oc % 3]
                LD("pool", sl.t[:], wout_d[oc], "w%d" % (oc % 3), [sl])
                for half in range(2):
                    hsl = slice(half * 512, half * 512 + 512)
                    pt = pp[pi % 4]; pi += 1
                    for kk in range(16):
                        M(lambda e, pt=pt, sl=sl, kk=kk, hsl=hsl: e.matmul(pt.t[:], sl.t[:, kk, :], (yTc.t[:, kk, hsl] if kk < 8 else yTr.t[:, kk - 8, hsl]), start=(kk == 0), stop=(kk == 15)), [sl, yTc, yTr], [pt], sig=(kk == 15))
                    V(lambda e, pt=pt, oc=oc, hsl=hsl: e.scalar_tensor_tensor(xT[:, oc, hsl], pt.t[:], modv[:, 32 + oc:33 + oc], xT[:, oc, hsl], ALU.mult, ALU.add), [pt, Bmodv, BxT], [BxT])
            S.barrier(); S.emit()
        if stop <= 5:
            r_nc = _finish(nc, S, st, dram_in, dbg_fn=lambda: S.dma("sp", dbg["xT"], xT[:], "dbg", reads=[t_xT], batch=True, is_out=True) if debug else None)
            st_x.close(); st_y2.close(); st_y.close()
            return r_nc

        wr_d = din("wr", [128, 16, 36]); br_d = din("br", [128, 36])
        wg_d = din("wg_r", [NE, 4, 128, 16, 128]); wu_d = din("wu_r", [NE, 4, 128, 16, 128]); wd_d = din("wd_r", [NE, 4, 128, 4, 512])
        WT = nc.dram_tensor("WT", [NE, TOK], F32, kind="Internal").ap()
        with ExitStack() as ph:
            hn2T = B(ph, "hn2T", [128, 16, TOK], BF16)
            with ExitStack() as p2:
                wr = B(p2, "wr", [128, 16, 36]); br = B(p2, "br", [128, 36])
                LD("sp", wr.t[:], wr_d, "const", [wr], batch=True); LD("sp", br.t[:], br_d, "const", [br], batch=True)
                sq = B(p2, "nsq", [128, TOK]); rs2 = B(p2, "rs2", [128, TOK]); hf = B(p2, "hf", [128, TOK])
                pS = [PB(p2, "nS%d" % i, [128, 512]) for i in range(2)]; pL = [PB(p2, "nL%d" % i, [128, 512]) for i in range(2)]
                pT = PB(p2, "nT", [128, 512]); pW = [PB(p2, "nW%d" % i, [128, 512]) for i in range(2)]
                for fc in range(16):
                    A(lambda e, fc=fc: e.activation(sq.t[:], xT[:, fc, :], AF.Square), [BxT], [sq])
                    for half in range(2):
                        hsl = slice(half * 512, half * 512 + 512)
                        M(lambda e, fc=fc, hsl=hsl, half=half: e.matmul(pS[half].t[:], ones.t[:], sq.t[:, hsl], start=(fc == 0), stop=(fc == 15)), [ones, sq], [pS[half]])
                for half in range(2):
                    hsl = slice(half * 512, half * 512 + 512)
                    V(lambda e, hsl=hsl, half=half: e.tensor_scalar(rs2.t[:, hsl], pS[half].t[:], 1.0 / D, RMS_EPS, ALU.mult, ALU.add), [pS[half]], [rs2])
                A(lambda e: e.activation(rs2.t[:], rs2.t[:], AF.Sqrt), [rs2], [rs2])
                V(lambda e: e.reciprocal(rs2.t[:], rs2.t[:]), [rs2], [rs2])
                for fc in range(16):
                    V(lambda e, fc=fc: e.tensor_tensor(hf.t[:], xT[:, fc, :], rs2.t[:], ALU.mult), [BxT, rs2], [hf])
                    V(lambda e, fc=fc: e.tensor_scalar(hf.t[:], hf.t[:], a2[:, fc:fc + 1], modv[:, 48 + fc:49 + fc], ALU.mult, ALU.add), [hf, Ba2, Bmodv], [hf])
                    A(lambda e, fc=fc: e.copy(hn2T.t[:, fc, :], hf.t[:]), [hf], [hn2T])
                    for half in range(2):
                        hsl = slice(half * 512, half * 512 + 512)
                        M(lambda e, fc=fc, hsl=hsl, half=half: e.matmul(pL[half].t[0:36, :], wr.t[:, fc, :], hf.t[:, hsl], start=(fc == 0), stop=(fc == 15)), [wr, hf], [pL[half]])
                LT = B(p2, "LT", [36, TOK]); L = B(p2, "L", [128, 8, 36])
                for half in range(2):
                    A(lambda e, half=half: e.copy(LT.t[:, half * 512:(half + 1) * 512], pL[half].t[0:36, :]), [pL[half]], [LT])
                for t_ in range(8):
                    M(lambda e, t_=t_: e.matmul(pT.t[:, t_ * 36:(t_ + 1) * 36], LT.t[:, t_ * 128:(t_ + 1) * 128], ident[0:36, 0:36], start=True, stop=True), [LT, Bident], [pT], sig=(t_ == 7))
                V(lambda e: e.tensor_tensor(L.t[:], pT.t[:, 0:288].rearrange("p (t c) -> p t c", c=36), br.t[:].rearrange("p (o c) -> p o c", o=1).to_broadcast([128, 8, 36]), ALU.add), [pT, br], [L])
                gl = L.t[:, :, 0:4]
                el = L.t[:, :, 4:36]
                gmax = B(p2, "gmax", [128, 8]); gmask = B(p2, "gmask", [128, 8, 4]); gex = B(p2, "gex", [128, 8, 4]); pg = B(p2, "pg", [128, 8])
                elm = B(p2, "elm", [128, 8, 32]); m1 = B(p2, "m1", [128, 8]); m2 = B(p2, "m2", [128, 8]); k1 = B(p2, "k1", [128, 8, 32]); k2 = B(p2, "k2", [128, 8, 32])
                w1 = B(p2, "w1", [128, 8]); w2_ = B(p2, "w2_", [128, 8]); wt = B(p2, "wt", [128, 8, 32])
                bc8 = lambda b_, n: b_.t[:].rearrange("p (t o) -> p t o", o=1).to_broadcast([128, 8, n])
                V(lambda e: e.tensor_reduce(gmax.t[:], gl, AX.X, ALU.max), [L], [gmax])
                V(lambda e: e.tensor_tensor(gmask.t[:], gl, bc8(gmax, 4), ALU.is_equal), [L, gmax], [gmask])
                V(lambda e: e.tensor_tensor(gex.t[:], gl, bc8(gmax, 4), ALU.subtract), [L, gmax], [gex])
                A(lambda e: e.activation(gex.t[:], gex.t[:], AF.Exp), [gex], [gex])
                V(lambda e: e.tensor_reduce(pg.t[:], gex.t[:], AX.X, ALU.add), [gex], [pg])
                V(lambda e: e.reciprocal(pg.t[:], pg.t[:]), [pg], [pg])
                V(lambda e: e.tensor_scalar(gmask.t[:], gmask.t[:], 1e30, -1e30, ALU.mult, ALU.add), [gmask], [gmask])
                V(lambda e: e.tensor_tensor(elm.t[:].rearrange("p t (g x) -> p t g x", x=8), el.rearrange("p t (g x) -> p t g x", x=8),
                                            gmask.t[:].rearrange("p t (g o) -> p t g o", o=1).to_broadcast([128, 8, 4, 8]), ALU.add), [L, gmask], [elm])
                V(lambda e: e.tensor_reduce(m1.t[:], elm.t[:], AX.X, ALU.max), [elm], [m1])
                V(lambda e: e.tensor_tensor(k1.t[:], elm.t[:], bc8(m1, 32), ALU.is_equal), [elm, m1], [k1])
                V(lambda e: e.scalar_tensor_tensor(elm.t[:], k1.t[:], -1e30, elm.t[:], ALU.mult, ALU.add), [k1, elm], [elm])
                V(lambda e: e.tensor_reduce(m2.t[:], elm.t[:], AX.X, ALU.max), [elm], [m2])
                V(lambda e: e.tensor_tensor(k2.t[:], elm.t[:], bc8(m2, 32), ALU.is_equal), [elm, m2], [k2])
                V(lambda e: e.tensor_tensor(w1.t[:], m1.t[:], m2.t[:], ALU.subtract), [m1, m2], [w1])
                A(lambda e: e.activation(w2_.t[:], w1.t[:], AF.Sigmoid, scale=-1.0), [w1], [w2_])
                A(lambda e: e.activation(w1.t[:], w1.t[:], AF.Sigmoid), [w1], [w1])
                V(lambda e: e.tensor_tensor(w1.t[:], w1.t[:], pg.t[:], ALU.mult), [w1, pg], [w1])
                V(lambda e: e.tensor_tensor(w2_.t[:], w2_.t[:], pg.t[:], ALU.mult), [w2_, pg], [w2_])
                V(lambda e: e.tensor_tensor(wt.t[:], k1.t[:], bc8(w1, 32), ALU.mult), [k1, w1], [wt])
                V(lambda e: e.tensor_tensor(k2.t[:], k2.t[:], bc8(w2_, 32), ALU.mult), [k2, w2_], [k2])
                V(lambda e: e.tensor_tensor(wt.t[:], wt.t[:], k2.t[:], ALU.add), [wt, k2], [wt])
                wtT = B(p2, "wtT", [32, TOK])
                for t_ in range(8):
                    M(lambda e, t_=t_: e.matmul(pW[t_ // 4].t[0:32, (t_ % 4) * 128:(t_ % 4 + 1) * 128], wt.t[:, t_, :], ident[:], start=True, stop=True), [wt, Bident], [pW[t_ // 4]], sig=(t_ % 4 == 3))
                for half in range(2):
                    A(lambda e, half=half: e.copy(wtT.t[:, half * 512:(half + 1) * 512], pW[half].t[0:32, :]), [pW[half]], [wtT])
                S.dma("sp", WT, wtT.t[:], "wt", reads=[wtT.k])
                S.barrier(); S.emit()
            with ExitStack() as p2:
                gu = [B(p2, "gu%d" % i, [128, 16, 128], BF16) for i in range(4)]
                gu = [Buf(g0.t[:], g0.k) for g0 in gu]
                for ysrc in (yTr, yTc):
                    for j4 in range(4):
                        gu.append(Buf(ysrc.t[:, 2 * j4:2 * j4 + 2, :].rearrange("p a (b c) -> p (a b) c", c=128)))
                NGU = len(gu)
                wdn = [B(p2, "wdn%d" % i, [128, 4, 512], BF16) for i in range(4)]
                act = B(p2, "act", [128, 4, TOK], BF16)
                wb = [B(p2, "wb%d" % i, [128, TOK]) for i in range(2)]
                sl_ = [B(p2, "sl%d" % i, [128, 512]) for i in range(2)]
                pG = [PB(p2, "pG%d" % i, [128, 512]) for i in range(2)]; pU = [PB(p2, "pU%d" % i, [128, 512]) for i in range(2)]
                pD = [PB(p2, "pD%d" % i, [128, 512]) for i in range(4)]
                gi = 0; di = 0; si = 0
                for ex in range(NE):
                    wbe = wb[ex % 2]
                    LD("sp", wbe.t[:], WT[ex:ex + 1, :].to_broadcast([128, TOK]), "wb%d" % (ex % 2), [wbe])
                    for dc in range(4):
                        g_ = gu[gi % NGU]; LD("pool", g_.t, wg_d[ex, dc], "gu%d" % (gi % NGU), [g_]); gi += 1
                        u_ = gu[gi % NGU]; LD("pool", u_.t, wu_d[ex, dc], "gu%d" % (gi % NGU), [u_]); gi += 1
                        for half in range(2):
                            hsl = slice(half * 512, half * 512 + 512)
                            for kk in range(16):
                                M(lambda e, g_=g_, kk=kk, hsl=hsl, half=half: e.matmul(pG[half].t[:], g_.t[:, kk, :], hn2T.t[:, kk, hsl], start=(kk == 0), stop=(kk == 15)), [g_, hn2T], [pG[half]], sig=(kk == 15))
                            for kk in range(16):
                                M(lambda e, u_=u_, kk=kk, hsl=hsl, half=half: e.matmul(pU[half].t[:], u_.t[:, kk, :], hn2T.t[:, kk, hsl], start=(kk == 0), stop=(kk == 15)), [u_, hn2T], [pU[half]], sig=(kk == 15))
                            s_ = sl_[si % 2]; si += 1
                            A(lambda e, s_=s_, half=half: e.activation(s_.t[:], pG[half].t[:], AF.Silu), [pG[half]], [s_])
                            P(lambda e, s_=s_, wbe=wbe, hsl=hsl: e.tensor_tensor(s_.t[:], s_.t[:], wbe.t[:, hsl], ALU.mult), [s_, wbe], [s_])
                            V(lambda e, s_=s_, dc=dc, hsl=hsl, half=half: e.tensor_tensor(act.t[:, dc, hsl], pU[half].t[:], s_.t[:], ALU.mult), [pU[half], s_], [act])
                    for og in range(4):
                        wd_ = wdn[di % 4]; LD("pool", wd_.t[:], wd_d[ex, og], "wd%d" % (di % 4), [wd_]); di += 1
                        for o4 in range(4):
                            oc = og * 4 + o4
                            for half in range(2):
                                hsl = slice(half * 512, half * 512 + 512)
                                pt = pD[(o4 * 2 + half) % 4]
                                for kk in range(4):
                                    M(lambda e, pt=pt, wd_=wd_, kk=kk, o4=o4, hsl=hsl: e.matmul(pt.t[:], wd_.t[:, kk, o4 * 128:(o4 + 1) * 128], act.t[:, kk, hsl], start=(kk == 0), stop=(kk == 3)), [wd_, act], [pt], sig=(kk == 3))
                                V(lambda e, pt=pt, oc=oc, hsl=hsl: e.scalar_tensor_tensor(xT[:, oc, hsl], pt.t[:], modv[:, 80 + oc:81 + oc], xT[:, oc, hsl], ALU.mult, ALU.add), [pt, Bmodv, BxT], [BxT])
                S.barrier(); S.emit()
        if stop <= 6:
            r_nc = _finish(nc, S, st, dram_in, dbg_fn=lambda: S.dma("sp", dbg["xT"], xT[:], "dbg", reads=[t_xT], batch=True, is_out=True) if debug else None)
            st_x.close(); st_y2.close(); st_y.close()
            return r_nc

        with ExitStack() as ph:
            dgf = B(ph, "dgf", [128, 16, 128]); sqs = [B(ph, "fsq%d" % i, [128, 128]) for i in range(2)]
            rt = B(ph, "frt", [128, 8]); ot = [B(ph, "ot%d" % i, [128, D]) for i in range(2)]
            pss = PB(ph, "pss", [128, 8]); pf = [PB(ph, "pf%d" % i, [128, 512]) for i in range(4)]
            for fc in range(16):
                V(lambda e, fc=fc: e.tensor_scalar(dgf.t[:, fc, :], ident[:], gnv[:, 2, fc:fc + 1], None, ALU.mult), [Bident, Bgnv], [dgf])
            qi = 0
            for t_ in range(8):
                for fc in range(16):
                    q_ = sqs[qi % 2]; qi += 1
                    A(lambda e, q_=q_, fc=fc, t_=t_: e.activation(q_.t[:], xT[:, fc, t_ * 128:(t_ + 1) * 128], AF.Square), [BxT], [q_])
                    M(lambda e, q_=q_, fc=fc, t_=t_: e.matmul(pss.t[:, t_:t_ + 1], q_.t[:], ones.t[:, 0:1], start=(fc == 0), stop=(fc == 15)), [q_, ones], [pss])
            V(lambda e: e.tensor_scalar(rt.t[:], pss.t[:], 1.0 / D, RMS_EPS, ALU.mult, ALU.add), [pss], [rt])
            A(lambda e: e.activation(rt.t[:], rt.t[:], AF.Sqrt), [rt], [rt])
            V(lambda e: e.reciprocal(rt.t[:], rt.t[:]), [rt], [rt])
            pi = 0
            for t_ in range(8):
                o_ = ot[t_ % 2]
                for fg in range(4):
                    pt = pf[pi % 4]; pi += 1
                    for j4 in range(4):
                        fc = fg * 4 + j4
                        M(lambda e, pt=pt, fc=fc, j4=j4, t_=t_: e.matmul(pt.t[:, j4 * 128:(j4 + 1) * 128], xT[:, fc, t_ * 128:(t_ + 1) * 128], dgf.t[:, fc, :], start=True, stop=True), [BxT, dgf], [pt], sig=(j4 == 3))
                    if fg % 2 == 0:
                        V(lambda e, pt=pt, o_=o_, fg=fg, t_=t_: e.tensor_scalar(o_.t[:, fg * 512:(fg + 1) * 512], pt.t[:], rt.t[:, t_:t_ + 1], None, ALU.mult), [pt, rt], [o_])
                    else:
                        A(lambda e, pt=pt, o_=o_, fg=fg, t_=t_: e.mul(o_.t[:, fg * 512:(fg + 1) * 512], pt.t[:], rt.t[:, t_:t_ + 1]), [pt, rt], [o_])
                S.dma("sp", out_d[t_ * 128:(t_ + 1) * 128, :], o_.t[:], "out%d" % (t_ % 2), reads=[o_.k], is_out=True)
            r_nc = _finish(nc, S, st, dram_in)
        st_x.close(); st_y2.close(); st_y.close()
        return r_nc


_IN_NAMES = []


def _finish(nc, S, st, dram_in=None, dbg_fn=None):
    _IN_NAMES[:] = list(dram_in.keys())
    if dbg_fn is not None:
        dbg_fn()
    S.barrier()
    S.emit(final=True)
    return nc


def _colvec(v, n):
    return np.ascontiguousarray(np.asarray(v, np.float32).reshape(n, 128).T)


def _kslab(w, nchunk):
    K, nc_ = w.shape
    pad = nchunk * 128 - nc_
    if pad:
        w = np.concatenate([w, np.zeros((K, pad), np.float32)], axis=1)
    return np.ascontiguousarray(w.reshape(K // 128, 128, nchunk, 128).transpose(2, 1, 0, 3))


def prep_shared(inp):
    f = lambda k: np.asarray(inp[k], np.float32)
    sh = {}
    sh["w_ada_r"] = _kslab(f("w_ada")[0], 96)
    sh["b_ada_r"] = _colvec(f("b_ada")[0], 96)
    g = np.stack([f("norm1_g")[0], f("norm2_g")[0], f("normf_g")])
    sh["gnorm"] = np.ascontiguousarray(g.reshape(3, 16, 128).transpose(2, 0, 1))
    sh["w_in_r"] = _kslab(f("w_in")[0], NCC)
    sh["ident"] = np.eye(128, dtype=np.float32)
    idx = np.arange(128)
    bm = lambda L: (idx[:, None] // L) == (idx[None, :] // L)
    cm = np.stack([idx[:, None] < idx[None, :], idx[:, None] <= idx[None, :], idx[:, None] > idx[None, :], idx[:, None] >= idx[None, :],
                   bm(16), bm(32) & ~bm(16), bm(64) & ~bm(32), ~bm(64)], axis=1)
    sh["cmask"] = np.ascontiguousarray(cm.astype(np.float32))
    rst = np.ones((128, 512), np.float32); rst[:, ::128] = 0.0
    sh["rst"] = rst
    sh["blk64"] = np.kron(np.eye(2, dtype=np.float32), np.ones((64, 64), np.float32))
    sh["ones"] = np.ones((128, 128), np.float32)
    sh["ident2"] = np.ascontiguousarray(np.concatenate([np.eye(64, dtype=np.float32)] * 2, axis=0))
    mu = np.zeros((2, NCC * 128), np.float32)
    mu[0, 2048:2048 + 3488] = f("mu_prev")[0]
    mu[1, 2048:2048 + 3488] = f("mu_next")[0]
    sh["mu"] = np.ascontiguousarray(mu.reshape(2, NCC, 128).transpose(2, 0, 1))
    rvn = ["w0_f", "w0_b", "a0_f", "a0_b", "k_k", "k_a", "r_k", "lnx_g", "lnx_b"]
    sh["rv"] = np.ascontiguousarray(np.stack([f(k)[0].reshape(8, 128).T for k in rvn], axis=1))
    sh["w2"] = np.ascontiguousarray(np.concatenate([f("w2_f")[0], f("w2_b")[0]], axis=0))
    sh["a2w"] = np.ascontiguousarray(np.concatenate([f("a2_f")[0], f("a2_b")[0]], axis=0))
    sh["g2a"] = np.ascontiguousarray(f("g2")[0][:128])
    sh["g2b"] = np.ascontiguousarray(f("g2")[0][128:160])
    cw = f("conv_w")[0][:, 0, :]
    sh["convw"] = np.ascontiguousarray(cw.reshape(KW, 8, 128).transpose(2, 1, 0))
    sh["convv"] = np.ascontiguousarray(np.stack([f(k)[0].reshape(8, 128).T for k in ("conv_b", "conv_ln_g", "conv_ln_b")], axis=1))
    sh["w_out_r"] = _kslab(f("w_out")[0], 16)
    wr = np.concatenate([f("w_rg")[0], f("w_re")[0]], axis=1)
    sh["wr"] = np.ascontiguousarray(wr.reshape(16, 128, 36).transpose(1, 0, 2))
    br = np.concatenate([f("b_rg")[0], f("b_re")[0]])
    sh["br"] = np.ascontiguousarray(np.broadcast_to(br[None, :], (128, 36)))
    wg, wu, wd = f("w_gate")[0], f("w_up")[0], f("w_down")[0]
    sh["wg_r"] = np.ascontiguousarray(wg.reshape(NE, 16, 128, 4, 128).transpose(0, 3, 2, 1, 4))
    sh["wu_r"] = np.ascontiguousarray(wu.reshape(NE, 16, 128, 4, 128).transpose(0, 3, 2, 1, 4))
    sh["wd_r"] = np.ascontiguousarray(wd.reshape(NE, 4, 128, 4, 512).transpose(0, 3, 2, 1, 4))
    return sh


def prep_core(inp, i):
    b, q = i // 4, i % 4
    x = np.asarray(inp["x"], np.float32)
    m = {}
    m["xf"] = np.ascontiguousarray(x[b])
    lo = q * TOK - HALO
    xh = np.zeros((TH, D), np.float32)
    hm = np.zeros((TH,), np.float32)
    s0, s1 = max(lo, 0), min(lo + TH, SEQ)
    xh[s0 - lo:s1 - lo] = x[b, s0:s1]
    hm[s0 - lo:s1 - lo] = 1.0
    m["xh"] = xh
    m["hmask"] = np.ascontiguousarray(np.broadcast_to(hm[None, :], (128, TH)))
    qf = np.zeros((128, 4), np.float32)
    qf[:, q] = 1.0
    m["qflag"] = qf
    m["c_col"] = _colvec(np.asarray(inp["c"], np.float32)[b], 16)
    return m


_NC_CACHE = {}


def kernel(**inputs):
    if "nc" not in _NC_CACHE:
        _NC_CACHE["nc"] = build()
    nc = _NC_CACHE["nc"]
    sh = prep_shared(inputs)
    in_maps = []
    for i in range(NCORES):
        m = dict(sh)
        m.update(prep_core(inputs, i))
        in_maps.append({k: m[k] for k in _IN_NAMES})
    res = run_bass_kernel_spmd(nc, in_maps, core_ids=list(range(NCORES)))
    out = np.zeros((2, SEQ, D), np.float32)
    for i in range(NCORES):
        b, q = i // 4, i % 4
        out[b, q * TOK:(q + 1) * TOK] = res.results[i]["out"]
    return out
```

```python
import numpy as np
from contextlib import ExitStack
import concourse.bass as bass
import concourse.mybir as mybir
from concourse.bass_utils import run_bass_kernel_spmd

F32 = mybir.dt.float32
BF16 = mybir.dt.bfloat16
AF = mybir.ActivationFunctionType
ALU = mybir.AluOpType
AX = mybir.AxisListType

NCORES = 8
D = 2048
SEQ = 4096
TOK = 1024
HALO = 64
TH = TOK + 2 * HALO
DC = 1024
DR = 1024
NCOL = 5536
NCC = 44
NRC = 28
KW = 31
NE = 32
DE = 512
CH = 128
RMS_EPS = 1e-6
LN_EPS = 1e-5
GN_EPS = 64e-5


class Ev:
    __slots__ = ("sem", "val")

    def __init__(self, sem, val=None):
        self.sem = sem
        self.val = val


class Tok:
    __slots__ = ("w", "r", "name")

    def __init__(self, name=""):
        self.w = None
        self.r = []
        self.name = name


class DSem:
    def __init__(self, sem, batch):
        self.sem = sem
        self.total = 0
        self.batch = batch
        self.ev = Ev(sem, 0) if batch else None


class Sched:
    ENG = ("pe", "act", "dve", "pool", "sp")

    def __init__(self, nc, stack, n_dma_sems=72):
        self.nc = nc
        self.sem = {e: stack.enter_context(nc.semaphore("s_" + e)) for e in self.ENG}
        self.count = {e: 0 for e in self.ENG}
        self.ops = {e: [] for e in self.ENG}
        self.pending = {e: [] for e in self.ENG}
        self.free_dsems = [stack.enter_context(nc.semaphore("d%d" % i)) for i in range(n_dma_sems)]
        self.dsems = {}
        self.out_evs = []
        self.waited = {e: {} for e in self.ENG}
        self.nops = 0

    def dsem(self, key, batch=False):
        if key not in self.dsems:
            self.dsems[key] = DSem(self.free_dsems.pop(), batch)
        return self.dsems[key]

    def _collect(self, reads, writes):
        waits = []
        for t in reads:
            if t.w is not None:
                waits.append(t.w)
        for t in writes:
            if t.w is not None:
                waits.append(t.w)
            waits.extend(t.r)
        return waits

    def op(self, eng, fn, reads=(), writes=(), signal=True):
        waits = self._collect(reads, writes)
        ev = Ev(self.sem[eng])
        if signal:
            self.count[eng] += 1
            ev.val = self.count[eng]
            for p in self.pending[eng]:
                p.val = ev.val
            self.pending[eng] = []
        else:
            self.pending[eng].append(ev)
        self.ops[eng].append((waits, fn, (self.sem[eng], 1) if signal else None))
        for t in reads:
            if len(t.r) > 6:
                t.r = t.r[-6:] + [e for e in t.r[:-6] if e.val is None]
            t.r.append(ev)
        for t in writes:
            t.w = ev
            t.r = []
        self.nops += 1
        return ev

    def dma(self, q, out, in_, key, reads=(), writes=(), batch=False, is_out=False, **kw):
        ds = self.dsem(key, batch)
        waits = self._collect(reads, writes)
        ds.total += 16
        if ds.batch:
            ev = ds.ev
            ev.val = ds.total
        else:
            ev = Ev(ds.sem, ds.total)

        def fn(e, out=out, in_=in_, kw=kw):
            return e.dma_start(out=out, in_=in_, **kw)

        self.ops[q].append((waits, fn, (ds.sem, 16)))
        for t in reads:
            t.r.append(ev)
        for t in writes:
            t.w = ev
            t.r = []
        if is_out:
            self.out_evs.append(ev)
        self.nops += 1
        return ev

    def barrier(self):
        evs = []
        for e in self.ENG:
            assert not self.pending[e], "engine %s has non-signalled tail" % e
            if self.count[e]:
                evs.append(Ev(self.sem[e], self.count[e]))
        for ds in self.dsems.values():
            if ds.total:
                evs.append(Ev(ds.sem, ds.total))
        for e in self.ENG:
            self.ops[e].append((list(evs), None, None))

    def emit(self, final=False):
        nc = self.nc
        if final:
            self.ops["sp"].append((list(self.out_evs), None, None))
        for e in self.ENG:
            assert not self.pending[e], "engine %s ends with non-signaling op" % e
        with nc.Block() as block:
            def mk(ename):
                def body(eng):
                    waited = self.waited[ename]
                    for waits, fn, inc in self.ops[ename]:
                        need = {}
                        for ev in waits:
                            assert ev.val is not None
                            if ename == "pe" and ev.sem is self.sem["pe"]:
                                continue
                            k = id(ev.sem)
                            if need.get(k, (None, 0))[1] < ev.val:
                                need[k] = (ev.sem, ev.val)
                        for k, (s, v) in need.items():
                            if waited.get(k, 0) >= v:
                                continue
                            waited[k] = v
                            eng.wait_ge(s, v)
                        if fn is not None:
                            ins = fn(eng)
                            if inc is not None:
                                ins.then_inc(inc[0], inc[1])
                    self.ops[ename] = []
                return body
            block.tensor(mk("pe"))
            block.scalar(mk("act"))
            block.vector(mk("dve"))
            block.gpsimd(mk("pool"))
            block.sync(mk("sp"))


class Ring:
    def __init__(self, aps, name):
        self.aps = aps
        self.toks = [Tok("%s%d" % (name, i)) for i in range(len(aps))]
        self.i = 0

    def next(self):
        k = self.i % len(self.aps)
        self.i += 1
        return self.aps[k], self.toks[k], k


def build(stop=99, debug=False, dbg_pc=1, dbg_d=0, dbg_tb=0):
    nc = bass.Bass("TRN2", target_bir_lowering=False)
    dram_in = {}

    def din(name, shape, dt=F32):
        dram_in[name] = nc.dram_tensor(name, list(shape), dt, kind="ExternalInput").ap()
        return dram_in[name]

    xf = din("xf", [SEQ, D])
    xh = din("xh", [TH, D])
    hmask_d = din("hmask", [128, TH])
    qflag_d = din("qflag", [128, 4])
    ccol_d = din("c_col", [128, 16])
    wada_d = din("w_ada_r", [24, 128, 16, 512])
    bada_d = din("b_ada_r", [128, 96])
    gn_d = din("gnorm", [128, 3, 16])
    win_d = din("w_in_r", [NCC, 128, 16, 128])
    ident_d = din("ident", [128, 128])
    out_d = nc.dram_tensor("out", [TOK, D], F32, kind="ExternalOutput").ap()

    PT = nc.dram_tensor("PT", [NRC * 128, SEQ], F32, kind="ExternalOutput" if debug == 2 else "Internal").ap()
    PH = nc.dram_tensor("PH", [NCC * 128, TH], F32, kind="ExternalOutput" if debug == 2 else "Internal").ap()
    dbg = {}
    if debug:
        dbg["modv"] = nc.dram_tensor("dbg_modv", [128, 96], F32, kind="ExternalOutput").ap()
        dbg["xT"] = nc.dram_tensor("dbg_xT", [128, 16, TOK], F32, kind="ExternalOutput").ap()
    dbg_y = nc.dram_tensor("dbg_y", [128, 16, TOK], BF16, kind="ExternalOutput").ap() if debug else None

    with ExitStack() as st:
        S = Sched(nc, st)

        uid = [0]

        def sb(stack, name, shape, dt=F32):
            uid[0] += 1
            return stack.enter_context(nc.sbuf_tensor("sb%d_%s" % (uid[0], name), list(shape), dt))

        def ps(stack, name, shape, dt=F32):
            uid[0] += 1
            return stack.enter_context(nc.psum_tensor("ps%d_%s" % (uid[0], name), list(shape), dt))

        ident = sb(st, "ident", [128, 128]); t_ident = Tok("ident")
        identb = sb(st, "identb", [128, 128], BF16); t_identb = Tok("identb")
        modv = sb(st, "modv", [128, 96]); t_modv = Tok("modv")
        gnv = sb(st, "gnv", [128, 3, 16]); t_gnv = Tok("gnv")
        a1 = sb(st, "a1", [128, 16]); t_a1 = Tok("a1")
        a2 = sb(st, "a2", [128, 16]); t_a2 = Tok("a2")
        qflag = sb(st, "qflag", [128, 4]); t_qflag = Tok("qflag")

        stage_tbl = {}
        if debug == 3:
            for nm_ in ("r", "k", "lw", "lr", "kk", "kd", "bb", "cum", "pref", "Y0s", "QT"):
                stage_tbl[nm_] = ([128, 512], F32)
            for nm_ in ("At", "Rt", "Bt", "Kt", "Bh", "Kh", "Vb"):
                stage_tbl[nm_] = ([128, 512], BF16)
            for nm_ in ("Atok", "Vtok", "AkT0", "ArbT0", "ArkT0", "TT0", "AkT1", "ArbT1", "ArkT1", "TT1", "N1", "NT1", "Gtok", "U0tok"):
                stage_tbl[nm_] = ([128, 4, 128], BF16)
            stage_tbl["WC"] = ([128, 4], F32); stage_tbl["Zloc"] = ([128, 4, 64], F32); stage_tbl["PhiT"] = ([128, 4, 64], F32); stage_tbl["Zin"] = ([128, 64], F32)
        stage_slots = {k_: (sb(st, "stage_" + k_, v_[0], v_[1]), Tok()) for k_, v_ in stage_tbl.items()}
        S.dma("sp", ident[:], ident_d, "const", writes=[t_ident], batch=True)
        S.dma("pool", identb[:], ident_d, "constc", writes=[t_identb], batch=True)
        S.dma("sp", gnv[:], gn_d, "const", writes=[t_gnv], batch=True)
        S.dma("sp", qflag[:], qflag_d, "const", writes=[t_qflag], batch=True)

        with ExitStack() as ph:
            ccol = sb(ph, "ccol", [128, 16]); t_ccol = Tok("ccol")
            cs = sb(ph, "cs", [128, 16]); t_cs = Tok("cs")
            bada = sb(ph, "bada", [128, 96]); t_bada = Tok("bada")
            slabs = [sb(ph, "wa%d" % i, [128, 16, 512]) for i in range(3)]
            ring = Ring(slabs, "wa")
            modrow = sb(ph, "modrow", [1, 96 * 128]); t_modrow = Tok("modrow")
            prow = [ps(ph, "prow%d" % i, [1, 512]) for i in range(2)]; t_prow = [Tok("prow0"), Tok("prow1")]
            pmod = ps(ph, "pmod", [128, 96]); t_pmod = Tok("pmod")
            S.dma("sp", ccol[:], ccol_d, "const", writes=[t_ccol], batch=True)
            S.dma("sp", bada[:], bada_d, "const", writes=[t_bada], batch=True)
            S.op("act", lambda e: e.activation(cs[:], ccol[:], AF.Silu), reads=[t_ccol], writes=[t_cs])
            for g in range(24):
                slab, tk, k = ring.next()
                S.dma("sp" if g % 2 == 0 else "act", slab[:], wada_d[g], "wa%d" % k, writes=[tk])
                pt, tp = prow[g % 2], t_prow[g % 2]
                for kk in range(16):
                    S.op("pe", lambda e, slab=slab, kk=kk, pt=pt: e.matmul(pt[0:1, :], cs[:, kk:kk + 1], slab[:, kk, :], start=(kk == 0), stop=(kk == 15)),
                         reads=[tk, t_cs], writes=[tp], signal=(kk == 15))
                if g % 2 == 0:
                    S.op("act", lambda e, pt=pt, g=g: e.copy(modrow[0:1, g * 512:(g + 1) * 512], pt[0:1, :]), reads=[tp], writes=[t_modrow])
                else:
                    S.op("dve", lambda e, pt=pt, g=g: e.tensor_copy(modrow[0:1, g * 512:(g + 1) * 512], pt[0:1, :]), reads=[tp], writes=[t_modrow])
            for cc in range(96):
                S.op("pe", lambda e, cc=cc: e.matmul(pmod[:, cc:cc + 1], modrow[0:1, cc * 128:(cc + 1) * 128], ident[0:1, 0:1], start=True, stop=True),
                     reads=[t_modrow, t_ident], writes=[t_pmod], signal=(cc == 95))
            S.op("dve", lambda e: e.tensor_tensor(modv[:], pmod[:], bada[:], ALU.add),
                 reads=[t_pmod, t_bada], writes=[t_modv])
            S.op("dve", lambda e: e.scalar_tensor_tensor(a1[:], modv[:, 16:32], 1.0, gnv[:, 0, :], ALU.add, ALU.mult),
                 reads=[t_modv, t_gnv], writes=[t_a1])
            S.op("dve", lambda e: e.scalar_tensor_tensor(a2[:], modv[:, 64:80], 1.0, gnv[:, 1, :], ALU.add, ALU.mult),
                 reads=[t_modv, t_gnv], writes=[t_a2])
            if debug:
                S.dma("sp", dbg["modv"], modv[:], "dbg", reads=[t_modv], batch=True, is_out=True)
            S.barrier()
            S.emit()
        if stop <= 0:
            return _finish(nc, S, st, dram_in)

        def transpose_pass(ph, tag, src, ntile, with_norm, dst_fn, t_dst):
            xts = [sb(ph, tag + "xt%d" % i, [128, D]) for i in range(4)]
            xring = Ring(xts, tag + "xt")
            sq = sb(ph, tag + "sq", [128, D]); t_sq = Tok("sq")
            ss = [sb(ph, tag + "ss%d" % i, [128, 1]) for i in range(4)]
            dg = [sb(ph, tag + "dg%d" % i, [128, 128]) for i in range(4)]
            t_ss = [Tok() for _ in range(4)]; t_dg = [Tok() for _ in range(4)]
            ptr = [ps(ph, tag + "ptr%d" % i, [128, 256]) for i in range(4)]
            pring = Ring(ptr, tag + "ptr")
            ev_i = 0
            g = 0
            ti = 0
            while ti < ntile:
                n_in = min(2, ntile - ti)
                tiles = []
                for j in range(n_in):
                    xt, tx, k = xring.next()
                    S.dma("sp" if (ti + j) % 2 == 0 else "act", xt[:], src[(ti + j) * 128:(ti + j + 1) * 128, :],
                          "xt%d" % k, writes=[tx])
                    if with_norm:
                        S.op("act", lambda e, xt=xt, k=k: e.activation(sq[:], xt[:], AF.Square, accum_out=ss[k][:]),
                             reads=[tx], writes=[t_sq, t_ss[k]])
                        S.op("dve", lambda e, k=k: e.tensor_scalar(ss[k][:], ss[k][:], 1.0 / D, RMS_EPS, ALU.mult, ALU.add),
                             reads=[t_ss[k]], writes=[t_ss[k]])
                        S.op("act", lambda e, k=k: e.activation(ss[k][:], ss[k][:], AF.Sqrt),
                             reads=[t_ss[k]], writes=[t_ss[k]])
                        S.op("dve", lambda e, k=k: e.reciprocal(ss[k][:], ss[k][:]),
                             reads=[t_ss[k]], writes=[t_ss[k]])
                        S.op("dve", lambda e, k=k: e.tensor_scalar(dg[k][:], ident[:], ss[k][:], None, ALU.mult),
                             reads=[t_ss[k], t_ident], writes=[t_dg[k]])
                    tiles.append((xt, tx, k))
                for fc in range(16):
                    pt, tp, _ = pring.next()
                    for j, (xt, tx, k) in enumerate(tiles):
                        rhs = dg[k] if with_norm else ident
                        rt = t_dg[k] if with_norm else t_ident
                        S.op("pe", lambda e, pt=pt, xt=xt, rhs=rhs, j=j, fc=fc: e.matmul(
                            pt[:, j * 128:(j + 1) * 128], xt[:, fc * 128:(fc + 1) * 128], rhs[:], start=True, stop=True),
                            reads=[tx, rt], writes=[tp], signal=(j == n_in - 1))
                    eng = "act" if ev_i % 2 == 0 else "dve"
                    ev_i += 1
                    dst = dst_fn(fc, ti * 128, n_in * 128)
                    src_ps = pt[:, 0:n_in * 128]
                    if with_norm:
                        if eng == "act":
                            S.op("act", lambda e, src_ps=src_ps, dst=dst, fc=fc: e.activation(
                                dst, src_ps, AF.Identity, bias=modv[:, fc:fc + 1], scale=a1[:, fc:fc + 1]),
                                reads=[tp, t_modv, t_a1], writes=[t_dst])
                        else:
                            S.op("dve", lambda e, src_ps=src_ps, dst=dst, fc=fc: e.tensor_scalar(
                                dst, src_ps, a1[:, fc:fc + 1], modv[:, fc:fc + 1], ALU.mult, ALU.add),
                                reads=[tp, t_modv, t_a1], writes=[t_dst])
                    else:
                        if eng == "act":
                            S.op("act", lambda e, src_ps=src_ps, dst=dst: e.copy(dst, src_ps), reads=[tp], writes=[t_dst])
                        else:
                            S.op("dve", lambda e, src_ps=src_ps, dst=dst: e.tensor_copy(dst, src_ps), reads=[tp], writes=[t_dst])
                ti += n_in

        def gemm_to_dram(ph, tag, hnT, t_hnT, ntok, chunks, dram, col0=0, mask=None):
            slabs = [sb(ph, tag + "w%d" % i, [128, 16, 128], BF16) for i in range(3)]
            wring = Ring(slabs, tag + "w")
            stg = [sb(ph, tag + "stg%d" % i, [128, ntok]) for i in range(2)]
            sring = Ring(stg, tag + "stg")
            pg = [ps(ph, tag + "pg%d" % i, [128, 512]) for i in range(4)]
            pring = Ring(pg, tag + "pg")
            groups = []
            o = 0
            while o < ntok:
                n = min(512, ntok - o)
                groups.append((o, n))
                o += n
            ev_i = 0
            for gc, rc in chunks:
                slab, tw, k = wring.next()
                S.dma("pool", slab[:], win_d[gc], "w%d" % k, writes=[tw])
                sg, tsg, k2 = sring.next()
                for (o, n) in groups:
                    pt, tp, _ = pring.next()
                    for kk in range(16):
                        S.op("pe", lambda e, pt=pt, slab=slab, kk=kk, o=o, n=n: e.matmul(
                            pt[:, 0:n], slab[:, kk, :], hnT[:, kk, o:o + n], start=(kk == 0), stop=(kk == 15)),
                            reads=[tw, t_hnT], writes=[tp], signal=(kk == 15))
                    if mask is not None:
                        S.op("dve", lambda e, pt=pt, sg=sg, o=o, n=n: e.tensor_tensor(sg[:, o:o + n], pt[:, 0:n], hmask[:, o:o + n], ALU.mult),
                             reads=[tp, t_hmask], writes=[tsg])
                    else:
                        eng = "act" if ev_i % 2 == 0 else "dve"
                        ev_i += 1
                        if eng == "act":
                            S.op("act", lambda e, pt=pt, sg=sg, o=o, n=n: e.copy(sg[:, o:o + n], pt[:, 0:n]), reads=[tp], writes=[tsg])
                        else:
                            S.op("dve", lambda e, pt=pt, sg=sg, o=o, n=n: e.tensor_copy(sg[:, o:o + n], pt[:, 0:n]), reads=[tp], writes=[tsg])
                S.dma("sp", dram[rc * 128:(rc + 1) * 128, col0:col0 + ntok], sg[:], "st%d" % k2, reads=[tsg], is_out=(debug == 2))

        HALF = SEQ // 2
        for half in range(2):
            with ExitStack() as ph:
                hnT = sb(ph, "hnT%d" % half, [128, 16, HALF], BF16); t_hnT = Tok("hnT")
                with ExitStack() as ph2:
                    transpose_pass(ph2, "a%d" % half, xf[half * HALF:(half + 1) * HALF, :], HALF // 128, True,
                                   lambda fc, o, n, hnT=hnT: hnT[:, fc, o:o + n], t_hnT)
                    S.barrier(); S.emit()
                with ExitStack() as ph2:
                    gemm_to_dram(ph2, "a%d" % half, hnT, t_hnT, HALF, [(16 + rc, rc) for rc in range(NRC)], PT, col0=half * HALF)
                    S.barrier(); S.emit()
        if stop <= 1:
            return _finish(nc, S, st, dram_in)
        with ExitStack() as ph:
            hnT = sb(ph, "hnTh", [128, 16, TH], BF16); t_hnT = Tok("hnTh")
            hmask = sb(ph, "hmask", [128, TH]); t_hmask = Tok("hmask")
            S.dma("sp", hmask[:], hmask_d, "hm", writes=[t_hmask])
            with ExitStack() as ph2:
                transpose_pass(ph2, "b", xh, TH // 128, True, lambda fc, o, n: hnT[:, fc, o:o + n], t_hnT)
                S.barrier(); S.emit()
            with ExitStack() as ph2:
                gemm_to_dram(ph2, "b", hnT, t_hnT, TH, [(gc, gc) for gc in range(NCC)], PH, mask=True)
                S.barrier(); S.emit()
        if stop <= 2:
            return _finish(nc, S, st, dram_in)

        class Buf:
            def __init__(self, t, k=None):
                self.t = t
                self.k = k if k is not None else Tok()

        def B(stack, name, shape, dt=F32):
            return Buf(sb(stack, name, shape, dt))

        def PB(stack, name, shape):
            return Buf(ps(stack, name, shape))

        def _ks(bs):
            return [b.k for b in bs]

        def V(fn, r=(), w=()):
            return S.op("dve", fn, reads=_ks(r), writes=_ks(w))

        def A(fn, r=(), w=()):
            return S.op("act", fn, reads=_ks(r), writes=_ks(w))

        def P(fn, r=(), w=()):
            return S.op("pool", fn, reads=_ks(r), writes=_ks(w))

        def M(fn, r=(), w=(), sig=True):
            return S.op("pe", fn, reads=_ks(r), writes=_ks(w), signal=sig)

        def LD(q, dst, src, key, w, batch=False):
            return S.dma(q, dst, src, key, writes=_ks(w), batch=batch)

        dumps = {}

        def DUMP(name, ap, shape, dt=F32):
            if debug != 3:
                return
            dten = nc.dram_tensor("dmp_" + name, list(shape), dt, kind="ExternalOutput").ap()
            dumps[name] = dten
            return dten

        staged = []

        def dump_buf(name, b_, shape, dt=F32, view=None):
            if debug != 3:
                return
            dten = DUMP(name, None, shape, dt)
            slot = Buf(stage_slots[name][0], stage_slots[name][1])
            P(lambda e: e.tensor_copy(slot.t[:], b_.t[:]), [b_], [slot])
            staged.append((dten, slot))

        def flush_dumps():
            for dten, slot in staged:
                S.dma("sp", dten, slot.t[:], "dmp", reads=[slot.k], batch=True, is_out=True)
            staged[:] = []

        Bident = Buf(ident, t_ident); Bidentb = Buf(identb, t_identb); Bmodv = Buf(modv, t_modv)
        Bgnv = Buf(gnv, t_gnv); Ba2 = Buf(a2, t_a2); Bqflag = Buf(qflag, t_qflag)

        cmask = B(st, "cmask", [128, 8, 128]); rst = B(st, "rst", [128, 512]); blk64 = B(st, "blk64", [128, 128])
        ones = B(st, "ones", [128, 128]); ident2 = B(st, "ident2", [128, 64])
        mu = B(st, "mu", [128, 2, NCC]); c0 = B(st, "c0", [128, NCC]); rv = B(st, "rv", [128, 9, 8]); omka = B(st, "omka", [128, 8])
        w2 = B(st, "w2", [128, DR], BF16); a2w = B(st, "a2w", [128, DR], BF16)
        g2a = B(st, "g2a", [128, DR], BF16); g2b = B(st, "g2b", [32, DR], BF16)
        convw = B(st, "convw", [128, 8, KW]); convv = B(st, "convv", [128, 3, 8])
        st_y = ExitStack()
        yTr = B(st_y, "yTr", [128, 8, TOK], BF16)
        for (b_, d_) in ((cmask, din("cmask", [128, 8, 128])), (rst, din("rst", [128, 512])), (blk64, din("blk64", [128, 128])),
                         (ones, din("ones", [128, 128])), (ident2, din("ident2", [128, 64])), (mu, din("mu", [128, 2, NCC])),
                         (rv, din("rv", [128, 9, 8])), (convw, din("convw", [128, 8, KW])), (convv, din("convv", [128, 3, 8]))):
            LD("sp", b_.t[:], d_, "const", [b_], batch=True)
        for (b_, d_) in ((w2, din("w2", [128, DR])), (a2w, din("a2w", [128, DR])), (g2a, din("g2a", [128, DR])), (g2b, din("g2b", [32, DR]))):
            LD("pool", b_.t[:], d_, "constc", [b_], batch=True)
        V(lambda e: e.tensor_tensor(c0.t[:], mu.t[:, 0, :], mu.t[:, 1, :], ALU.add), [mu], [c0])
        V(lambda e: e.tensor_scalar(c0.t[:], c0.t[:], -1.0, 1.0, ALU.mult, ALU.add), [c0], [c0])
        V(lambda e: e.tensor_scalar(omka.t[:], rv.t[:, 5, :], -1.0, 1.0, ALU.mult, ALU.add), [rv], [omka])

        def load_shift(ph_, dst, dram, gc, rows, col0, n, lo_edge, hi_edge, raw, nrows=128, key="raw"):
            a = 0 if lo_edge else 1
            bnd = 0 if hi_edge else 1
            if lo_edge or hi_edge:
                P(lambda e: e.memset(raw.t[:nrows, 0:n + 2], 0.0), [], [raw])
            LD("sp", raw.t[:nrows, 1 - a:n + 1 + bnd], dram[rows:rows + nrows, col0 - a:col0 + n + bnd], key, [raw])
            V(lambda e: e.tensor_scalar(dst.t[:nrows, 0:n], raw.t[:nrows, 1:n + 1], c0.t[:nrows, gc:gc + 1], None, ALU.mult), [raw, c0], [dst])
            V(lambda e: e.scalar_tensor_tensor(dst.t[:nrows, 0:n], raw.t[:nrows, 0:n], mu.t[:nrows, 0, gc:gc + 1], dst.t[:nrows, 0:n], ALU.mult, ALU.add), [raw, mu, dst], [dst])
            V(lambda e: e.scalar_tensor_tensor(dst.t[:nrows, 0:n], raw.t[:nrows, 2:n + 2], mu.t[:nrows, 1, gc:gc + 1], dst.t[:nrows, 0:n], ALU.mult, ALU.add), [raw, mu, dst], [dst])

        def issue_load(raw, dram, rows, col0, n, lo_edge, hi_edge, key):
            a = 0 if lo_edge else 1
            bnd = 0 if hi_edge else 1
            if lo_edge or hi_edge:
                P(lambda e: e.memset(raw.t[:, 0:n + 2], 0.0), [], [raw])
            LD("sp", raw.t[:, 1 - a:n + 1 + bnd], dram[rows:rows + 128, col0 - a:col0 + n + bnd], key, [raw])

        def apply_shift(dst, raw, gc, n):
            V(lambda e: e.tensor_scalar(dst.t[:, 0:n], raw.t[:, 1:n + 1], c0.t[:, gc:gc + 1], None, ALU.mult), [raw, c0], [dst])
            V(lambda e: e.scalar_tensor_tensor(dst.t[:, 0:n], raw.t[:, 0:n], mu.t[:, 0, gc:gc + 1], dst.t[:, 0:n], ALU.mult, ALU.add), [raw, mu, dst], [dst])
            V(lambda e: e.scalar_tensor_tensor(dst.t[:, 0:n], raw.t[:, 2:n + 2], mu.t[:, 1, gc:gc + 1], dst.t[:, 0:n], ALU.mult, ALU.add), [raw, mu, dst], [dst])

        NB = SEQ // 512
        with ExitStack() as ph:
            TWD = nc.dram_tensor("TWD", [128, SEQ], BF16, kind="Internal").ap(); ADD = nc.dram_tensor("ADD", [128, SEQ], BF16, kind="Internal").ap()
            ado = B(ph, "ado", [128, TOK], BF16); sga = B(ph, "sga", [128, TOK], BF16); sgb = B(ph, "sgb", [32, TOK], BF16)
            YD = [nc.dram_tensor("YF", [128, SEQ], F32, kind="Internal").ap(), nc.dram_tensor("YB", [128, SEQ], F32, kind="Internal").ap()]
            with ExitStack() as p2:
                raw = B(p2, "raw0", [128, 1026]); tmp = B(p2, "tmp0", [128, 1024]); tb1 = B(p2, "tb1", [128, 1024], BF16); tb2 = B(p2, "tb2", [128, 1024], BF16)
                for blk in range(SEQ // 1024):
                    lo, hi = blk == 0, blk == SEQ // 1024 - 1
                    load_shift(p2, tmp, PT, 40, 24 * 128, blk * 1024, 1024, lo, hi, raw)
                    A(lambda e: e.activation(tb1.t[:], tmp.t[:], AF.Tanh), [tmp], [tb1])
                    S.dma("sp", TWD[:, blk * 1024:(blk + 1) * 1024], tb1.t[:], "twd", reads=[tb1.k])
                    load_shift(p2, tmp, PT, 41, 25 * 128, blk * 1024, 1024, lo, hi, raw)
                    A(lambda e: e.copy(tb2.t[:], tmp.t[:]), [tmp], [tb2])
                    S.dma("sp", ADD[:, blk * 1024:(blk + 1) * 1024], tb2.t[:], "add", reads=[tb2.k])
                load_shift(p2, tmp, PH, 41, 41 * 128, HALO, TOK, False, False, raw)
                A(lambda e: e.copy(ado.t[:], tmp.t[:]), [tmp], [ado])
                load_shift(p2, tmp, PH, 42, 42 * 128, HALO, TOK, False, False, raw)
                A(lambda e: e.activation(sga.t[:], tmp.t[:], AF.Sigmoid), [tmp], [sga])
                load_shift(p2, tmp, PH, 43, 43 * 128, HALO, TOK, False, False, raw, nrows=32)
                A(lambda e: e.activation(sgb.t[:], tmp.t[0:32, :], AF.Sigmoid), [tmp], [sgb])
                S.barrier(); S.emit()

            for pc in ([dbg_pc] if debug == 3 else range(8)):
                cs_ = slice(pc * 128, (pc + 1) * 128)
                with ExitStack() as p2:
                    WS_F32 = ("r", "k", "lw", "lr", "kk", "t1", "t2", "pref", "cum", "E", "bb", "kd")
                    WS_BF = ("At", "Rt", "Bt", "Kt", "Bh", "Kh", "Vb")
                    WS_TOK = ("Atok", "Bhtok", "Khtok", "Vtok", "Gtok", "AkVs", "U0tok")

                    def mk_ws(tag):
                        W = {"raws": [[B(p2, "raw%s%d%s" % (x_, s_, tag), [128, 514]) for x_ in "rkv"] for s_ in range(2)],
                             "tws": [B(p2, "twb%d%s" % (s_, tag), [128, 512], BF16) for s_ in range(2)],
                             "ads": [B(p2, "adb%d%s" % (s_, tag), [128, 512], BF16) for s_ in range(2)]}
                        for n_ in WS_F32:
                            W[n_] = B(p2, n_ + tag, [128, 512])
                        for n_ in WS_BF:
                            W[n_] = B(p2, n_ + tag, [128, 512], BF16)
                        for n_ in WS_TOK:
                            W[n_] = B(p2, n_ + tag, [128, 4, 128], BF16)
                        W["hb"] = [{n_: B(p2, "%s%d%s" % (n_, h, tag), [128, 4, 128], BF16) for n_ in
                                    ("AkT", "ArbT", "ArkT", "TT", "N", "NT", "Na", "NTa", "Nb", "NTb", "Tt")} for h in range(2)]
                        W["PhiT"] = B(p2, "PhiT" + tag, [128, 4, 64]); W["Zloc"] = B(p2, "Zloc" + tag, [128, 4, 64])
                        W["Z"] = B(p2, "Z" + tag, [128, 64]); W["WC"] = B(p2, "WC" + tag, [128, 4])
                        W["py"] = PB(p2, "py" + tag, [128, 512]); W["pz"] = PB(p2, "pz" + tag, [128, 64])
                        return W

                    WSP = [mk_ws("f"), mk_ws("b")]
                    pr = [PB(p2, "pr%d" % i, [128, 512]) for i in range(4)]
                    pring = Ring(pr, "pr")

                    def pnext():
                        k = pring.i % len(pr)
                        pring.i += 1
                        return pr[k]

                    v3 = lambda b_: b_.t[:].rearrange("p (c t) -> p c t", t=128)

                    def issue_block_loads(d, tb, W, s_):
                        sfx = "fb"[d]
                        lo, hi = tb == 0, tb == NB - 1
                        c0_ = tb * 512
                        ts_ = slice(c0_, c0_ + 512)
                        LD("act", W["tws"][s_].t[:], TWD[:, ts_], "twl%d%s" % (s_, sfx), [W["tws"][s_]])
                        LD("act", W["ads"][s_].t[:], ADD[:, ts_], "adl%d%s" % (s_, sfx), [W["ads"][s_]])
                        issue_load(W["raws"][s_][0], PT, pc * 128, c0_, 512, lo, hi, "rawr%d%s" % (s_, sfx))
                        issue_load(W["raws"][s_][1], PT, (8 + pc) * 128, c0_, 512, lo, hi, "rawk%d%s" % (s_, sfx))
                        issue_load(W["raws"][s_][2], PT, (16 + pc) * 128, c0_, 512, lo, hi, "rawv%d%s" % (s_, sfx))

                    def block_gen(d, tb, W, idx, nxt_tb):
                        s_ = idx % 2
                        if idx == 0:
                            issue_block_loads(d, tb, W, s_)
                        if nxt_tb is not None:
                            issue_block_loads(d, nxt_tb, W, 1 - s_)
                        tw, adf = W["tws"][s_], W["ads"][s_]
                        r_, k_, lw, lr, kk = W["r"], W["k"], W["lw"], W["lr"], W["kk"]
                        t1, t2, pref, cum, E, bb, kd = W["t1"], W["t2"], W["pref"], W["cum"], W["E"], W["bb"], W["kd"]
                        v_ = t2; QT = E; Y0s = t1
                        At, Rt, Bt, Kt, Bh, Kh, Vb = W["At"], W["Rt"], W["Bt"], W["Kt"], W["Bh"], W["Kh"], W["Vb"]
                        Atok, Bhtok, Khtok, Vtok, Gtok, AkVs, U0tok = [W[n_] for n_ in WS_TOK]
                        hb = W["hb"]; PhiT, Zloc, Z, WC, py, pz = W["PhiT"], W["Zloc"], W["Z"], W["WC"], W["py"], W["pz"]
                        hs_w = slice(d * 64, d * 64 + 64)
                        mN, mI, mNT = (0, 1, 2) if d == 0 else (2, 3, 0)
                        sfx = "fb"[d]
                        lo, hi = tb == 0, tb == NB - 1
                        c0_ = tb * 512
                        ts_ = slice(c0_, c0_ + 512)
                        apply_shift(r_, W["raws"][s_][0], 16 + pc, 512)
                        apply_shift(k_, W["raws"][s_][1], 24 + pc, 512)
                        apply_shift(v_, W["raws"][s_][2], 32 + pc, 512)
                        P(lambda e: e.tensor_copy(Vb.t[:], v_.t[:]), [v_], [Vb])
                        yield
                        pt = pnext()
                        M(lambda e, pt=pt, hs_w=hs_w: e.matmul(pt.t[:], w2.t[hs_w, cs_], tw.t[hs_w, :], start=True, stop=True), [w2, tw], [pt])
                        A(lambda e, pt=pt, d=d: e.activation(lw.t[:], pt.t[:], AF.Sigmoid, bias=rv.t[:, d, pc:pc + 1]), [pt, rv], [lw])
                        V(lambda e: e.tensor_scalar(lw.t[:], lw.t[:], -0.6065306597126334, None, ALU.mult), [lw], [lw])
                        pt = pnext()
                        M(lambda e, pt=pt, hs_w=hs_w: e.matmul(pt.t[:], a2w.t[hs_w, cs_], adf.t[hs_w, :], start=True, stop=True), [a2w, adf], [pt])
                        A(lambda e, pt=pt, d=d: e.activation(lr.t[:], pt.t[:], AF.Sigmoid, bias=rv.t[:, 2 + d, pc:pc + 1]), [pt, rv], [lr])
                        yield
                        V(lambda e: e.tensor_scalar(kk.t[:], k_.t[:], rv.t[:, 4, pc:pc + 1], None, ALU.mult), [k_, rv], [kk])
                        P(lambda e: e.tensor_tensor(t1.t[:], kk.t[:], kk.t[:], ALU.mult), [kk], [t1])
                        pt = pnext()
                        M(lambda e, pt=pt: e.matmul(pt.t[:], blk64.t[:], t1.t[:], start=True, stop=True), [blk64, t1], [pt])
                        A(lambda e, pt=pt: e.activation(t2.t[:], pt.t[:], AF.Sqrt), [pt], [t2])
                        V(lambda e: e.tensor_scalar(t2.t[:], t2.t[:], 1e-12, None, ALU.max), [t2], [t2])
                        V(lambda e: e.reciprocal(t2.t[:], t2.t[:]), [t2], [t2])
                        P(lambda e: e.tensor_tensor(kk.t[:], kk.t[:], t2.t[:], ALU.mult), [kk, t2], [kk])
                        yield
                        V(lambda e: e.tensor_scalar(t1.t[:], lr.t[:], rv.t[:, 5, pc:pc + 1], omka.t[:, pc:pc + 1], ALU.mult, ALU.add), [lr, rv, omka], [t1])
                        P(lambda e: e.tensor_tensor(kd.t[:], k_.t[:], t1.t[:], ALU.mult), [k_, t1], [kd])
                        P(lambda e: e.tensor_tensor(bb.t[:], kk.t[:], lr.t[:], ALU.mult), [kk, lr], [bb])
                        yield
                        V(lambda e: e.tensor_tensor_scan(pref.t[:], rst.t[:], lw.t[:], 0.0, ALU.mult, ALU.add), [rst, lw], [pref])
                        tot_bc = v3(pref)[:, :, 127:128].to_broadcast([128, 4, 128])
                        if d == 0:
                            P(lambda e: e.tensor_copy(cum.t[:], pref.t[:]), [pref], [cum])
                        else:
                            V(lambda e: e.tensor_tensor(cum.t[:], lw.t[:], pref.t[:], ALU.subtract), [lw, pref], [cum])
                            V(lambda e: e.tensor_tensor(v3(cum), v3(cum), tot_bc, ALU.add), [cum, pref], [cum])
                        A(lambda e: e.activation(WC.t[:], v3(pref)[:, :, 127], AF.Exp), [pref], [WC])
                        yield
                        A(lambda e: e.activation(E.t[:], cum.t[:], AF.Exp, scale=-1.0), [cum], [E])
                        V(lambda e: e.tensor_tensor(Bt.t[:], bb.t[:], E.t[:], ALU.mult), [bb, E], [Bt])
                        P(lambda e: e.tensor_tensor(Kt.t[:], kd.t[:], E.t[:], ALU.mult), [kd, E], [Kt])
                        yield
                        V(lambda e: e.tensor_tensor(v3(t1), tot_bc, v3(cum), ALU.subtract), [pref, cum], [t1])
                        A(lambda e: e.activation(E.t[:], t1.t[:], AF.Exp), [t1], [E])
                        V(lambda e: e.tensor_tensor(Bh.t[:], bb.t[:], E.t[:], ALU.mult), [bb, E], [Bh])
                        P(lambda e: e.tensor_tensor(Kh.t[:], kd.t[:], E.t[:], ALU.mult), [kd, E], [Kh])
                        yield
                        A(lambda e: e.activation(E.t[:], cum.t[:], AF.Exp), [cum], [E])
                        V(lambda e: e.tensor_tensor(Rt.t[:], r_.t[:], E.t[:], ALU.mult), [r_, E], [Rt])
                        V(lambda e: e.tensor_tensor(t1.t[:], cum.t[:], lw.t[:], ALU.subtract), [cum, lw], [t1])
                        A(lambda e: e.activation(E.t[:], t1.t[:], AF.Exp), [t1], [E])
                        V(lambda e: e.scalar_tensor_tensor(At.t[:], kk.t[:], -1.0, E.t[:], ALU.mult, ALU.mult), [kk, E], [At])
                        if debug == 3 and d == dbg_d and tb == dbg_tb:
                            for nm_, b__, dt_ in (("r", r_, F32), ("k", k_, F32), ("lw", lw, F32), ("lr", lr, F32), ("kk", kk, F32), ("kd", kd, F32), ("bb", bb, F32),
                                                  ("cum", cum, F32), ("pref", pref, F32), ("At", At, BF16), ("Rt", Rt, BF16), ("Bt", Bt, BF16), ("Kt", Kt, BF16),
                                                  ("Bh", Bh, BF16), ("Kh", Kh, BF16), ("Vb", Vb, BF16)):
                                dump_buf(nm_, b__, [128, 512], dt_)
                            dump_buf("WC", WC, [128, 4])
                        yield
                        for (src_, dst_) in ((At, Atok), (Bh, Bhtok), (Kh, Khtok), (Vb, Vtok)):
                            pt = pnext()
                            for c in range(4):
                                M(lambda e, pt=pt, src_=src_, c=c: e.matmul(pt.t[:, c * 128:(c + 1) * 128], src_.t[:, c * 128:(c + 1) * 128], identb[:], start=True, stop=True),
                                  [src_, Bidentb], [pt], sig=(c == 3))
                            A(lambda e, pt=pt, dst_=dst_: e.copy(dst_.t[:].rearrange("p c t -> p (c t)"), pt.t[:]), [pt], [dst_])
                        yield
                        for h in range(2):
                            hs = slice(h * 64, h * 64 + 64)
                            H = hb[h]
                            for (nm, lh, rh, mk) in (("N", Bt, At, mN), ("NT", At, Bt, mNT), ("AkT", Kt, At, mN), ("ArbT", Bt, Rt, mI), ("ArkT", Kt, Rt, mI)):
                                pt = pnext()
                                for c in range(4):
                                    M(lambda e, pt=pt, lh=lh, rh=rh, c=c, hs=hs: e.matmul(pt.t[:, c * 128:(c + 1) * 128], lh.t[hs, c * 128:(c + 1) * 128], rh.t[hs, c * 128:(c + 1) * 128], start=True, stop=True),
                                      [lh, rh], [pt], sig=(c == 3))
                                V(lambda e, pt=pt, nm=nm, mk=mk, H=H: e.tensor_tensor(H[nm].t[:], pt.t[:].rearrange("p (c t) -> p c t", t=128), cmask.t[:, mk:mk + 1, :].to_broadcast([128, 4, 128]), ALU.mult),
                                  [pt, cmask], [H[nm]])
                        yield
                        c4 = lambda b_: b_.t[:].rearrange("p c t -> p (c t)")
                        mk4 = lambda m_: cmask.t[:, m_:m_ + 1, :].to_broadcast([128, 4, 128])
                        idb = identb[:].rearrange("p (o t) -> p o t", o=1).to_broadcast([128, 4, 128])

                        def mm4(lhs, rhs):
                            pt_ = pnext()
                            for c in range(4):
                                M(lambda e, pt_=pt_, lhs=lhs, rhs=rhs, c=c: e.matmul(pt_.t[:, c * 128:(c + 1) * 128], lhs.t[:, c, :], rhs.t[:, c, :], start=True, stop=True),
                                  [lhs, rhs], [pt_], sig=(c == 3))
                            return pt_

                        def cp4(dst, pt_, eng):
                            if eng == "act":
                                A(lambda e, dst=dst, pt_=pt_: e.copy(c4(dst), pt_.t[:]), [pt_], [dst])
                            else:
                                V(lambda e, dst=dst, pt_=pt_: e.tensor_copy(c4(dst), pt_.t[:]), [pt_], [dst])

                        def acc4(dst, pt_):
                            V(lambda e, dst=dst, pt_=pt_: e.tensor_tensor(c4(dst), pt_.t[:], c4(dst), ALU.add), [pt_, dst], [dst])

                        def inv_gen(H):
                            TTb, Ttb = H["TT"], H["Tt"]
                            Nk, NTk = H["Na"], H["NTa"]
                            P(lambda e, Nk=Nk: e.tensor_tensor(Nk.t[:], H["N"].t[:], mk4(4), ALU.mult), [H["N"], cmask], [Nk])
                            P(lambda e, NTk=NTk: e.tensor_tensor(NTk.t[:], H["NT"].t[:], mk4(4), ALU.mult), [H["NT"], cmask], [NTk])
                            V(lambda e, Nk=Nk: e.tensor_tensor(TTb.t[:], Nk.t[:], idb, ALU.add), [Nk, Bidentb], [TTb])
                            V(lambda e, NTk=NTk: e.tensor_tensor(Ttb.t[:], NTk.t[:], idb, ALU.add), [NTk, Bidentb], [Ttb])
                            yield
                            for lev in range(3):
                                N2, NT2 = (H["Nb"], H["NTb"]) if lev % 2 == 0 else (H["Na"], H["NTa"])
                                p1 = mm4(Nk, NTk)
                                p2 = mm4(NTk, Nk)
                                yield
                                cp4(NT2, p1, "act"); cp4(N2, p2, "act")
                                yield
                                p3 = mm4(NT2, TTb); p4 = mm4(N2, Ttb)
                                yield
                                acc4(TTb, p3); acc4(Ttb, p4)
                                yield
                                Nk, NTk = N2, NT2
                            for mi, mk_ in enumerate((5, 6, 7)):
                                O_, Ot_, X_, Xt_ = H["Na"], H["NTa"], H["Nb"], H["NTb"]
                                last = (mi == 2)
                                P(lambda e, O_=O_, mk_=mk_: e.tensor_tensor(O_.t[:], H["N"].t[:], mk4(mk_), ALU.mult), [H["N"], cmask], [O_])
                                P(lambda e, Ot_=Ot_, mk_=mk_: e.tensor_tensor(Ot_.t[:], H["NT"].t[:], mk4(mk_), ALU.mult), [H["NT"], cmask], [Ot_])
                                yield
                                px = mm4(Ot_, TTb)
                                pxt = mm4(O_, Ttb) if not last else None
                                yield
                                cp4(X_, px, "act")
                                if not last:
                                    cp4(Xt_, pxt, "act")
                                yield
                                pa = mm4(Ttb, X_)
                                pb = mm4(TTb, Xt_) if not last else None
                                yield
                                acc4(TTb, pa)
                                if not last:
                                    acc4(Ttb, pb)
                                yield

                        gens = [inv_gen(hb[0]), inv_gen(hb[1])]
                        while gens:
                            for g_ in list(gens):
                                try:
                                    next(g_)
                                except StopIteration:
                                    gens.remove(g_)
                        if debug == 3 and d == dbg_d and tb == dbg_tb:
                            dump_buf("Atok", Atok, [128, 4, 128], BF16); dump_buf("Vtok", Vtok, [128, 4, 128], BF16)
                            for h in range(2):
                                for nm_ in ("AkT", "ArbT", "ArkT", "TT"):
                                    dump_buf("%s%d" % (nm_, h), hb[h][nm_], [128, 4, 128], BF16)
                            dump_buf("N1", hb[1]["N"], [128, 4, 128], BF16); dump_buf("NT1", hb[1]["NT"], [128, 4, 128], BF16)
                        yield
                        pt = pnext()
                        for h in range(2):
                            for c in range(4):
                                M(lambda e, pt=pt, h=h, c=c: e.matmul(pt.t[:, c * 128 + h * 64:c * 128 + h * 64 + 64], hb[h]["TT"].t[:, c, :], Atok.t[:, c, h * 64:h * 64 + 64], start=True, stop=True),
                                  [hb[h]["TT"], Atok], [pt], sig=(h == 1 and c == 3))
                        A(lambda e, pt=pt: e.copy(Gtok.t[:].rearrange("p c t -> p (c t)"), pt.t[:]), [pt], [Gtok])
                        yield
                        pt = pnext()
                        for h in range(2):
                            for c in range(4):
                                M(lambda e, pt=pt, h=h, c=c: e.matmul(pt.t[:, c * 128 + h * 64:c * 128 + h * 64 + 64], hb[h]["AkT"].t[:, c, :], Vtok.t[:, c, h * 64:h * 64 + 64], start=True, stop=True),
                                  [hb[h]["AkT"], Vtok], [pt], sig=(h == 1 and c == 3))
                        V(lambda e, pt=pt: e.tensor_copy(AkVs.t[:].rearrange("p c t -> p (c t)"), pt.t[:]), [pt], [AkVs])
                        yield
                        pt = pnext()
                        for h in range(2):
                            for c in range(4):
                                M(lambda e, pt=pt, h=h, c=c: e.matmul(pt.t[:, c * 128 + h * 64:c * 128 + h * 64 + 64], hb[h]["TT"].t[:, c, :], AkVs.t[:, c, h * 64:h * 64 + 64], start=True, stop=True),
                                  [hb[h]["TT"], AkVs], [pt], sig=(h == 1 and c == 3))
                        A(lambda e, pt=pt: e.copy(U0tok.t[:].rearrange("p c t -> p (c t)"), pt.t[:]), [pt], [U0tok])
                        yield
                        ptY = pnext()
                        for h in range(2):
                            hs = slice(h * 64, h * 64 + 64)
                            for c in range(4):
                                M(lambda e, h=h, c=c, hs=hs, ptY=ptY: e.matmul(ptY.t[hs, c * 128:(c + 1) * 128], U0tok.t[:, c, hs], hb[h]["ArbT"].t[:, c, :], start=True, stop=False), [U0tok, hb[h]["ArbT"]], [ptY], sig=False)
                                M(lambda e, h=h, c=c, hs=hs, ptY=ptY: e.matmul(ptY.t[hs, c * 128:(c + 1) * 128], Vtok.t[:, c, hs], hb[h]["ArkT"].t[:, c, :], start=False, stop=True), [Vtok, hb[h]["ArkT"]], [ptY], sig=(h == 1 and c == 3))
                        A(lambda e, ptY=ptY: e.copy(Y0s.t[:], ptY.t[:]), [ptY], [Y0s])
                        ptZ = pnext()
                        for h in range(2):
                            hs = slice(h * 64, h * 64 + 64)
                            for c in range(4):
                                M(lambda e, c=c, hs=hs, ptZ=ptZ: e.matmul(ptZ.t[hs, c * 64:(c + 1) * 64], Bhtok.t[:, c, hs], U0tok.t[:, c, hs], start=True, stop=False), [Bhtok, U0tok], [ptZ], sig=False)
                                M(lambda e, c=c, hs=hs, ptZ=ptZ: e.matmul(ptZ.t[hs, c * 64:(c + 1) * 64], Khtok.t[:, c, hs], Vtok.t[:, c, hs], start=False, stop=True), [Khtok, Vtok], [ptZ], sig=(h == 1 and c == 3))
                        V(lambda e, ptZ=ptZ: e.tensor_copy(Zloc.t[:].rearrange("p c j -> p (c j)"), ptZ.t[:, 0:256]), [ptZ], [Zloc])
                        ptP = pnext()
                        for h in range(2):
                            hs = slice(h * 64, h * 64 + 64)
                            for c in range(4):
                                M(lambda e, c=c, hs=hs, ptP=ptP: e.matmul(ptP.t[hs, c * 64:(c + 1) * 64], Gtok.t[:, c, hs], Bhtok.t[:, c, hs], start=True, stop=True), [Gtok, Bhtok], [ptP], sig=(h == 1 and c == 3))
                        for c in range(4):
                            V(lambda e, c=c, ptP=ptP: e.scalar_tensor_tensor(PhiT.t[:, c, :], ident2.t[:], WC.t[:, c:c + 1], ptP.t[:, c * 64:(c + 1) * 64], ALU.mult, ALU.add), [ident2, WC, ptP], [PhiT])
                        ptQ = pnext()
                        for h in range(2):
                            hs = slice(h * 64, h * 64 + 64)
                            for c in range(4):
                                M(lambda e, h=h, c=c, hs=hs, ptQ=ptQ: e.matmul(ptQ.t[hs, c * 128:(c + 1) * 128], Gtok.t[:, c, hs], hb[h]["ArbT"].t[:, c, :], start=True, stop=True), [Gtok, hb[h]["ArbT"]], [ptQ], sig=(h == 1 and c == 3))
                        V(lambda e, ptQ=ptQ: e.tensor_tensor(QT.t[:], ptQ.t[:], Rt.t[:], ALU.add), [ptQ, Rt], [QT])
                        if debug == 3 and d == dbg_d and tb == dbg_tb:
                            dump_buf("Gtok", Gtok, [128, 4, 128], BF16); dump_buf("U0tok", U0tok, [128, 4, 128], BF16)
                            dump_buf("Y0s", Y0s, [128, 512]); dump_buf("Zloc", Zloc, [128, 4, 64]); dump_buf("PhiT", PhiT, [128, 4, 64]); dump_buf("QT", QT, [128, 512])
                            dump_buf("Zin", Z, [128, 64])
                        yield
                        corder = range(4) if d == 0 else range(3, -1, -1)
                        for ci, c in enumerate(corder):
                            for h in range(2):
                                hs = slice(h * 64, h * 64 + 64)
                                M(lambda e, c=c, hs=hs: e.matmul(py.t[hs, c * 128:(c + 1) * 128], Z.t[hs, :], QT.t[hs, c * 128:(c + 1) * 128], start=True, stop=True), [Z, QT], [py], sig=False)
                                M(lambda e, c=c, hs=hs: e.matmul(pz.t[hs, :], PhiT.t[hs, c, :], Z.t[hs, :], start=True, stop=True), [PhiT, Z], [pz], sig=(h == 1))
                            V(lambda e, c=c: e.tensor_tensor(Z.t[:], pz.t[:], Zloc.t[:, c, :], ALU.add), [pz, Zloc], [Z])
                        V(lambda e: e.tensor_tensor(Y0s.t[:], py.t[:], Y0s.t[:], ALU.add), [py, Y0s], [Y0s])
                        S.dma("sp", YD[d][:, ts_], Y0s.t[:], "yst" + sfx, reads=[Y0s.k])
                        yield

                    for d in range(2):
                        P(lambda e, d=d: e.memset(WSP[d]["Z"].t[:], 0.0), [], [WSP[d]["Z"]])
                    ords = [list(range(NB)), list(range(NB - 1, -1, -1))]
                    seqs = [iter([block_gen(d_, tb, WSP[d_], i_, (ords[d_][i_ + 1] if i_ + 1 < NB else None)) for i_, tb in enumerate(ords[d_])]) for d_ in range(2)]
                    cur = [next(seqs[0]), next(seqs[1])]
                    while any(c_ is not None for c_ in cur):
                        for di in range(2):
                            if cur[di] is None:
                                continue
                            try:
                                next(cur[di])
                            except StopIteration:
                                cur[di] = next(seqs[di], None)
                    if debug == 3:
                        flush_dumps()

                    S.barrier(); S.emit()
                with ExitStack() as p2:
                    raw = B(p2, "rawo", [128, TOK + 2])
                    ro = B(p2, "ro", [128, TOK]); ko = B(p2, "ko", [128, TOK]); vo = B(p2, "vo", [128, TOK])
                    ys = B(p2, "ys", [128, TOK]); u1 = B(p2, "u1", [128, TOK]); u2 = B(p2, "u2", [128, TOK]); u3 = B(p2, "u3", [128, TOK])
                    po = [PB(p2, "po%d" % i, [128, 512]) for i in range(6)]
                    rawk_o = B(p2, "rawok", [128, TOK + 2]); rawv_o = B(p2, "rawov", [128, TOK + 2])
                    yl = [[B(p2, "yl%d_%d" % (q_, d_), [128, TOK]) for d_ in range(2)] for q_ in range(4)]
                    for q_ in range(4):
                        for d_ in range(2):
                            LD("sp" if d_ == 0 else "act", yl[q_][d_].t[:], YD[d_][:, q_ * TOK:(q_ + 1) * TOK], ("wa0", "wa1", "wa2", "wa3", "wa4", "wa5", "xt0", "xt1")[q_ * 2 + d_], [yl[q_][d_]])
                    load_shift(p2, ro, PH, 16 + pc, (16 + pc) * 128, HALO, TOK, False, False, raw, key="xt2")
                    load_shift(p2, ko, PH, 24 + pc, (24 + pc) * 128, HALO, TOK, False, False, rawk_o, key="xt3")
                    load_shift(p2, vo, PH, 32 + pc, (32 + pc) * 128, HALO, TOK, False, False, rawv_o, key="hm")
                    for q_ in range(4):
                        for d in range(2):
                            if q_ == 0 and d == 0:
                                V(lambda e, d=d, q_=q_: e.tensor_scalar(ys.t[:], yl[q_][d].t[:], qflag[:, q_:q_ + 1], None, ALU.mult), [yl[q_][d], Bqflag], [ys])
                            else:
                                V(lambda e, d=d, q_=q_: e.scalar_tensor_tensor(ys.t[:], yl[q_][d].t[:], qflag[:, q_:q_ + 1], ys.t[:], ALU.mult, ALU.add), [yl[q_][d], Bqflag, ys], [ys])
                    for half in range(2):
                        hsl = slice(half * 512, half * 512 + 512)
                        for d in range(2):
                            M(lambda e, d=d, hsl=hsl, half=half: e.matmul(po[d].t[:], a2w.t[d * 64:d * 64 + 64, cs_], ado.t[d * 64:d * 64 + 64, hsl], start=True, stop=True), [a2w, ado], [po[d]])
                        A(lambda e, hsl=hsl: e.activation(u1.t[:, hsl], po[0].t[:], AF.Sigmoid, bias=rv.t[:, 2, pc:pc + 1]), [po[0], rv], [u1])
                        A(lambda e, hsl=hsl: e.activation(u2.t[:, hsl], po[1].t[:], AF.Sigmoid, bias=rv.t[:, 3, pc:pc + 1]), [po[1], rv], [u2])
                    V(lambda e: e.tensor_tensor(u1.t[:], u1.t[:], u2.t[:], ALU.add), [u1, u2], [u1])
                    V(lambda e: e.tensor_scalar(u2.t[:], omka.t[:, pc:pc + 1].to_broadcast([128, TOK]), 2.0, None, ALU.mult), [omka], [u2])
                    V(lambda e: e.scalar_tensor_tensor(u1.t[:], u1.t[:], rv.t[:, 5, pc:pc + 1], u2.t[:], ALU.mult, ALU.add), [u1, rv, u2], [u1])
                    V(lambda e: e.tensor_tensor(u1.t[:], u1.t[:], ko.t[:], ALU.mult), [u1, ko], [u1])
                    V(lambda e: e.scalar_tensor_tensor(u1.t[:], u1.t[:], rv.t[:, 6, pc:pc + 1], ro.t[:], ALU.mult, ALU.mult), [u1, rv, ro], [u1])
                    A(lambda e: e.activation(u2.t[:], ys.t[:], AF.Square), [ys], [u2])
                    for half in range(2):
                        hsl = slice(half * 512, half * 512 + 512)
                        M(lambda e, hsl=hsl, half=half: e.matmul(po[0 + half].t[:], blk64.t[:], ys.t[:, hsl], start=True, stop=True), [blk64, ys], [po[0 + half]])
                        M(lambda e, hsl=hsl, half=half: e.matmul(po[2 + half].t[:], blk64.t[:], u2.t[:, hsl], start=True, stop=True), [blk64, u2], [po[2 + half]])
                        M(lambda e, hsl=hsl, half=half: e.matmul(po[4 + half].t[:], blk64.t[:], u1.t[:, hsl], start=True, stop=True), [blk64, u1], [po[4 + half]])
                    for half in range(2):
                        hsl = slice(half * 512, half * 512 + 512)
                        A(lambda e, hsl=hsl, half=half: e.mul(u3.t[:, hsl], po[0 + half].t[:], 1.0 / 64), [po[0 + half]], [u3])
                        V(lambda e, hsl=hsl, half=half: e.tensor_tensor(u1.t[:, hsl], po[4 + half].t[:], vo.t[:, hsl], ALU.mult), [po[4 + half], vo], [u1])
                        V(lambda e, hsl=hsl: e.tensor_tensor(ro.t[:, hsl], u3.t[:, hsl], u3.t[:, hsl], ALU.mult), [u3], [ro])
                        V(lambda e, hsl=hsl, half=half: e.scalar_tensor_tensor(u2.t[:, hsl], po[2 + half].t[:], 1.0 / 64, ro.t[:, hsl], ALU.mult, ALU.subtract), [po[2 + half], ro], [u2])
                    V(lambda e: e.tensor_scalar(u2.t[:], u2.t[:], GN_EPS, None, ALU.add), [u2], [u2])
                    A(lambda e: e.activation(u2.t[:], u2.t[:], AF.Sqrt), [u2], [u2])
                    V(lambda e: e.reciprocal(u2.t[:], u2.t[:]), [u2], [u2])
                    V(lambda e: e.tensor_tensor(ys.t[:], ys.t[:], u3.t[:], ALU.subtract), [ys, u3], [ys])
                    V(lambda e: e.tensor_tensor(ys.t[:], ys.t[:], u2.t[:], ALU.mult), [ys, u2], [ys])
                    V(lambda e: e.tensor_scalar(ys.t[:], ys.t[:], rv.t[:, 7, pc:pc + 1], rv.t[:, 8, pc:pc + 1], ALU.mult, ALU.add), [ys, rv], [ys])
                    V(lambda e: e.tensor_tensor(ys.t[:], ys.t[:], u1.t[:], ALU.add), [ys, u1], [ys])
                    for half in range(2):
                        hsl = slice(half * 512, half * 512 + 512)
                        M(lambda e, hsl=hsl, half=half: e.matmul(po[half].t[:], g2a.t[:, cs_], sga.t[:, hsl], start=True, stop=False), [g2a, sga], [po[half]], sig=False)
                        M(lambda e, hsl=hsl, half=half: e.matmul(po[half].t[:], g2b.t[0:32, cs_], sgb.t[0:32, hsl], start=False, stop=True), [g2b, sgb], [po[half]])
                        V(lambda e, hsl=hsl, half=half: e.tensor_tensor(yTr.t[:, pc, hsl], po[half].t[:], ys.t[:, hsl], ALU.mult), [po[half], ys], [yTr])
                    S.barrier(); S.emit()
        if stop <= 3:
            r_nc = _finish(nc, S, st, dram_in)
            st_y.close()
            return r_nc

        st_y2 = ExitStack()
        yTc = B(st_y2, "yTc", [128, 8, TOK], BF16)
        with ExitStack() as ph:
            W = TOK + KW - 1
            zc = B(ph, "zc", [128, 8, TOK]); val = B(ph, "val", [128, W]); gate = B(ph, "gate", [128, W]); z = B(ph, "z", [128, W])
            sq = B(ph, "csq", [128, TOK]); mean = B(ph, "cmean", [128, TOK]); rstd = B(ph, "crstd", [128, TOK]); tmpc = B(ph, "ctmp", [128, TOK])
            pS = [PB(ph, "pS%d" % i, [128, 512]) for i in range(2)]; pQ = [PB(ph, "pQ%d" % i, [128, 512]) for i in range(2)]
            c_lo = HALO - KW // 2
            for cc in range(8):
                LD("sp", val.t[:], PH[cc * 128:(cc + 1) * 128, c_lo:c_lo + W], "cv", [val])
                LD("act", gate.t[:], PH[(8 + cc) * 128:(9 + cc) * 128, c_lo:c_lo + W], "cg", [gate])
                A(lambda e: e.activation(gate.t[:], gate.t[:], AF.Sigmoid), [gate], [gate])
                V(lambda e: e.tensor_tensor(z.t[:], val.t[:], gate.t[:], ALU.mult), [val, gate], [z])
                V(lambda e, cc=cc: e.tensor_scalar(zc.t[:, cc, :], z.t[:, 0:TOK], convw.t[:, cc, 0:1], convv.t[:, 0, cc:cc + 1], ALU.mult, ALU.add), [z, convw, convv], [zc])
                for k in range(1, KW):
                    V(lambda e, cc=cc, k=k: e.scalar_tensor_tensor(zc.t[:, cc, :], z.t[:, k:k + TOK], convw.t[:, cc, k:k + 1], zc.t[:, cc, :], ALU.mult, ALU.add), [z, convw, zc], [zc])
                A(lambda e, cc=cc: e.activation(sq.t[:], zc.t[:, cc, :], AF.Square), [zc], [sq])
                for half in range(2):
                    hsl = slice(half * 512, half * 512 + 512)
                    M(lambda e, cc=cc, hsl=hsl, half=half: e.matmul(pS[half].t[:], ones.t[:], zc.t[:, cc, hsl], start=(cc == 0), stop=(cc == 7)), [ones, zc], [pS[half]])
                    M(lambda e, cc=cc, hsl=hsl, half=half: e.matmul(pQ[half].t[:], ones.t[:], sq.t[:, hsl], start=(cc == 0), stop=(cc == 7)), [ones, sq], [pQ[half]])
            for half in range(2):
                hsl = slice(half * 512, half * 512 + 512)
                A(lambda e, hsl=hsl, half=half: e.mul(mean.t[:, hsl], pS[half].t[:], 1.0 / DC), [pS[half]], [mean])
                V(lambda e, hsl=hsl: e.tensor_tensor(tmpc.t[:, hsl], mean.t[:, hsl], mean.t[:, hsl], ALU.mult), [mean], [tmpc])
                V(lambda e, hsl=hsl, half=half: e.scalar_tensor_tensor(rstd.t[:, hsl], pQ[half].t[:], 1.0 / DC, tmpc.t[:, hsl], ALU.mult, ALU.subtract), [pQ[half], tmpc], [rstd])
            V(lambda e: e.tensor_scalar(rstd.t[:], rstd.t[:], LN_EPS, None, ALU.add), [rstd], [rstd])
            A(lambda e: e.activation(rstd.t[:], rstd.t[:], AF.Sqrt), [rstd], [rstd])
            V(lambda e: e.reciprocal(rstd.t[:], rstd.t[:]), [rstd], [rstd])
            for cc in range(8):
                V(lambda e, cc=cc: e.tensor_tensor(tmpc.t[:], zc.t[:, cc, :], mean.t[:], ALU.subtract), [zc, mean], [tmpc])
                V(lambda e: e.tensor_tensor(tmpc.t[:], tmpc.t[:], rstd.t[:], ALU.mult), [tmpc, rstd], [tmpc])
                A(lambda e, cc=cc: e.activation(yTc.t[:, cc, :], tmpc.t[:], AF.Silu, bias=convv.t[:, 2, cc:cc + 1], scale=convv.t[:, 1, cc:cc + 1]), [tmpc, convv], [yTc])
            S.barrier(); S.emit()
        if stop <= 4:
            r_nc = _finish(nc, S, st, dram_in)
            st_y2.close(); st_y.close()
            return r_nc

        wout_d = din("w_out_r", [16, 128, 16, 128])
        st_x = ExitStack()
        xT = sb(st_x, "xT", [128, 16, TOK]); t_xT = Tok("xT")
        BxT = Buf(xT, t_xT)
        with ExitStack() as ph2:
            transpose_pass(ph2, "c", xh[HALO:HALO + TOK, :], TOK // 128, False, lambda fc, o, n: xT[:, fc, o:o + n], t_xT)
            S.barrier(); S.emit()
        with ExitStack() as ph:
            slabs = [B(ph, "wo%d" % i, [128, 16, 128], BF16) for i in range(3)]
            pp = [PB(ph, "ppo%d" % i, [128, 512]) for i in range(4)]
            pi = 0
            for oc in range(16):
                sl = slabs[oc % 3]
                LD("pool", sl.t[:], wout_d[oc], "w%d" % (oc % 3), [sl])
                for half in range(2):
                    hsl = slice(half * 512, half * 512 + 512)
                    pt = pp[pi % 4]; pi += 1
                    for kk in range(16):
                        M(lambda e, pt=pt, sl=sl, kk=kk, hsl=hsl: e.matmul(pt.t[:], sl.t[:, kk, :], (yTc.t[:, kk, hsl] if kk < 8 else yTr.t[:, kk - 8, hsl]), start=(kk == 0), stop=(kk == 15)), [sl, yTc, yTr], [pt], sig=(kk == 15))
                    V(lambda e, pt=pt, oc=oc, hsl=hsl: e.scalar_tensor_tensor(xT[:, oc, hsl], pt.t[:], modv[:, 32 + oc:33 + oc], xT[:, oc, hsl], ALU.mult, ALU.add), [pt, Bmodv, BxT], [BxT])
            S.barrier(); S.emit()
        if stop <= 5:
            r_nc = _finish(nc, S, st, dram_in, dbg_fn=lambda: S.dma("sp", dbg["xT"], xT[:], "dbg", reads=[t_xT], batch=True, is_out=True) if debug else None)
            st_x.close(); st_y2.close(); st_y.close()
            return r_nc

        wr_d = din("wr", [128, 16, 36]); br_d = din("br", [128, 36])
        wg_d = din("wg_r", [NE, 4, 128, 16, 128]); wu_d = din("wu_r", [NE, 4, 128, 16, 128]); wd_d = din("wd_r", [NE, 4, 128, 4, 512])
        WT = nc.dram_tensor("WT", [NE, TOK], F32, kind="Internal").ap()
        with ExitStack() as ph:
            hn2T = B(ph, "hn2T", [128, 16, TOK], BF16)
            with ExitStack() as p2:
                wr = B(p2, "wr", [128, 16, 36]); br = B(p2, "br", [128, 36])
                LD("sp", wr.t[:], wr_d, "const", [wr], batch=True); LD("sp", br.t[:], br_d, "const", [br], batch=True)
                sq = B(p2, "nsq", [128, TOK]); rs2 = B(p2, "rs2", [128, TOK]); hf = B(p2, "hf", [128, TOK])
                pS = [PB(p2, "nS%d" % i, [128, 512]) for i in range(2)]; pL = [PB(p2, "nL%d" % i, [128, 512]) for i in range(2)]
                pT = PB(p2, "nT", [128, 512]); pW = [PB(p2, "nW%d" % i, [128, 512]) for i in range(2)]
                for fc in range(16):
                    A(lambda e, fc=fc: e.activation(sq.t[:], xT[:, fc, :], AF.Square), [BxT], [sq])
                    for half in range(2):
                        hsl = slice(half * 512, half * 512 + 512)
                        M(lambda e, fc=fc, hsl=hsl, half=half: e.matmul(pS[half].t[:], ones.t[:], sq.t[:, hsl], start=(fc == 0), stop=(fc == 15)), [ones, sq], [pS[half]])
                for half in range(2):
                    hsl = slice(half * 512, half * 512 + 512)
                    V(lambda e, hsl=hsl, half=half: e.tensor_scalar(rs2.t[:, hsl], pS[half].t[:], 1.0 / D, RMS_EPS, ALU.mult, ALU.add), [pS[half]], [rs2])
                A(lambda e: e.activation(rs2.t[:], rs2.t[:], AF.Sqrt), [rs2], [rs2])
                V(lambda e: e.reciprocal(rs2.t[:], rs2.t[:]), [rs2], [rs2])
                for fc in range(16):
                    V(lambda e, fc=fc: e.tensor_tensor(hf.t[:], xT[:, fc, :], rs2.t[:], ALU.mult), [BxT, rs2], [hf])
                    V(lambda e, fc=fc: e.tensor_scalar(hf.t[:], hf.t[:], a2[:, fc:fc + 1], modv[:, 48 + fc:49 + fc], ALU.mult, ALU.add), [hf, Ba2, Bmodv], [hf])
                    A(lambda e, fc=fc: e.copy(hn2T.t[:, fc, :], hf.t[:]), [hf], [hn2T])
                    for half in range(2):
                        hsl = slice(half * 512, half * 512 + 512)
                        M(lambda e, fc=fc, hsl=hsl, half=half: e.matmul(pL[half].t[0:36, :], wr.t[:, fc, :], hf.t[:, hsl], start=(fc == 0), stop=(fc == 15)), [wr, hf], [pL[half]])
                LT = B(p2, "LT", [36, TOK]); L = B(p2, "L", [128, 8, 36])
                for half in range(2):
                    A(lambda e, half=half: e.copy(LT.t[:, half * 512:(half + 1) * 512], pL[half].t[0:36, :]), [pL[half]], [LT])
                for t_ in range(8):
                    M(lambda e, t_=t_: e.matmul(pT.t[:, t_ * 36:(t_ + 1) * 36], LT.t[:, t_ * 128:(t_ + 1) * 128], ident[0:36, 0:36], start=True, stop=True), [LT, Bident], [pT], sig=(t_ == 7))
                V(lambda e: e.tensor_tensor(L.t[:], pT.t[:, 0:288].rearrange("p (t c) -> p t c", c=36), br.t[:].rearrange("p (o c) -> p o c", o=1).to_broadcast([128, 8, 36]), ALU.add), [pT, br], [L])
                gl = L.t[:, :, 0:4]
                el = L.t[:, :, 4:36]
                gmax = B(p2, "gmax", [128, 8]); gmask = B(p2, "gmask", [128, 8, 4]); gex = B(p2, "gex", [128, 8, 4]); pg = B(p2, "pg", [128, 8])
                elm = B(p2, "elm", [128, 8, 32]); m1 = B(p2, "m1", [128, 8]); m2 = B(p2, "m2", [128, 8]); k1 = B(p2, "k1", [128, 8, 32]); k2 = B(p2, "k2", [128, 8, 32])
                w1 = B(p2, "w1", [128, 8]); w2_ = B(p2, "w2_", [128, 8]); wt = B(p2, "wt", [128, 8, 32])
                bc8 = lambda b_, n: b_.t[:].rearrange("p (t o) -> p t o", o=1).to_broadcast([128, 8, n])
                V(lambda e: e.tensor_reduce(gmax.t[:], gl, AX.X, ALU.max), [L], [gmax])
                V(lambda e: e.tensor_tensor(gmask.t[:], gl, bc8(gmax, 4), ALU.is_equal), [L, gmax], [gmask])
                V(lambda e: e.tensor_tensor(gex.t[:], gl, bc8(gmax, 4), ALU.subtract), [L, gmax], [gex])
                A(lambda e: e.activation(gex.t[:], gex.t[:], AF.Exp), [gex], [gex])
                V(lambda e: e.tensor_reduce(pg.t[:], gex.t[:], AX.X, ALU.add), [gex], [pg])
                V(lambda e: e.reciprocal(pg.t[:], pg.t[:]), [pg], [pg])
                V(lambda e: e.tensor_scalar(gmask.t[:], gmask.t[:], 1e30, -1e30, ALU.mult, ALU.add), [gmask], [gmask])
                V(lambda e: e.tensor_tensor(elm.t[:].rearrange("p t (g x) -> p t g x", x=8), el.rearrange("p t (g x) -> p t g x", x=8),
                                            gmask.t[:].rearrange("p t (g o) -> p t g o", o=1).to_broadcast([128, 8, 4, 8]), ALU.add), [L, gmask], [elm])
                V(lambda e: e.tensor_reduce(m1.t[:], elm.t[:], AX.X, ALU.max), [elm], [m1])
                V(lambda e: e.tensor_tensor(k1.t[:], elm.t[:], bc8(m1, 32), ALU.is_equal), [elm, m1], [k1])
                V(lambda e: e.scalar_tensor_tensor(elm.t[:], k1.t[:], -1e30, elm.t[:], ALU.mult, ALU.add), [k1, elm], [elm])
                V(lambda e: e.tensor_reduce(m2.t[:], elm.t[:], AX.X, ALU.max), [elm], [m2])
                V(lambda e: e.tensor_tensor(k2.t[:], elm.t[:], bc8(m2, 32), ALU.is_equal), [elm, m2], [k2])
                V(lambda e: e.tensor_tensor(w1.t[:], m1.t[:], m2.t[:], ALU.subtract), [m1, m2], [w1])
                A(lambda e: e.activation(w2_.t[:], w1.t[:], AF.Sigmoid, scale=-1.0), [w1], [w2_])
                A(lambda e: e.activation(w1.t[:], w1.t[:], AF.Sigmoid), [w1], [w1])
                V(lambda e: e.tensor_tensor(w1.t[:], w1.t[:], pg.t[:], ALU.mult), [w1, pg], [w1])
                V(lambda e: e.tensor_tensor(w2_.t[:], w2_.t[:], pg.t[:], ALU.mult), [w2_, pg], [w2_])
                V(lambda e: e.tensor_tensor(wt.t[:], k1.t[:], bc8(w1, 32), ALU.mult), [k1, w1], [wt])
                V(lambda e: e.tensor_tensor(k2.t[:], k2.t[:], bc8(w2_, 32), ALU.mult), [k2, w2_], [k2])
                V(lambda e: e.tensor_tensor(wt.t[:], wt.t[:], k2.t[:], ALU.add), [wt, k2], [wt])
                wtT = B(p2, "wtT", [32, TOK])
                for t_ in range(8):
                    M(lambda e, t_=t_: e.matmul(pW[t_ // 4].t[0:32, (t_ % 4) * 128:(t_ % 4 + 1) * 128], wt.t[:, t_, :], ident[:], start=True, stop=True), [wt, Bident], [pW[t_ // 4]], sig=(t_ % 4 == 3))
                for half in range(2):
                    A(lambda e, half=half: e.copy(wtT.t[:, half * 512:(half + 1) * 512], pW[half].t[0:32, :]), [pW[half]], [wtT])
                S.dma("sp", WT, wtT.t[:], "wt", reads=[wtT.k])
                S.barrier(); S.emit()
            with ExitStack() as p2:
                gu = [B(p2, "gu%d" % i, [128, 16, 128], BF16) for i in range(4)]
                gu = [Buf(g0.t[:], g0.k) for g0 in gu]
                for ysrc in (yTr, yTc):
                    for j4 in range(4):
                        gu.append(Buf(ysrc.t[:, 2 * j4:2 * j4 + 2, :].rearrange("p a (b c) -> p (a b) c", c=128)))
                NGU = len(gu)
                wdn = [B(p2, "wdn%d" % i, [128, 4, 512], BF16) for i in range(4)]
                act = B(p2, "act", [128, 4, TOK], BF16)
                wb = [B(p2, "wb%d" % i, [128, TOK]) for i in range(2)]
                sl_ = [B(p2, "sl%d" % i, [128, 512]) for i in range(2)]
                pG = [PB(p2, "pG%d" % i, [128, 512]) for i in range(2)]; pU = [PB(p2, "pU%d" % i, [128, 512]) for i in range(2)]
                pD = [PB(p2, "pD%d" % i, [128, 512]) for i in range(4)]
                gi = 0; di = 0; si = 0
                for ex in range(NE):
                    wbe = wb[ex % 2]
                    LD("sp", wbe.t[:], WT[ex:ex + 1, :].to_broadcast([128, TOK]), "wb%d" % (ex % 2), [wbe])
                    for dc in range(4):
                        g_ = gu[gi % NGU]; LD("pool", g_.t, wg_d[ex, dc], "gu%d" % (gi % NGU), [g_]); gi += 1
                        u_ = gu[gi % NGU]; LD("pool", u_.t, wu_d[ex, dc], "gu%d" % (gi % NGU), [u_]); gi += 1
                        for half in range(2):
                            hsl = slice(half * 512, half * 512 + 512)
                            for kk in range(16):
                                M(lambda e, g_=g_, kk=kk, hsl=hsl, half=half: e.matmul(pG[half].t[:], g_.t[:, kk, :], hn2T.t[:, kk, hsl], start=(kk == 0), stop=(kk == 15)), [g_, hn2T], [pG[half]], sig=(kk == 15))
                            for kk in range(16):
                                M(lambda e, u_=u_, kk=kk, hsl=hsl, half=half: e.matmul(pU[half].t[:], u_.t[:, kk, :], hn2T.t[:, kk, hsl], start=(kk == 0), stop=(kk == 15)), [u_, hn2T], [pU[half]], sig=(kk == 15))
                            s_ = sl_[si % 2]; si += 1
                            A(lambda e, s_=s_, half=half: e.activation(s_.t[:], pG[half].t[:], AF.Silu), [pG[half]], [s_])
                            P(lambda e, s_=s_, wbe=wbe, hsl=hsl: e.tensor_tensor(s_.t[:], s_.t[:], wbe.t[:, hsl], ALU.mult), [s_, wbe], [s_])
                            V(lambda e, s_=s_, dc=dc, hsl=hsl, half=half: e.tensor_tensor(act.t[:, dc, hsl], pU[half].t[:], s_.t[:], ALU.mult), [pU[half], s_], [act])
                    for og in range(4):
                        wd_ = wdn[di % 4]; LD("pool", wd_.t[:], wd_d[ex, og], "wd%d" % (di % 4), [wd_]); di += 1
                        for o4 in range(4):
                            oc = og * 4 + o4
                            for half in range(2):
                                hsl = slice(half * 512, half * 512 + 512)
                                pt = pD[(o4 * 2 + half) % 4]
                                for kk in range(4):
                                    M(lambda e, pt=pt, wd_=wd_, kk=kk, o4=o4, hsl=hsl: e.matmul(pt.t[:], wd_.t[:, kk, o4 * 128:(o4 + 1) * 128], act.t[:, kk, hsl], start=(kk == 0), stop=(kk == 3)), [wd_, act], [pt], sig=(kk == 3))
                                V(lambda e, pt=pt, oc=oc, hsl=hsl: e.scalar_tensor_tensor(xT[:, oc, hsl], pt.t[:], modv[:, 80 + oc:81 + oc], xT[:, oc, hsl], ALU.mult, ALU.add), [pt, Bmodv, BxT], [BxT])
                S.barrier(); S.emit()
        if stop <= 6:
            r_nc = _finish(nc, S, st, dram_in, dbg_fn=lambda: S.dma("sp", dbg["xT"], xT[:], "dbg", reads=[t_xT], batch=True, is_out=True) if debug else None)
            st_x.close(); st_y2.close(); st_y.close()
            return r_nc

        with ExitStack() as ph:
            dgf = B(ph, "dgf", [128, 16, 128]); sqs = [B(ph, "fsq%d" % i, [128, 128]) for i in range(2)]
            rt = B(ph, "frt", [128, 8]); ot = [B(ph, "ot%d" % i, [128, D]) for i in range(2)]
            pss = PB(ph, "pss", [128, 8]); pf = [PB(ph, "pf%d" % i, [128, 512]) for i in range(4)]
            for fc in range(16):
                V(lambda e, fc=fc: e.tensor_scalar(dgf.t[:, fc, :], ident[:], gnv[:, 2, fc:fc + 1], None, ALU.mult), [Bident, Bgnv], [dgf])
            qi = 0
            for t_ in range(8):
                for fc in range(16):
                    q_ = sqs[qi % 2]; qi += 1
                    A(lambda e, q_=q_, fc=fc, t_=t_: e.activation(q_.t[:], xT[:, fc, t_ * 128:(t_ + 1) * 128], AF.Square), [BxT], [q_])
                    M(lambda e, q_=q_, fc=fc, t_=t_: e.matmul(pss.t[:, t_:t_ + 1], q_.t[:], ones.t[:, 0:1], start=(fc == 0), stop=(fc == 15)), [q_, ones], [pss])
            V(lambda e: e.tensor_scalar(rt.t[:], pss.t[:], 1.0 / D, RMS_EPS, ALU.mult, ALU.add), [pss], [rt])
            A(lambda e: e.activation(rt.t[:], rt.t[:], AF.Sqrt), [rt], [rt])
            V(lambda e: e.reciprocal(rt.t[:], rt.t[:]), [rt], [rt])
            pi = 0
            for t_ in range(8):
                o_ = ot[t_ % 2]
                for fg in range(4):
                    pt = pf[pi % 4]; pi += 1
                    for j4 in range(4):
                        fc = fg * 4 + j4
                        M(lambda e, pt=pt, fc=fc, j4=j4, t_=t_: e.matmul(pt.t[:, j4 * 128:(j4 + 1) * 128], xT[:, fc, t_ * 128:(t_ + 1) * 128], dgf.t[:, fc, :], start=True, stop=True), [BxT, dgf], [pt], sig=(j4 == 3))
                    if fg % 2 == 0:
                        V(lambda e, pt=pt, o_=o_, fg=fg, t_=t_: e.tensor_scalar(o_.t[:, fg * 512:(fg + 1) * 512], pt.t[:], rt.t[:, t_:t_ + 1], None, ALU.mult), [pt, rt], [o_])
                    else:
                        A(lambda e, pt=pt, o_=o_, fg=fg, t_=t_: e.mul(o_.t[:, fg * 512:(fg + 1) * 512], pt.t[:], rt.t[:, t_:t_ + 1]), [pt, rt], [o_])
                S.dma("sp", out_d[t_ * 128:(t_ + 1) * 128, :], o_.t[:], "out%d" % (t_ % 2), reads=[o_.k], is_out=True)
            r_nc = _finish(nc, S, st, dram_in)
        st_x.close(); st_y2.close(); st_y.close()
        return r_nc


_IN_NAMES = []


def _finish(nc, S, st, dram_in=None, dbg_fn=None):
    _IN_NAMES[:] = list(dram_in.keys())
    if dbg_fn is not None:
        dbg_fn()
    S.barrier()
    S.emit(final=True)
    return nc


def _colvec(v, n):
    return np.ascontiguousarray(np.asarray(v, np.float32).reshape(n, 128).T)


def _kslab(w, nchunk):
    K, nc_ = w.shape
    pad = nchunk * 128 - nc_
    if pad:
        w = np.concatenate([w, np.zeros((K, pad), np.float32)], axis=1)
    return np.ascontiguousarray(w.reshape(K // 128, 128, nchunk, 128).transpose(2, 1, 0, 3))


def prep_shared(inp):
    f = lambda k: np.asarray(inp[k], np.float32)
    sh = {}
    sh["w_ada_r"] = np.ascontiguousarray(f("w_ada")[0].reshape(16, 128, 24, 512).transpose(2, 1, 0, 3))
    sh["b_ada_r"] = _colvec(f("b_ada")[0], 96)
    g = np.stack([f("norm1_g")[0], f("norm2_g")[0], f("normf_g")])
    sh["gnorm"] = np.ascontiguousarray(g.reshape(3, 16, 128).transpose(2, 0, 1))
    sh["w_in_r"] = _kslab(f("w_in")[0], NCC)
    sh["ident"] = np.eye(128, dtype=np.float32)
    idx = np.arange(128)
    bm = lambda L: (idx[:, None] // L) == (idx[None, :] // L)
    cm = np.stack([idx[:, None] < idx[None, :], idx[:, None] <= idx[None, :], idx[:, None] > idx[None, :], idx[:, None] >= idx[None, :],
                   bm(16), bm(32) & ~bm(16), bm(64) & ~bm(32), ~bm(64)], axis=1)
    sh["cmask"] = np.ascontiguousarray(cm.astype(np.float32))
    rst = np.ones((128, 512), np.float32); rst[:, ::128] = 0.0
    sh["rst"] = rst
    sh["blk64"] = np.kron(np.eye(2, dtype=np.float32), np.ones((64, 64), np.float32))
    sh["ones"] = np.ones((128, 128), np.float32)
    sh["ident2"] = np.ascontiguousarray(np.concatenate([np.eye(64, dtype=np.float32)] * 2, axis=0))
    mu = np.zeros((2, NCC * 128), np.float32)
    mu[0, 2048:2048 + 3488] = f("mu_prev")[0]
    mu[1, 2048:2048 + 3488] = f("mu_next")[0]
    sh["mu"] = np.ascontiguousarray(mu.reshape(2, NCC, 128).transpose(2, 0, 1))
    rvn = ["w0_f", "w0_b", "a0_f", "a0_b", "k_k", "k_a", "r_k", "lnx_g", "lnx_b"]
    sh["rv"] = np.ascontiguousarray(np.stack([f(k)[0].reshape(8, 128).T for k in rvn], axis=1))
    sh["w2"] = np.ascontiguousarray(np.concatenate([f("w2_f")[0], f("w2_b")[0]], axis=0))
    sh["a2w"] = np.ascontiguousarray(np.concatenate([f("a2_f")[0], f("a2_b")[0]], axis=0))
    sh["g2a"] = np.ascontiguousarray(f("g2")[0][:128])
    sh["g2b"] = np.ascontiguousarray(f("g2")[0][128:160])
    cw = f("conv_w")[0][:, 0, :]
    sh["convw"] = np.ascontiguousarray(cw.reshape(KW, 8, 128).transpose(2, 1, 0))
    sh["convv"] = np.ascontiguousarray(np.stack([f(k)[0].reshape(8, 128).T for k in ("conv_b", "conv_ln_g", "conv_ln_b")], axis=1))
    sh["w_out_r"] = _kslab(f("w_out")[0], 16)
    wr = np.concatenate([f("w_rg")[0], f("w_re")[0]], axis=1)
    sh["wr"] = np.ascontiguousarray(wr.reshape(16, 128, 36).transpose(1, 0, 2))
    br = np.concatenate([f("b_rg")[0], f("b_re")[0]])
    sh["br"] = np.ascontiguousarray(np.broadcast_to(br[None, :], (128, 36)))
    wg, wu, wd = f("w_gate")[0], f("w_up")[0], f("w_down")[0]
    sh["wg_r"] = np.ascontiguousarray(wg.reshape(NE, 16, 128, 4, 128).transpose(0, 3, 2, 1, 4))
    sh["wu_r"] = np.ascontiguousarray(wu.reshape(NE, 16, 128, 4, 128).transpose(0, 3, 2, 1, 4))
    sh["wd_r"] = np.ascontiguousarray(wd.reshape(NE, 4, 128, 4, 512).transpose(0, 3, 2, 1, 4))
    return sh


def prep_core(inp, i):
    b, q = i // 4, i % 4
    x = np.asarray(inp["x"], np.float32)
    m = {}
    m["xf"] = np.ascontiguousarray(x[b])
    lo = q * TOK - HALO
    xh = np.zeros((TH, D), np.float32)
    hm = np.zeros((TH,), np.float32)
    s0, s1 = max(lo, 0), min(lo + TH, SEQ)
    xh[s0 - lo:s1 - lo] = x[b, s0:s1]
    hm[s0 - lo:s1 - lo] = 1.0
    m["xh"] = xh
    m["hmask"] = np.ascontiguousarray(np.broadcast_to(hm[None, :], (128, TH)))
    qf = np.zeros((128, 4), np.float32)
    qf[:, q] = 1.0
    m["qflag"] = qf
    m["c_col"] = _colvec(np.asarray(inp["c"], np.float32)[b], 16)
    return m


_NC_CACHE = {}


def kernel(**inputs):
    if "nc" not in _NC_CACHE:
        _NC_CACHE["nc"] = build()
    nc = _NC_CACHE["nc"]
    sh = prep_shared(inputs)
    in_maps = []
    for i in range(NCORES):
        m = dict(sh)
        m.update(prep_core(inputs, i))
        in_maps.append({k: m[k] for k in _IN_NAMES})
    res = run_bass_kernel_spmd(nc, in_maps, core_ids=list(range(NCORES)))
    out = np.zeros((2, SEQ, D), np.float32)
    for i in range(NCORES):
        b, q = i // 4, i % 4
        out[b, q * TOK:(q + 1) * TOK] = res.results[i]["out"]
    return out
```

```python
import numpy as np
from contextlib import ExitStack
import concourse.bass as bass
import concourse.mybir as mybir
from concourse.bass_utils import run_bass_kernel_spmd

F32 = mybir.dt.float32
BF16 = mybir.dt.bfloat16
AF = mybir.ActivationFunctionType
ALU = mybir.AluOpType
AX = mybir.AxisListType

NCORES = 8
D = 2048
SEQ = 4096
TOK = 1024
HALO = 64
TH = TOK + 2 * HALO
DC = 1024
DR = 1024
NCOL = 5536
NCC = 44
NRC = 28
KW = 31
NE = 32
DE = 512
CH = 128
RMS_EPS = 1e-6
LN_EPS = 1e-5
GN_EPS = 64e-5


class Ev:
    __slots__ = ("sem", "val")

    def __init__(self, sem, val=None):
        self.sem = sem
        self.val = val


class Tok:
    __slots__ = ("w", "r", "name")

    def __init__(self, name=""):
        self.w = None
        self.r = []
        self.name = name


class DSem:
    def __init__(self, sem, batch):
        self.sem = sem
        self.total = 0
        self.batch = batch
        self.ev = Ev(sem, 0) if batch else None


class Sched:
    ENG = ("pe", "act", "dve", "pool", "sp")

    def __init__(self, nc, stack, n_dma_sems=72):
        self.nc = nc
        self.sem = {e: stack.enter_context(nc.semaphore("s_" + e)) for e in self.ENG}
        self.count = {e: 0 for e in self.ENG}
        self.ops = {e: [] for e in self.ENG}
        self.pending = {e: [] for e in self.ENG}
        self.free_dsems = [stack.enter_context(nc.semaphore("d%d" % i)) for i in range(n_dma_sems)]
        self.dsems = {}
        self.out_evs = []
        self.waited = {e: {} for e in self.ENG}
        self.nops = 0

    def dsem(self, key, batch=False):
        if key not in self.dsems:
            self.dsems[key] = DSem(self.free_dsems.pop(), batch)
        return self.dsems[key]

    def _collect(self, reads, writes):
        waits = []
        for t in reads:
            if t.w is not None:
                waits.append(t.w)
        for t in writes:
            if t.w is not None:
                waits.append(t.w)
            waits.extend(t.r)
        return waits

    def op(self, eng, fn, reads=(), writes=(), signal=True):
        waits = self._collect(reads, writes)
        ev = Ev(self.sem[eng])
        if signal:
            self.count[eng] += 1
            ev.val = self.count[eng]
            for p in self.pending[eng]:
                p.val = ev.val
            self.pending[eng] = []
        else:
            self.pending[eng].append(ev)
        self.ops[eng].append((waits, fn, (self.sem[eng], 1) if signal else None))
        for t in reads:
            if len(t.r) > 6:
                t.r = t.r[-6:] + [e for e in t.r[:-6] if e.val is None]
            t.r.append(ev)
        for t in writes:
            t.w = ev
            t.r = []
        self.nops += 1
        return ev

    def dma(self, q, out, in_, key, reads=(), writes=(), batch=False, is_out=False, **kw):
        ds = self.dsem(key, batch)
        waits = self._collect(reads, writes)
        ds.total += 16
        if ds.batch:
            ev = ds.ev
            ev.val = ds.total
        else:
            ev = Ev(ds.sem, ds.total)

        def fn(e, out=out, in_=in_, kw=kw):
            return e.dma_start(out=out, in_=in_, **kw)

        self.ops[q].append((waits, fn, (ds.sem, 16)))
        for t in reads:
            t.r.append(ev)
        for t in writes:
            t.w = ev
            t.r = []
        if is_out:
            self.out_evs.append(ev)
        self.nops += 1
        return ev

    def barrier(self):
        evs = []
        for e in self.ENG:
            assert not self.pending[e], "engine %s has non-signalled tail" % e
            if self.count[e]:
                evs.append(Ev(self.sem[e], self.count[e]))
        for ds in self.dsems.values():
            if ds.total:
                evs.append(Ev(ds.sem, ds.total))
        for e in self.ENG:
            self.ops[e].append((list(evs), None, None))

    def emit(self, final=False):
        nc = self.nc
        if final:
            self.ops["sp"].append((list(self.out_evs), None, None))
        for e in self.ENG:
            assert not self.pending[e], "engine %s ends with non-signaling op" % e
        with nc.Block() as block:
            def mk(ename):
                def body(eng):
                    waited = self.waited[ename]
                    for waits, fn, inc in self.ops[ename]:
                        need = {}
                        for ev in waits:
                            assert ev.val is not None
                            if ename == "pe" and ev.sem is self.sem["pe"]:
                                continue
                            k = id(ev.sem)
                            if need.get(k, (None, 0))[1] < ev.val:
                                need[k] = (ev.sem, ev.val)
                        for k, (s, v) in need.items():
                            if waited.get(k, 0) >= v:
                                continue
                            waited[k] = v
                            eng.wait_ge(s, v)
                        if fn is not None:
                            ins = fn(eng)
                            if inc is not None:
                                ins.then_inc(inc[0], inc[1])
                    self.ops[ename] = []
                return body
            block.tensor(mk("pe"))
            block.scalar(mk("act"))
            block.vector(mk("dve"))
            block.gpsimd(mk("pool"))
            block.sync(mk("sp"))


class Ring:
    def __init__(self, aps, name):
        self.aps = aps
        self.toks = [Tok("%s%d" % (name, i)) for i in range(len(aps))]
        self.i = 0

    def next(self):
        k = self.i % len(self.aps)
        self.i += 1
        return self.aps[k], self.toks[k], k


def build(stop=99, debug=False, dbg_pc=1, dbg_d=0, dbg_tb=0):
    nc = bass.Bass("TRN2", target_bir_lowering=False)
    dram_in = {}

    def din(name, shape, dt=F32):
        dram_in[name] = nc.dram_tensor(name, list(shape), dt, kind="ExternalInput").ap()
        return dram_in[name]

    xf = din("xf", [SEQ, D])
    xh = din("xh", [TH, D])
    hmask_d = din("hmask", [128, TH])
    qflag_d = din("qflag", [128, 4])
    ccol_d = din("c_col", [128, 16])
    wada_d = din("w_ada_r", [96, 128, 16, 128])
    bada_d = din("b_ada_r", [128, 96])
    gn_d = din("gnorm", [128, 3, 16])
    win_d = din("w_in_r", [NCC, 128, 16, 128])
    ident_d = din("ident", [128, 128])
    out_d = nc.dram_tensor("out", [TOK, D], F32, kind="ExternalOutput").ap()

    PT = nc.dram_tensor("PT", [NRC * 128, SEQ], F32, kind="ExternalOutput" if debug == 2 else "Internal").ap()
    PH = nc.dram_tensor("PH", [NCC * 128, TH], F32, kind="ExternalOutput" if debug == 2 else "Internal").ap()
    dbg = {}
    if debug:
        dbg["modv"] = nc.dram_tensor("dbg_modv", [128, 96], F32, kind="ExternalOutput").ap()
        dbg["xT"] = nc.dram_tensor("dbg_xT", [128, 16, TOK], F32, kind="ExternalOutput").ap()
    dbg_y = nc.dram_tensor("dbg_y", [128, 16, TOK], BF16, kind="ExternalOutput").ap() if debug else None

    with ExitStack() as st:
        S = Sched(nc, st)

        uid = [0]

        def sb(stack, name, shape, dt=F32):
            uid[0] += 1
            return stack.enter_context(nc.sbuf_tensor("sb%d_%s" % (uid[0], name), list(shape), dt))

        def ps(stack, name, shape, dt=F32):
            uid[0] += 1
            return stack.enter_context(nc.psum_tensor("ps%d_%s" % (uid[0], name), list(shape), dt))

        ident = sb(st, "ident", [128, 128]); t_ident = Tok("ident")
        identb = sb(st, "identb", [128, 128], BF16); t_identb = Tok("identb")
        modv = sb(st, "modv", [128, 96]); t_modv = Tok("modv")
        gnv = sb(st, "gnv", [128, 3, 16]); t_gnv = Tok("gnv")
        a1 = sb(st, "a1", [128, 16]); t_a1 = Tok("a1")
        a2 = sb(st, "a2", [128, 16]); t_a2 = Tok("a2")
        qflag = sb(st, "qflag", [128, 4]); t_qflag = Tok("qflag")

        stage_tbl = {}
        if debug == 3:
            for nm_ in ("r", "k", "lw", "lr", "kk", "kd", "bb", "cum", "pref", "Y0s", "QT"):
                stage_tbl[nm_] = ([128, 512], F32)
            for nm_ in ("At", "Rt", "Bt", "Kt", "Bh", "Kh", "Vb"):
                stage_tbl[nm_] = ([128, 512], BF16)
            for nm_ in ("Atok", "Vtok", "AkT0", "ArbT0", "ArkT0", "TT0", "AkT1", "ArbT1", "ArkT1", "TT1", "N1", "NT1", "Gtok", "U0tok"):
                stage_tbl[nm_] = ([128, 4, 128], BF16)
            stage_tbl["WC"] = ([128, 4], F32); stage_tbl["Zloc"] = ([128, 4, 64], F32); stage_tbl["PhiT"] = ([128, 4, 64], F32); stage_tbl["Zin"] = ([128, 64], F32)
        stage_slots = {k_: (sb(st, "stage_" + k_, v_[0], v_[1]), Tok()) for k_, v_ in stage_tbl.items()}
        S.dma("sp", ident[:], ident_d, "const", writes=[t_ident], batch=True)
        S.dma("pool", identb[:], ident_d, "constc", writes=[t_identb], batch=True)
        S.dma("sp", gnv[:], gn_d, "const", writes=[t_gnv], batch=True)
        S.dma("sp", qflag[:], qflag_d, "const", writes=[t_qflag], batch=True)

        with ExitStack() as ph:
            ccol = sb(ph, "ccol", [128, 16]); t_ccol = Tok("ccol")
            cs = sb(ph, "cs", [128, 16]); t_cs = Tok("cs")
            bada = sb(ph, "bada", [128, 96]); t_bada = Tok("bada")
            slabs = [sb(ph, "wa%d" % i, [128, 16, 128]) for i in range(6)]
            ring = Ring(slabs, "wa")
            pmod = ps(ph, "pmod", [128, 96]); t_pmod = Tok("pmod")
            S.dma("sp", ccol[:], ccol_d, "const", writes=[t_ccol], batch=True)
            S.dma("sp", bada[:], bada_d, "const", writes=[t_bada], batch=True)
            S.op("act", lambda e: e.activation(cs[:], ccol[:], AF.Silu), reads=[t_ccol], writes=[t_cs])
            for cc in range(96):
                slab, tk, k = ring.next()
                S.dma("sp" if cc % 2 == 0 else "act", slab[:], wada_d[cc], "wa%d" % k, writes=[tk])
                for kk in range(16):
                    S.op("pe", lambda e, slab=slab, kk=kk, cc=cc: e.matmul(
                        pmod[:, cc:cc + 1], slab[:, kk, :], cs[:, kk:kk + 1], start=(kk == 0), stop=(kk == 15)),
                        reads=[tk, t_cs], writes=[t_pmod], signal=(kk == 15))
            S.op("dve", lambda e: e.tensor_tensor(modv[:], pmod[:], bada[:], ALU.add),
                 reads=[t_pmod, t_bada], writes=[t_modv])
            S.op("dve", lambda e: e.scalar_tensor_tensor(a1[:], modv[:, 16:32], 1.0, gnv[:, 0, :], ALU.add, ALU.mult),
                 reads=[t_modv, t_gnv], writes=[t_a1])
            S.op("dve", lambda e: e.scalar_tensor_tensor(a2[:], modv[:, 64:80], 1.0, gnv[:, 1, :], ALU.add, ALU.mult),
                 reads=[t_modv, t_gnv], writes=[t_a2])
            if debug:
                S.dma("sp", dbg["modv"], modv[:], "dbg", reads=[t_modv], batch=True, is_out=True)
            S.barrier()
            S.emit()
        if stop <= 0:
            return _finish(nc, S, st, dram_in)

        def transpose_pass(ph, tag, src, ntile, with_norm, dst_fn, t_dst):
            xts = [sb(ph, tag + "xt%d" % i, [128, D]) for i in range(4)]
            xring = Ring(xts, tag + "xt")
            sq = sb(ph, tag + "sq", [128, D]); t_sq = Tok("sq")
            ss = [sb(ph, tag + "ss%d" % i, [128, 1]) for i in range(4)]
            dg = [sb(ph, tag + "dg%d" % i, [128, 128]) for i in range(4)]
            t_ss = [Tok() for _ in range(4)]; t_dg = [Tok() for _ in range(4)]
            ptr = [ps(ph, tag + "ptr%d" % i, [128, 256]) for i in range(4)]
            pring = Ring(ptr, tag + "ptr")
            ev_i = 0
            g = 0
            ti = 0
            while ti < ntile:
                n_in = min(2, ntile - ti)
                tiles = []
                for j in range(n_in):
                    xt, tx, k = xring.next()
                    S.dma("sp" if (ti + j) % 2 == 0 else "act", xt[:], src[(ti + j) * 128:(ti + j + 1) * 128, :],
                          "xt%d" % k, writes=[tx])
                    if with_norm:
                        S.op("act", lambda e, xt=xt, k=k: e.activation(sq[:], xt[:], AF.Square, accum_out=ss[k][:]),
                             reads=[tx], writes=[t_sq, t_ss[k]])
                        S.op("dve", lambda e, k=k: e.tensor_scalar(ss[k][:], ss[k][:], 1.0 / D, RMS_EPS, ALU.mult, ALU.add),
                             reads=[t_ss[k]], writes=[t_ss[k]])
                        S.op("act", lambda e, k=k: e.activation(ss[k][:], ss[k][:], AF.Sqrt),
                             reads=[t_ss[k]], writes=[t_ss[k]])
                        S.op("dve", lambda e, k=k: e.reciprocal(ss[k][:], ss[k][:]),
                             reads=[t_ss[k]], writes=[t_ss[k]])
                        S.op("dve", lambda e, k=k: e.tensor_scalar(dg[k][:], ident[:], ss[k][:], None, ALU.mult),
                             reads=[t_ss[k], t_ident], writes=[t_dg[k]])
                    tiles.append((xt, tx, k))
                for fc in range(16):
                    pt, tp, _ = pring.next()
                    for j, (xt, tx, k) in enumerate(tiles):
                        rhs = dg[k] if with_norm else ident
                        rt = t_dg[k] if with_norm else t_ident
                        S.op("pe", lambda e, pt=pt, xt=xt, rhs=rhs, j=j, fc=fc: e.matmul(
                            pt[:, j * 128:(j + 1) * 128], xt[:, fc * 128:(fc + 1) * 128], rhs[:], start=True, stop=True),
                            reads=[tx, rt], writes=[tp], signal=(j == n_in - 1))
                    eng = "act" if ev_i % 2 == 0 else "dve"
                    ev_i += 1
                    dst = dst_fn(fc, ti * 128, n_in * 128)
                    src_ps = pt[:, 0:n_in * 128]
                    if with_norm:
                        if eng == "act":
                            S.op("act", lambda e, src_ps=src_ps, dst=dst, fc=fc: e.activation(
                                dst, src_ps, AF.Identity, bias=modv[:, fc:fc + 1], scale=a1[:, fc:fc + 1]),
                                reads=[tp, t_modv, t_a1], writes=[t_dst])
                        else:
                            S.op("dve", lambda e, src_ps=src_ps, dst=dst, fc=fc: e.tensor_scalar(
                                dst, src_ps, a1[:, fc:fc + 1], modv[:, fc:fc + 1], ALU.mult, ALU.add),
                                reads=[tp, t_modv, t_a1], writes=[t_dst])
                    else:
                        if eng == "act":
                            S.op("act", lambda e, src_ps=src_ps, dst=dst: e.copy(dst, src_ps), reads=[tp], writes=[t_dst])
                        else:
                            S.op("dve", lambda e, src_ps=src_ps, dst=dst: e.tensor_copy(dst, src_ps), reads=[tp], writes=[t_dst])
                ti += n_in

        def gemm_to_dram(ph, tag, hnT, t_hnT, ntok, chunks, dram, col0=0, mask=None):
            slabs = [sb(ph, tag + "w%d" % i, [128, 16, 128], BF16) for i in range(3)]
            wring = Ring(slabs, tag + "w")
            stg = [sb(ph, tag + "stg%d" % i, [128, ntok]) for i in range(2)]
            sring = Ring(stg, tag + "stg")
            pg = [ps(ph, tag + "pg%d" % i, [128, 512]) for i in range(4)]
            pring = Ring(pg, tag + "pg")
            groups = []
            o = 0
            while o < ntok:
                n = min(512, ntok - o)
                groups.append((o, n))
                o += n
            ev_i = 0
            for gc, rc in chunks:
                slab, tw, k = wring.next()
                S.dma("pool", slab[:], win_d[gc], "w%d" % k, writes=[tw])
                sg, tsg, k2 = sring.next()
                for (o, n) in groups:
                    pt, tp, _ = pring.next()
                    for kk in range(16):
                        S.op("pe", lambda e, pt=pt, slab=slab, kk=kk, o=o, n=n: e.matmul(
                            pt[:, 0:n], slab[:, kk, :], hnT[:, kk, o:o + n], start=(kk == 0), stop=(kk == 15)),
                            reads=[tw, t_hnT], writes=[tp], signal=(kk == 15))
                    if mask is not None:
                        S.op("dve", lambda e, pt=pt, sg=sg, o=o, n=n: e.tensor_tensor(sg[:, o:o + n], pt[:, 0:n], hmask[:, o:o + n], ALU.mult),
                             reads=[tp, t_hmask], writes=[tsg])
                    else:
                        eng = "act" if ev_i % 2 == 0 else "dve"
                        ev_i += 1
                        if eng == "act":
                            S.op("act", lambda e, pt=pt, sg=sg, o=o, n=n: e.copy(sg[:, o:o + n], pt[:, 0:n]), reads=[tp], writes=[tsg])
                        else:
                            S.op("dve", lambda e, pt=pt, sg=sg, o=o, n=n: e.tensor_copy(sg[:, o:o + n], pt[:, 0:n]), reads=[tp], writes=[tsg])
                S.dma("sp", dram[rc * 128:(rc + 1) * 128, col0:col0 + ntok], sg[:], "st%d" % k2, reads=[tsg], is_out=(debug == 2))

        HALF = SEQ // 2
        for half in range(2):
            with ExitStack() as ph:
                hnT = sb(ph, "hnT%d" % half, [128, 16, HALF], BF16); t_hnT = Tok("hnT")
                with ExitStack() as ph2:
                    transpose_pass(ph2, "a%d" % half, xf[half * HALF:(half + 1) * HALF, :], HALF // 128, True,
                                   lambda fc, o, n, hnT=hnT: hnT[:, fc, o:o + n], t_hnT)
                    S.barrier(); S.emit()
                with ExitStack() as ph2:
                    gemm_to_dram(ph2, "a%d" % half, hnT, t_hnT, HALF, [(16 + rc, rc) for rc in range(NRC)], PT, col0=half * HALF)
                    S.barrier(); S.emit()
        if stop <= 1:
            return _finish(nc, S, st, dram_in)
        with ExitStack() as ph:
            hnT = sb(ph, "hnTh", [128, 16, TH], BF16); t_hnT = Tok("hnTh")
            hmask = sb(ph, "hmask", [128, TH]); t_hmask = Tok("hmask")
            S.dma("sp", hmask[:], hmask_d, "hm", writes=[t_hmask])
            with ExitStack() as ph2:
                transpose_pass(ph2, "b", xh, TH // 128, True, lambda fc, o, n: hnT[:, fc, o:o + n], t_hnT)
                S.barrier(); S.emit()
            with ExitStack() as ph2:
                gemm_to_dram(ph2, "b", hnT, t_hnT, TH, [(gc, gc) for gc in range(NCC)], PH, mask=True)
                S.barrier(); S.emit()
        if stop <= 2:
            return _finish(nc, S, st, dram_in)

        class Buf:
            def __init__(self, t, k=None):
                self.t = t
                self.k = k if k is not None else Tok()

        def B(stack, name, shape, dt=F32):
            return Buf(sb(stack, name, shape, dt))

        def PB(stack, name, shape):
            return Buf(ps(stack, name, shape))

        def _ks(bs):
            return [b.k for b in bs]

        def V(fn, r=(), w=()):
            return S.op("dve", fn, reads=_ks(r), writes=_ks(w))

        def A(fn, r=(), w=()):
            return S.op("act", fn, reads=_ks(r), writes=_ks(w))

        def P(fn, r=(), w=()):
            return S.op("pool", fn, reads=_ks(r), writes=_ks(w))

        def M(fn, r=(), w=(), sig=True):
            return S.op("pe", fn, reads=_ks(r), writes=_ks(w), signal=sig)

        def LD(q, dst, src, key, w, batch=False):
            return S.dma(q, dst, src, key, writes=_ks(w), batch=batch)

        dumps = {}

        def DUMP(name, ap, shape, dt=F32):
            if debug != 3:
                return
            dten = nc.dram_tensor("dmp_" + name, list(shape), dt, kind="ExternalOutput").ap()
            dumps[name] = dten
            return dten

        staged = []

        def dump_buf(name, b_, shape, dt=F32, view=None):
            if debug != 3:
                return
            dten = DUMP(name, None, shape, dt)
            slot = Buf(stage_slots[name][0], stage_slots[name][1])
            P(lambda e: e.tensor_copy(slot.t[:], b_.t[:]), [b_], [slot])
            staged.append((dten, slot))

        def flush_dumps():
            for dten, slot in staged:
                S.dma("sp", dten, slot.t[:], "dmp", reads=[slot.k], batch=True, is_out=True)
            staged[:] = []

        Bident = Buf(ident, t_ident); Bidentb = Buf(identb, t_identb); Bmodv = Buf(modv, t_modv)
        Bgnv = Buf(gnv, t_gnv); Ba2 = Buf(a2, t_a2); Bqflag = Buf(qflag, t_qflag)

        cmask = B(st, "cmask", [128, 8, 128]); rst = B(st, "rst", [128, 512]); blk64 = B(st, "blk64", [128, 128])
        ones = B(st, "ones", [128, 128]); ident2 = B(st, "ident2", [128, 64])
        mu = B(st, "mu", [128, 2, NCC]); c0 = B(st, "c0", [128, NCC]); rv = B(st, "rv", [128, 9, 8]); omka = B(st, "omka", [128, 8])
        w2 = B(st, "w2", [128, DR], BF16); a2w = B(st, "a2w", [128, DR], BF16)
        g2a = B(st, "g2a", [128, DR], BF16); g2b = B(st, "g2b", [32, DR], BF16)
        convw = B(st, "convw", [128, 8, KW]); convv = B(st, "convv", [128, 3, 8])
        st_y = ExitStack()
        yTr = B(st_y, "yTr", [128, 8, TOK], BF16)
        for (b_, d_) in ((cmask, din("cmask", [128, 8, 128])), (rst, din("rst", [128, 512])), (blk64, din("blk64", [128, 128])),
                         (ones, din("ones", [128, 128])), (ident2, din("ident2", [128, 64])), (mu, din("mu", [128, 2, NCC])),
                         (rv, din("rv", [128, 9, 8])), (convw, din("convw", [128, 8, KW])), (convv, din("convv", [128, 3, 8]))):
            LD("sp", b_.t[:], d_, "const", [b_], batch=True)
        for (b_, d_) in ((w2, din("w2", [128, DR])), (a2w, din("a2w", [128, DR])), (g2a, din("g2a", [128, DR])), (g2b, din("g2b", [32, DR]))):
            LD("pool", b_.t[:], d_, "constc", [b_], batch=True)
        V(lambda e: e.tensor_tensor(c0.t[:], mu.t[:, 0, :], mu.t[:, 1, :], ALU.add), [mu], [c0])
        V(lambda e: e.tensor_scalar(c0.t[:], c0.t[:], -1.0, 1.0, ALU.mult, ALU.add), [c0], [c0])
        V(lambda e: e.tensor_scalar(omka.t[:], rv.t[:, 5, :], -1.0, 1.0, ALU.mult, ALU.add), [rv], [omka])

        def load_shift(ph_, dst, dram, gc, rows, col0, n, lo_edge, hi_edge, raw, nrows=128, key="raw"):
            a = 0 if lo_edge else 1
            bnd = 0 if hi_edge else 1
            if lo_edge or hi_edge:
                P(lambda e: e.memset(raw.t[:nrows, 0:n + 2], 0.0), [], [raw])
            LD("sp", raw.t[:nrows, 1 - a:n + 1 + bnd], dram[rows:rows + nrows, col0 - a:col0 + n + bnd], key, [raw])
            V(lambda e: e.tensor_scalar(dst.t[:nrows, 0:n], raw.t[:nrows, 1:n + 1], c0.t[:nrows, gc:gc + 1], None, ALU.mult), [raw, c0], [dst])
            V(lambda e: e.scalar_tensor_tensor(dst.t[:nrows, 0:n], raw.t[:nrows, 0:n], mu.t[:nrows, 0, gc:gc + 1], dst.t[:nrows, 0:n], ALU.mult, ALU.add), [raw, mu, dst], [dst])
            V(lambda e: e.scalar_tensor_tensor(dst.t[:nrows, 0:n], raw.t[:nrows, 2:n + 2], mu.t[:nrows, 1, gc:gc + 1], dst.t[:nrows, 0:n], ALU.mult, ALU.add), [raw, mu, dst], [dst])

        def issue_load(raw, dram, rows, col0, n, lo_edge, hi_edge, key):
            a = 0 if lo_edge else 1
            bnd = 0 if hi_edge else 1
            if lo_edge or hi_edge:
                P(lambda e: e.memset(raw.t[:, 0:n + 2], 0.0), [], [raw])
            LD("sp", raw.t[:, 1 - a:n + 1 + bnd], dram[rows:rows + 128, col0 - a:col0 + n + bnd], key, [raw])

        def apply_shift(dst, raw, gc, n):
            V(lambda e: e.tensor_scalar(dst.t[:, 0:n], raw.t[:, 1:n + 1], c0.t[:, gc:gc + 1], None, ALU.mult), [raw, c0], [dst])
            V(lambda e: e.scalar_tensor_tensor(dst.t[:, 0:n], raw.t[:, 0:n], mu.t[:, 0, gc:gc + 1], dst.t[:, 0:n], ALU.mult, ALU.add), [raw, mu, dst], [dst])
            V(lambda e: e.scalar_tensor_tensor(dst.t[:, 0:n], raw.t[:, 2:n + 2], mu.t[:, 1, gc:gc + 1], dst.t[:, 0:n], ALU.mult, ALU.add), [raw, mu, dst], [dst])

        NB = SEQ // 512
        with ExitStack() as ph:
            TWD = nc.dram_tensor("TWD", [128, SEQ], BF16, kind="Internal").ap(); ADD = nc.dram_tensor("ADD", [128, SEQ], BF16, kind="Internal").ap()
            ado = B(ph, "ado", [128, TOK], BF16); sga = B(ph, "sga", [128, TOK], BF16); sgb = B(ph, "sgb", [32, TOK], BF16)
            YD = [nc.dram_tensor("YF", [128, SEQ], F32, kind="Internal").ap(), nc.dram_tensor("YB", [128, SEQ], F32, kind="Internal").ap()]
            with ExitStack() as p2:
                raw = B(p2, "raw0", [128, 1026]); tmp = B(p2, "tmp0", [128, 1024]); tb1 = B(p2, "tb1", [128, 1024], BF16); tb2 = B(p2, "tb2", [128, 1024], BF16)
                for blk in range(SEQ // 1024):
                    lo, hi = blk == 0, blk == SEQ // 1024 - 1
                    load_shift(p2, tmp, PT, 40, 24 * 128, blk * 1024, 1024, lo, hi, raw)
                    A(lambda e: e.activation(tb1.t[:], tmp.t[:], AF.Tanh), [tmp], [tb1])
                    S.dma("sp", TWD[:, blk * 1024:(blk + 1) * 1024], tb1.t[:], "twd", reads=[tb1.k])
                    load_shift(p2, tmp, PT, 41, 25 * 128, blk * 1024, 1024, lo, hi, raw)
                    A(lambda e: e.copy(tb2.t[:], tmp.t[:]), [tmp], [tb2])
                    S.dma("sp", ADD[:, blk * 1024:(blk + 1) * 1024], tb2.t[:], "add", reads=[tb2.k])
                load_shift(p2, tmp, PH, 41, 41 * 128, HALO, TOK, False, False, raw)
                A(lambda e: e.copy(ado.t[:], tmp.t[:]), [tmp], [ado])
                load_shift(p2, tmp, PH, 42, 42 * 128, HALO, TOK, False, False, raw)
                A(lambda e: e.activation(sga.t[:], tmp.t[:], AF.Sigmoid), [tmp], [sga])
                load_shift(p2, tmp, PH, 43, 43 * 128, HALO, TOK, False, False, raw, nrows=32)
                A(lambda e: e.activation(sgb.t[:], tmp.t[0:32, :], AF.Sigmoid), [tmp], [sgb])
                S.barrier(); S.emit()

            for pc in ([dbg_pc] if debug == 3 else range(8)):
                cs_ = slice(pc * 128, (pc + 1) * 128)
                with ExitStack() as p2:
                    WS_F32 = ("r", "k", "lw", "lr", "kk", "t1", "t2", "pref", "cum", "E", "bb", "kd")
                    WS_BF = ("At", "Rt", "Bt", "Kt", "Bh", "Kh", "Vb")
                    WS_TOK = ("Atok", "Bhtok", "Khtok", "Vtok", "Gtok", "AkVs", "U0tok")

                    def mk_ws(tag):
                        W = {"raws": [[B(p2, "raw%s%d%s" % (x_, s_, tag), [128, 514]) for x_ in "rkv"] for s_ in range(2)],
                             "tws": [B(p2, "twb%d%s" % (s_, tag), [128, 512], BF16) for s_ in range(2)],
                             "ads": [B(p2, "adb%d%s" % (s_, tag), [128, 512], BF16) for s_ in range(2)]}
                        for n_ in WS_F32:
                            W[n_] = B(p2, n_ + tag, [128, 512])
                        for n_ in WS_BF:
                            W[n_] = B(p2, n_ + tag, [128, 512], BF16)
                        for n_ in WS_TOK:
                            W[n_] = B(p2, n_ + tag, [128, 4, 128], BF16)
                        W["hb"] = [{n_: B(p2, "%s%d%s" % (n_, h, tag), [128, 4, 128], BF16) for n_ in
                                    ("AkT", "ArbT", "ArkT", "TT", "N", "NT", "Na", "NTa", "Nb", "NTb", "Tt")} for h in range(2)]
                        W["PhiT"] = B(p2, "PhiT" + tag, [128, 4, 64]); W["Zloc"] = B(p2, "Zloc" + tag, [128, 4, 64])
                        W["Z"] = B(p2, "Z" + tag, [128, 64]); W["WC"] = B(p2, "WC" + tag, [128, 4])
                        W["py"] = PB(p2, "py" + tag, [128, 512]); W["pz"] = PB(p2, "pz" + tag, [128, 64])
                        return W

                    WSP = [mk_ws("f"), mk_ws("b")]
                    pr = [PB(p2, "pr%d" % i, [128, 512]) for i in range(4)]
                    pring = Ring(pr, "pr")

                    def pnext():
                        k = pring.i % len(pr)
                        pring.i += 1
                        return pr[k]

                    v3 = lambda b_: b_.t[:].rearrange("p (c t) -> p c t", t=128)

                    def issue_block_loads(d, tb, W, s_):
                        sfx = "fb"[d]
                        lo, hi = tb == 0, tb == NB - 1
                        c0_ = tb * 512
                        ts_ = slice(c0_, c0_ + 512)
                        LD("act", W["tws"][s_].t[:], TWD[:, ts_], "twl%d%s" % (s_, sfx), [W["tws"][s_]])
                        LD("act", W["ads"][s_].t[:], ADD[:, ts_], "adl%d%s" % (s_, sfx), [W["ads"][s_]])
                        issue_load(W["raws"][s_][0], PT, pc * 128, c0_, 512, lo, hi, "rawr%d%s" % (s_, sfx))
                        issue_load(W["raws"][s_][1], PT, (8 + pc) * 128, c0_, 512, lo, hi, "rawk%d%s" % (s_, sfx))
                        issue_load(W["raws"][s_][2], PT, (16 + pc) * 128, c0_, 512, lo, hi, "rawv%d%s" % (s_, sfx))

                    def block_gen(d, tb, W, idx, nxt_tb):
                        s_ = idx % 2
                        if idx == 0:
                            issue_block_loads(d, tb, W, s_)
                        if nxt_tb is not None:
                            issue_block_loads(d, nxt_tb, W, 1 - s_)
                        tw, adf = W["tws"][s_], W["ads"][s_]
                        r_, k_, lw, lr, kk = W["r"], W["k"], W["lw"], W["lr"], W["kk"]
                        t1, t2, pref, cum, E, bb, kd = W["t1"], W["t2"], W["pref"], W["cum"], W["E"], W["bb"], W["kd"]
                        v_ = t2; QT = E; Y0s = t1
                        At, Rt, Bt, Kt, Bh, Kh, Vb = W["At"], W["Rt"], W["Bt"], W["Kt"], W["Bh"], W["Kh"], W["Vb"]
                        Atok, Bhtok, Khtok, Vtok, Gtok, AkVs, U0tok = [W[n_] for n_ in WS_TOK]
                        hb = W["hb"]; PhiT, Zloc, Z, WC, py, pz = W["PhiT"], W["Zloc"], W["Z"], W["WC"], W["py"], W["pz"]
                        hs_w = slice(d * 64, d * 64 + 64)
                        mN, mI, mNT = (0, 1, 2) if d == 0 else (2, 3, 0)
                        sfx = "fb"[d]
                        lo, hi = tb == 0, tb == NB - 1
                        c0_ = tb * 512
                        ts_ = slice(c0_, c0_ + 512)
                        apply_shift(r_, W["raws"][s_][0], 16 + pc, 512)
                        apply_shift(k_, W["raws"][s_][1], 24 + pc, 512)
                        apply_shift(v_, W["raws"][s_][2], 32 + pc, 512)
                        A(lambda e: e.copy(Vb.t[:], v_.t[:]), [v_], [Vb])
                        yield
                        pt = pnext()
                        M(lambda e, pt=pt, hs_w=hs_w: e.matmul(pt.t[:], w2.t[hs_w, cs_], tw.t[hs_w, :], start=True, stop=True), [w2, tw], [pt])
                        A(lambda e, pt=pt, d=d: e.activation(lw.t[:], pt.t[:], AF.Sigmoid, bias=rv.t[:, d, pc:pc + 1]), [pt, rv], [lw])
                        V(lambda e: e.tensor_scalar(lw.t[:], lw.t[:], -0.6065306597126334, None, ALU.mult), [lw], [lw])
                        pt = pnext()
                        M(lambda e, pt=pt, hs_w=hs_w: e.matmul(pt.t[:], a2w.t[hs_w, cs_], adf.t[hs_w, :], start=True, stop=True), [a2w, adf], [pt])
                        A(lambda e, pt=pt, d=d: e.activation(lr.t[:], pt.t[:], AF.Sigmoid, bias=rv.t[:, 2 + d, pc:pc + 1]), [pt, rv], [lr])
                        yield
                        V(lambda e: e.tensor_scalar(kk.t[:], k_.t[:], rv.t[:, 4, pc:pc + 1], None, ALU.mult), [k_, rv], [kk])
                        P(lambda e: e.tensor_tensor(t1.t[:], kk.t[:], kk.t[:], ALU.mult), [kk], [t1])
                        pt = pnext()
                        M(lambda e, pt=pt: e.matmul(pt.t[:], blk64.t[:], t1.t[:], start=True, stop=True), [blk64, t1], [pt])
                        A(lambda e, pt=pt: e.activation(t2.t[:], pt.t[:], AF.Sqrt), [pt], [t2])
                        V(lambda e: e.tensor_scalar(t2.t[:], t2.t[:], 1e-12, None, ALU.max), [t2], [t2])
                        V(lambda e: e.reciprocal(t2.t[:], t2.t[:]), [t2], [t2])
                        P(lambda e: e.tensor_tensor(kk.t[:], kk.t[:], t2.t[:], ALU.mult), [kk, t2], [kk])
                        yield
                        V(lambda e: e.tensor_scalar(t1.t[:], lr.t[:], rv.t[:, 5, pc:pc + 1], omka.t[:, pc:pc + 1], ALU.mult, ALU.add), [lr, rv, omka], [t1])
                        P(lambda e: e.tensor_tensor(kd.t[:], k_.t[:], t1.t[:], ALU.mult), [k_, t1], [kd])
                        P(lambda e: e.tensor_tensor(bb.t[:], kk.t[:], lr.t[:], ALU.mult), [kk, lr], [bb])
                        yield
                        V(lambda e: e.tensor_tensor_scan(pref.t[:], rst.t[:], lw.t[:], 0.0, ALU.mult, ALU.add), [rst, lw], [pref])
                        tot_bc = v3(pref)[:, :, 127:128].to_broadcast([128, 4, 128])
                        if d == 0:
                            cum = pref
                        else:
                            V(lambda e: e.tensor_tensor(cum.t[:], lw.t[:], pref.t[:], ALU.subtract), [lw, pref], [cum])
                            V(lambda e: e.tensor_tensor(v3(cum), v3(cum), tot_bc, ALU.add), [cum, pref], [cum])
                        A(lambda e: e.activation(WC.t[:], v3(pref)[:, :, 127], AF.Exp), [pref], [WC])
                        yield
                        A(lambda e: e.activation(E.t[:], cum.t[:], AF.Exp, scale=-1.0), [cum], [E])
                        V(lambda e: e.tensor_tensor(Bt.t[:], bb.t[:], E.t[:], ALU.mult), [bb, E], [Bt])
                        P(lambda e: e.tensor_tensor(Kt.t[:], kd.t[:], E.t[:], ALU.mult), [kd, E], [Kt])
                        yield
                        V(lambda e: e.tensor_tensor(v3(t1), tot_bc, v3(cum), ALU.subtract), [pref, cum], [t1])
                        A(lambda e: e.activation(E.t[:], t1.t[:], AF.Exp), [t1], [E])
                        V(lambda e: e.tensor_tensor(Bh.t[:], bb.t[:], E.t[:], ALU.mult), [bb, E], [Bh])
                        P(lambda e: e.tensor_tensor(Kh.t[:], kd.t[:], E.t[:], ALU.mult), [kd, E], [Kh])
                        yield
                        A(lambda e: e.activation(E.t[:], cum.t[:], AF.Exp), [cum], [E])
                        V(lambda e: e.tensor_tensor(Rt.t[:], r_.t[:], E.t[:], ALU.mult), [r_, E], [Rt])
                        V(lambda e: e.tensor_tensor(t1.t[:], cum.t[:], lw.t[:], ALU.subtract), [cum, lw], [t1])
                        A(lambda e: e.activation(E.t[:], t1.t[:], AF.Exp), [t1], [E])
                        V(lambda e: e.scalar_tensor_tensor(At.t[:], kk.t[:], -1.0, E.t[:], ALU.mult, ALU.mult), [kk, E], [At])
                        if debug == 3 and d == dbg_d and tb == dbg_tb:
                            for nm_, b__, dt_ in (("r", r_, F32), ("k", k_, F32), ("lw", lw, F32), ("lr", lr, F32), ("kk", kk, F32), ("kd", kd, F32), ("bb", bb, F32),
                                                  ("cum", cum, F32), ("pref", pref, F32), ("At", At, BF16), ("Rt", Rt, BF16), ("Bt", Bt, BF16), ("Kt", Kt, BF16),
                                                  ("Bh", Bh, BF16), ("Kh", Kh, BF16), ("Vb", Vb, BF16)):
                                dump_buf(nm_, b__, [128, 512], dt_)
                            dump_buf("WC", WC, [128, 4])
                        yield
                        for (src_, dst_) in ((At, Atok), (Bh, Bhtok), (Kh, Khtok), (Vb, Vtok)):
                            pt = pnext()
                            for c in range(4):
                                M(lambda e, pt=pt, src_=src_, c=c: e.matmul(pt.t[:, c * 128:(c + 1) * 128], src_.t[:, c * 128:(c + 1) * 128], identb[:], start=True, stop=True),
                                  [src_, Bidentb], [pt], sig=(c == 3))
                            A(lambda e, pt=pt, dst_=dst_: e.copy(dst_.t[:].rearrange("p c t -> p (c t)"), pt.t[:]), [pt], [dst_])
                        yield
                        for h in range(2):
                            hs = slice(h * 64, h * 64 + 64)
                            H = hb[h]
                            for (nm, lh, rh, mk) in (("N", Bt, At, mN), ("NT", At, Bt, mNT), ("AkT", Kt, At, mN), ("ArbT", Bt, Rt, mI), ("ArkT", Kt, Rt, mI)):
                                pt = pnext()
                                for c in range(4):
                                    M(lambda e, pt=pt, lh=lh, rh=rh, c=c, hs=hs: e.matmul(pt.t[:, c * 128:(c + 1) * 128], lh.t[hs, c * 128:(c + 1) * 128], rh.t[hs, c * 128:(c + 1) * 128], start=True, stop=True),
                                      [lh, rh], [pt], sig=(c == 3))
                                V(lambda e, pt=pt, nm=nm, mk=mk, H=H: e.tensor_tensor(H[nm].t[:], pt.t[:].rearrange("p (c t) -> p c t", t=128), cmask.t[:, mk:mk + 1, :].to_broadcast([128, 4, 128]), ALU.mult),
                                  [pt, cmask], [H[nm]])
                        yield
                        c4 = lambda b_: b_.t[:].rearrange("p c t -> p (c t)")
                        mk4 = lambda m_: cmask.t[:, m_:m_ + 1, :].to_broadcast([128, 4, 128])
                        idb = identb[:].rearrange("p (o t) -> p o t", o=1).to_broadcast([128, 4, 128])

                        def mm4(lhs, rhs):
                            pt_ = pnext()
                            for c in range(4):
                                M(lambda e, pt_=pt_, lhs=lhs, rhs=rhs, c=c: e.matmul(pt_.t[:, c * 128:(c + 1) * 128], lhs.t[:, c, :], rhs.t[:, c, :], start=True, stop=True),
                                  [lhs, rhs], [pt_], sig=(c == 3))
                            return pt_

                        def cp4(dst, pt_, eng):
                            if eng == "act":
                                A(lambda e, dst=dst, pt_=pt_: e.copy(c4(dst), pt_.t[:]), [pt_], [dst])
                            else:
                                V(lambda e, dst=dst, pt_=pt_: e.tensor_copy(c4(dst), pt_.t[:]), [pt_], [dst])

                        def acc4(dst, pt_):
                            V(lambda e, dst=dst, pt_=pt_: e.tensor_tensor(c4(dst), pt_.t[:], c4(dst), ALU.add), [pt_, dst], [dst])

                        def inv_gen(H):
                            TTb, Ttb = H["TT"], H["Tt"]
                            Nk, NTk = H["Na"], H["NTa"]
                            P(lambda e, Nk=Nk: e.tensor_tensor(Nk.t[:], H["N"].t[:], mk4(4), ALU.mult), [H["N"], cmask], [Nk])
                            P(lambda e, NTk=NTk: e.tensor_tensor(NTk.t[:], H["NT"].t[:], mk4(4), ALU.mult), [H["NT"], cmask], [NTk])
                            V(lambda e, Nk=Nk: e.tensor_tensor(TTb.t[:], Nk.t[:], idb, ALU.add), [Nk, Bidentb], [TTb])
                            V(lambda e, NTk=NTk: e.tensor_tensor(Ttb.t[:], NTk.t[:], idb, ALU.add), [NTk, Bidentb], [Ttb])
                            yield
                            for lev in range(3):
                                N2, NT2 = (H["Nb"], H["NTb"]) if lev % 2 == 0 else (H["Na"], H["NTa"])
                                p1 = mm4(Nk, NTk)
                                p2 = mm4(NTk, Nk)
                                yield
                                cp4(NT2, p1, "act"); cp4(N2, p2, "act")
                                yield
                                p3 = mm4(NT2, TTb); p4 = mm4(N2, Ttb)
                                yield
                                acc4(TTb, p3); acc4(Ttb, p4)
                                yield
                                Nk, NTk = N2, NT2
                            for mi, mk_ in enumerate((5, 6, 7)):
                                O_, Ot_, X_, Xt_ = H["Na"], H["NTa"], H["Nb"], H["NTb"]
                                last = (mi == 2)
                                P(lambda e, O_=O_, mk_=mk_: e.tensor_tensor(O_.t[:], H["N"].t[:], mk4(mk_), ALU.mult), [H["N"], cmask], [O_])
                                P(lambda e, Ot_=Ot_, mk_=mk_: e.tensor_tensor(Ot_.t[:], H["NT"].t[:], mk4(mk_), ALU.mult), [H["NT"], cmask], [Ot_])
                                yield
                                px = mm4(Ot_, TTb)
                                pxt = mm4(O_, Ttb) if not last else None
                                yield
                                cp4(X_, px, "act")
                                if not last:
                                    cp4(Xt_, pxt, "act")
                                yield
                                pa = mm4(Ttb, X_)
                                pb = mm4(TTb, Xt_) if not last else None
                                yield
                                acc4(TTb, pa)
                                if not last:
                                    acc4(Ttb, pb)
                                yield

                        gens = [inv_gen(hb[0]), inv_gen(hb[1])]
                        while gens:
                            for g_ in list(gens):
                                try:
                                    next(g_)
                                except StopIteration:
                                    gens.remove(g_)
                        if debug == 3 and d == dbg_d and tb == dbg_tb:
                            dump_buf("Atok", Atok, [128, 4, 128], BF16); dump_buf("Vtok", Vtok, [128, 4, 128], BF16)
                            for h in range(2):
                                for nm_ in ("AkT", "ArbT", "ArkT", "TT"):
                                    dump_buf("%s%d" % (nm_, h), hb[h][nm_], [128, 4, 128], BF16)
                            dump_buf("N1", hb[1]["N"], [128, 4, 128], BF16); dump_buf("NT1", hb[1]["NT"], [128, 4, 128], BF16)
                        yield
                        pt = pnext()
                        for h in range(2):
                            for c in range(4):
                                M(lambda e, pt=pt, h=h, c=c: e.matmul(pt.t[:, c * 128 + h * 64:c * 128 + h * 64 + 64], hb[h]["TT"].t[:, c, :], Atok.t[:, c, h * 64:h * 64 + 64], start=True, stop=True),
                                  [hb[h]["TT"], Atok], [pt], sig=(h == 1 and c == 3))
                        A(lambda e, pt=pt: e.copy(Gtok.t[:].rearrange("p c t -> p (c t)"), pt.t[:]), [pt], [Gtok])
                        yield
                        pt = pnext()
                        for h in range(2):
                            for c in range(4):
                                M(lambda e, pt=pt, h=h, c=c: e.matmul(pt.t[:, c * 128 + h * 64:c * 128 + h * 64 + 64], hb[h]["AkT"].t[:, c, :], Vtok.t[:, c, h * 64:h * 64 + 64], start=True, stop=True),
                                  [hb[h]["AkT"], Vtok], [pt], sig=(h == 1 and c == 3))
                        V(lambda e, pt=pt: e.tensor_copy(AkVs.t[:].rearrange("p c t -> p (c t)"), pt.t[:]), [pt], [AkVs])
                        yield
                        pt = pnext()
                        for h in range(2):
                            for c in range(4):
                                M(lambda e, pt=pt, h=h, c=c: e.matmul(pt.t[:, c * 128 + h * 64:c * 128 + h * 64 + 64], hb[h]["TT"].t[:, c, :], AkVs.t[:, c, h * 64:h * 64 + 64], start=True, stop=True),
                                  [hb[h]["TT"], AkVs], [pt], sig=(h == 1 and c == 3))
                        A(lambda e, pt=pt: e.copy(U0tok.t[:].rearrange("p c t -> p (c t)"), pt.t[:]), [pt], [U0tok])
                        yield
                        ptY = pnext()
                        for h in range(2):
                            hs = slice(h * 64, h * 64 + 64)
                            for c in range(4):
                                M(lambda e, h=h, c=c, hs=hs, ptY=ptY: e.matmul(ptY.t[hs, c * 128:(c + 1) * 128], U0tok.t[:, c, hs], hb[h]["ArbT"].t[:, c, :], start=True, stop=False), [U0tok, hb[h]["ArbT"]], [ptY], sig=False)
                                M(lambda e, h=h, c=c, hs=hs, ptY=ptY: e.matmul(ptY.t[hs, c * 128:(c + 1) * 128], Vtok.t[:, c, hs], hb[h]["ArkT"].t[:, c, :], start=False, stop=True), [Vtok, hb[h]["ArkT"]], [ptY], sig=(h == 1 and c == 3))
                        A(lambda e, ptY=ptY: e.copy(Y0s.t[:], ptY.t[:]), [ptY], [Y0s])
                        ptZ = pnext()
                        for h in range(2):
                            hs = slice(h * 64, h * 64 + 64)
                            for c in range(4):
                                M(lambda e, c=c, hs=hs, ptZ=ptZ: e.matmul(ptZ.t[hs, c * 64:(c + 1) * 64], Bhtok.t[:, c, hs], U0tok.t[:, c, hs], start=True, stop=False), [Bhtok, U0tok], [ptZ], sig=False)
                                M(lambda e, c=c, hs=hs, ptZ=ptZ: e.matmul(ptZ.t[hs, c * 64:(c + 1) * 64], Khtok.t[:, c, hs], Vtok.t[:, c, hs], start=False, stop=True), [Khtok, Vtok], [ptZ], sig=(h == 1 and c == 3))
                        V(lambda e, ptZ=ptZ: e.tensor_copy(Zloc.t[:].rearrange("p c j -> p (c j)"), ptZ.t[:, 0:256]), [ptZ], [Zloc])
                        ptP = pnext()
                        for h in range(2):
                            hs = slice(h * 64, h * 64 + 64)
                            for c in range(4):
                                M(lambda e, c=c, hs=hs, ptP=ptP: e.matmul(ptP.t[hs, c * 64:(c + 1) * 64], Gtok.t[:, c, hs], Bhtok.t[:, c, hs], start=True, stop=True), [Gtok, Bhtok], [ptP], sig=(h == 1 and c == 3))
                        for c in range(4):
                            V(lambda e, c=c, ptP=ptP: e.scalar_tensor_tensor(PhiT.t[:, c, :], ident2.t[:], WC.t[:, c:c + 1], ptP.t[:, c * 64:(c + 1) * 64], ALU.mult, ALU.add), [ident2, WC, ptP], [PhiT])
                        ptQ = pnext()
                        for h in range(2):
                            hs = slice(h * 64, h * 64 + 64)
                            for c in range(4):
                                M(lambda e, h=h, c=c, hs=hs, ptQ=ptQ: e.matmul(ptQ.t[hs, c * 128:(c + 1) * 128], Gtok.t[:, c, hs], hb[h]["ArbT"].t[:, c, :], start=True, stop=True), [Gtok, hb[h]["ArbT"]], [ptQ], sig=(h == 1 and c == 3))
                        V(lambda e, ptQ=ptQ: e.tensor_tensor(QT.t[:], ptQ.t[:], Rt.t[:], ALU.add), [ptQ, Rt], [QT])
                        if debug == 3 and d == dbg_d and tb == dbg_tb:
                            dump_buf("Gtok", Gtok, [128, 4, 128], BF16); dump_buf("U0tok", U0tok, [128, 4, 128], BF16)
                            dump_buf("Y0s", Y0s, [128, 512]); dump_buf("Zloc", Zloc, [128, 4, 64]); dump_buf("PhiT", PhiT, [128, 4, 64]); dump_buf("QT", QT, [128, 512])
                            dump_buf("Zin", Z, [128, 64])
                        yield
                        corder = range(4) if d == 0 else range(3, -1, -1)
                        for ci, c in enumerate(corder):
                            for h in range(2):
                                hs = slice(h * 64, h * 64 + 64)
                                M(lambda e, c=c, hs=hs: e.matmul(py.t[hs, c * 128:(c + 1) * 128], Z.t[hs, :], QT.t[hs, c * 128:(c + 1) * 128], start=True, stop=True), [Z, QT], [py], sig=False)
                                M(lambda e, c=c, hs=hs: e.matmul(pz.t[hs, :], PhiT.t[hs, c, :], Z.t[hs, :], start=True, stop=True), [PhiT, Z], [pz], sig=(h == 1))
                            V(lambda e, c=c: e.tensor_tensor(Z.t[:], pz.t[:], Zloc.t[:, c, :], ALU.add), [pz, Zloc], [Z])
                        V(lambda e: e.tensor_tensor(Y0s.t[:], py.t[:], Y0s.t[:], ALU.add), [py, Y0s], [Y0s])
                        S.dma("sp", YD[d][:, ts_], Y0s.t[:], "yst" + sfx, reads=[Y0s.k])
                        yield

                    for d in range(2):
                        P(lambda e, d=d: e.memset(WSP[d]["Z"].t[:], 0.0), [], [WSP[d]["Z"]])
                    ords = [list(range(NB)), list(range(NB - 1, -1, -1))]
                    seqs = [iter([block_gen(d_, tb, WSP[d_], i_, (ords[d_][i_ + 1] if i_ + 1 < NB else None)) for i_, tb in enumerate(ords[d_])]) for d_ in range(2)]
                    cur = [next(seqs[0]), next(seqs[1])]
                    while any(c_ is not None for c_ in cur):
                        for di in range(2):
                            if cur[di] is None:
                                continue
                            try:
                                next(cur[di])
                            except StopIteration:
                                cur[di] = next(seqs[di], None)
                    if debug == 3:
                        flush_dumps()

                    S.barrier(); S.emit()
                with ExitStack() as p2:
                    raw = B(p2, "rawo", [128, TOK + 2])
                    ro = B(p2, "ro", [128, TOK]); ko = B(p2, "ko", [128, TOK]); vo = B(p2, "vo", [128, TOK])
                    ys = B(p2, "ys", [128, TOK]); u1 = B(p2, "u1", [128, TOK]); u2 = B(p2, "u2", [128, TOK]); u3 = B(p2, "u3", [128, TOK])
                    po = [PB(p2, "po%d" % i, [128, 512]) for i in range(6)]
                    rawk_o = B(p2, "rawok", [128, TOK + 2]); rawv_o = B(p2, "rawov", [128, TOK + 2])
                    yl = [[B(p2, "yl%d_%d" % (q_, d_), [128, TOK]) for d_ in range(2)] for q_ in range(4)]
                    for q_ in range(4):
                        for d_ in range(2):
                            LD("sp" if d_ == 0 else "act", yl[q_][d_].t[:], YD[d_][:, q_ * TOK:(q_ + 1) * TOK], ("wa0", "wa1", "wa2", "wa3", "wa4", "wa5", "xt0", "xt1")[q_ * 2 + d_], [yl[q_][d_]])
                    load_shift(p2, ro, PH, 16 + pc, (16 + pc) * 128, HALO, TOK, False, False, raw, key="xt2")
                    load_shift(p2, ko, PH, 24 + pc, (24 + pc) * 128, HALO, TOK, False, False, rawk_o, key="xt3")
                    load_shift(p2, vo, PH, 32 + pc, (32 + pc) * 128, HALO, TOK, False, False, rawv_o, key="hm")
                    for q_ in range(4):
                        for d in range(2):
                            if q_ == 0 and d == 0:
                                V(lambda e, d=d, q_=q_: e.tensor_scalar(ys.t[:], yl[q_][d].t[:], qflag[:, q_:q_ + 1], None, ALU.mult), [yl[q_][d], Bqflag], [ys])
                            else:
                                V(lambda e, d=d, q_=q_: e.scalar_tensor_tensor(ys.t[:], yl[q_][d].t[:], qflag[:, q_:q_ + 1], ys.t[:], ALU.mult, ALU.add), [yl[q_][d], Bqflag, ys], [ys])
                    for half in range(2):
                        hsl = slice(half * 512, half * 512 + 512)
                        for d in range(2):
                            M(lambda e, d=d, hsl=hsl, half=half: e.matmul(po[d].t[:], a2w.t[d * 64:d * 64 + 64, cs_], ado.t[d * 64:d * 64 + 64, hsl], start=True, stop=True), [a2w, ado], [po[d]])
                        A(lambda e, hsl=hsl: e.activation(u1.t[:, hsl], po[0].t[:], AF.Sigmoid, bias=rv.t[:, 2, pc:pc + 1]), [po[0], rv], [u1])
                        A(lambda e, hsl=hsl: e.activation(u2.t[:, hsl], po[1].t[:], AF.Sigmoid, bias=rv.t[:, 3, pc:pc + 1]), [po[1], rv], [u2])
                    V(lambda e: e.tensor_tensor(u1.t[:], u1.t[:], u2.t[:], ALU.add), [u1, u2], [u1])
                    V(lambda e: e.tensor_scalar(u2.t[:], omka.t[:, pc:pc + 1].to_broadcast([128, TOK]), 2.0, None, ALU.mult), [omka], [u2])
                    V(lambda e: e.scalar_tensor_tensor(u1.t[:], u1.t[:], rv.t[:, 5, pc:pc + 1], u2.t[:], ALU.mult, ALU.add), [u1, rv, u2], [u1])
                    V(lambda e: e.tensor_tensor(u1.t[:], u1.t[:], ko.t[:], ALU.mult), [u1, ko], [u1])
                    V(lambda e: e.scalar_tensor_tensor(u1.t[:], u1.t[:], rv.t[:, 6, pc:pc + 1], ro.t[:], ALU.mult, ALU.mult), [u1, rv, ro], [u1])
                    A(lambda e: e.activation(u2.t[:], ys.t[:], AF.Square), [ys], [u2])
                    for half in range(2):
                        hsl = slice(half * 512, half * 512 + 512)
                        M(lambda e, hsl=hsl, half=half: e.matmul(po[0 + half].t[:], blk64.t[:], ys.t[:, hsl], start=True, stop=True), [blk64, ys], [po[0 + half]])
                        M(lambda e, hsl=hsl, half=half: e.matmul(po[2 + half].t[:], blk64.t[:], u2.t[:, hsl], start=True, stop=True), [blk64, u2], [po[2 + half]])
                        M(lambda e, hsl=hsl, half=half: e.matmul(po[4 + half].t[:], blk64.t[:], u1.t[:, hsl], start=True, stop=True), [blk64, u1], [po[4 + half]])
                    for half in range(2):
                        hsl = slice(half * 512, half * 512 + 512)
                        A(lambda e, hsl=hsl, half=half: e.mul(u3.t[:, hsl], po[0 + half].t[:], 1.0 / 64), [po[0 + half]], [u3])
                        V(lambda e, hsl=hsl, half=half: e.tensor_tensor(u1.t[:, hsl], po[4 + half].t[:], vo.t[:, hsl], ALU.mult), [po[4 + half], vo], [u1])
                        V(lambda e, hsl=hsl: e.tensor_tensor(ro.t[:, hsl], u3.t[:, hsl], u3.t[:, hsl], ALU.mult), [u3], [ro])
                        V(lambda e, hsl=hsl, half=half: e.scalar_tensor_tensor(u2.t[:, hsl], po[2 + half].t[:], 1.0 / 64, ro.t[:, hsl], ALU.mult, ALU.subtract), [po[2 + half], ro], [u2])
                    V(lambda e: e.tensor_scalar(u2.t[:], u2.t[:], GN_EPS, None, ALU.add), [u2], [u2])
                    A(lambda e: e.activation(u2.t[:], u2.t[:], AF.Sqrt), [u2], [u2])
                    V(lambda e: e.reciprocal(u2.t[:], u2.t[:]), [u2], [u2])
                    V(lambda e: e.tensor_tensor(ys.t[:], ys.t[:], u3.t[:], ALU.subtract), [ys, u3], [ys])
                    V(lambda e: e.tensor_tensor(ys.t[:], ys.t[:], u2.t[:], ALU.mult), [ys, u2], [ys])
                    V(lambda e: e.tensor_scalar(ys.t[:], ys.t[:], rv.t[:, 7, pc:pc + 1], rv.t[:, 8, pc:pc + 1], ALU.mult, ALU.add), [ys, rv], [ys])
                    V(lambda e: e.tensor_tensor(ys.t[:], ys.t[:], u1.t[:], ALU.add), [ys, u1], [ys])
                    for half in range(2):
                        hsl = slice(half * 512, half * 512 + 512)
                        M(lambda e, hsl=hsl, half=half: e.matmul(po[half].t[:], g2a.t[:, cs_], sga.t[:, hsl], start=True, stop=False), [g2a, sga], [po[half]], sig=False)
                        M(lambda e, hsl=hsl, half=half: e.matmul(po[half].t[:], g2b.t[0:32, cs_], sgb.t[0:32, hsl], start=False, stop=True), [g2b, sgb], [po[half]])
                        V(lambda e, hsl=hsl, half=half: e.tensor_tensor(yTr.t[:, pc, hsl], po[half].t[:], ys.t[:, hsl], ALU.mult), [po[half], ys], [yTr])
                    S.barrier(); S.emit()
        if stop <= 3:
            r_nc = _finish(nc, S, st, dram_in)
            st_y.close()
            return r_nc

        st_y2 = ExitStack()
        yTc = B(st_y2, "yTc", [128, 8, TOK], BF16)
        with ExitStack() as ph:
            W = TOK + KW - 1
            zc = B(ph, "zc", [128, 8, TOK]); val = B(ph, "val", [128, W]); gate = B(ph, "gate", [128, W]); z = B(ph, "z", [128, W])
            sq = B(ph, "csq", [128, TOK]); mean = B(ph, "cmean", [128, TOK]); rstd = B(ph, "crstd", [128, TOK]); tmpc = B(ph, "ctmp", [128, TOK])
            pS = [PB(ph, "pS%d" % i, [128, 512]) for i in range(2)]; pQ = [PB(ph, "pQ%d" % i, [128, 512]) for i in range(2)]
            c_lo = HALO - KW // 2
            for cc in range(8):
                LD("sp", val.t[:], PH[cc * 128:(cc + 1) * 128, c_lo:c_lo + W], "cv", [val])
                LD("act", gate.t[:], PH[(8 + cc) * 128:(9 + cc) * 128, c_lo:c_lo + W], "cg", [gate])
                A(lambda e: e.activation(gate.t[:], gate.t[:], AF.Sigmoid), [gate], [gate])
                V(lambda e: e.tensor_tensor(z.t[:], val.t[:], gate.t[:], ALU.mult), [val, gate], [z])
                V(lambda e, cc=cc: e.tensor_scalar(zc.t[:, cc, :], z.t[:, 0:TOK], convw.t[:, cc, 0:1], convv.t[:, 0, cc:cc + 1], ALU.mult, ALU.add), [z, convw, convv], [zc])
                for k in range(1, KW):
                    V(lambda e, cc=cc, k=k: e.scalar_tensor_tensor(zc.t[:, cc, :], z.t[:, k:k + TOK], convw.t[:, cc, k:k + 1], zc.t[:, cc, :], ALU.mult, ALU.add), [z, convw, zc], [zc])
                A(lambda e, cc=cc: e.activation(sq.t[:], zc.t[:, cc, :], AF.Square), [zc], [sq])
                for half in range(2):
                    hsl = slice(half * 512, half * 512 + 512)
                    M(lambda e, cc=cc, hsl=hsl, half=half: e.matmul(pS[half].t[:], ones.t[:], zc.t[:, cc, hsl], start=(cc == 0), stop=(cc == 7)), [ones, zc], [pS[half]])
                    M(lambda e, cc=cc, hsl=hsl, half=half: e.matmul(pQ[half].t[:], ones.t[:], sq.t[:, hsl], start=(cc == 0), stop=(cc == 7)), [ones, sq], [pQ[half]])
            for half in range(2):
                hsl = slice(half * 512, half * 512 + 512)
                A(lambda e, hsl=hsl, half=half: e.mul(mean.t[:, hsl], pS[half].t[:], 1.0 / DC), [pS[half]], [mean])
                V(lambda e, hsl=hsl: e.tensor_tensor(tmpc.t[:, hsl], mean.t[:, hsl], mean.t[:, hsl], ALU.mult), [mean], [tmpc])
                V(lambda e, hsl=hsl, half=half: e.scalar_tensor_tensor(rstd.t[:, hsl], pQ[half].t[:], 1.0 / DC, tmpc.t[:, hsl], ALU.mult, ALU.subtract), [pQ[half], tmpc], [rstd])
            V(lambda e: e.tensor_scalar(rstd.t[:], rstd.t[:], LN_EPS, None, ALU.add), [rstd], [rstd])
            A(lambda e: e.activation(rstd.t[:], rstd.t[:], AF.Sqrt), [rstd], [rstd])
            V(lambda e: e.reciprocal(rstd.t[:], rstd.t[:]), [rstd], [rstd])
            for cc in range(8):
                V(lambda e, cc=cc: e.tensor_tensor(tmpc.t[:], zc.t[:, cc, :], mean.t[:], ALU.subtract), [zc, mean], [tmpc])
                V(lambda e: e.tensor_tensor(tmpc.t[:], tmpc.t[:], rstd.t[:], ALU.mult), [tmpc, rstd], [tmpc])
                A(lambda e, cc=cc: e.activation(yTc.t[:, cc, :], tmpc.t[:], AF.Silu, bias=convv.t[:, 2, cc:cc + 1], scale=convv.t[:, 1, cc:cc + 1]), [tmpc, convv], [yTc])
            S.barrier(); S.emit()
        if stop <= 4:
            r_nc = _finish(nc, S, st, dram_in)
            st_y2.close(); st_y.close()
            return r_nc

        wout_d = din("w_out_r", [16, 128, 16, 128])
        st_x = ExitStack()
        xT = sb(st_x, "xT", [128, 16, TOK]); t_xT = Tok("xT")
        BxT = Buf(xT, t_xT)
        with ExitStack() as ph2:
            transpose_pass(ph2, "c", xh[HALO:HALO + TOK, :], TOK // 128, False, lambda fc, o, n: xT[:, fc, o:o + n], t_xT)
            S.barrier(); S.emit()
        with ExitStack() as ph:
            slabs = [B(ph, "wo%d" % i, [128, 16, 128], BF16) for i in range(3)]
            pp = [PB(ph, "ppo%d" % i, [128, 512]) for i in range(4)]
            pi = 0
            for oc in range(16):
                sl = slabs[oc % 3]
                LD("pool", sl.t[:], wout_d[oc], "w%d" % (oc % 3), [sl])
                for half in range(2):
                    hsl = slice(half * 512, half * 512 + 512)
                    pt = pp[pi % 4]; pi += 1
                    for kk in range(16):
                        M(lambda e, pt=pt, sl=sl, kk=kk, hsl=hsl: e.matmul(pt.t[:], sl.t[:, kk, :], (yTc.t[:, kk, hsl] if kk < 8 else yTr.t[:, kk - 8, hsl]), start=(kk == 0), stop=(kk == 15)), [sl, yTc, yTr], [pt], sig=(kk == 15))
                    V(lambda e, pt=pt, oc=oc, hsl=hsl: e.scalar_tensor_tensor(xT[:, oc, hsl], pt.t[:], modv[:, 32 + oc:33 + oc], xT[:, oc, hsl], ALU.mult, ALU.add), [pt, Bmodv, BxT], [BxT])
            S.barrier(); S.emit()
        if stop <= 5:
            r_nc = _finish(nc, S, st, dram_in, dbg_fn=lambda: S.dma("sp", dbg["xT"], xT[:], "dbg", reads=[t_xT], batch=True, is_out=True) if debug else None)
            st_x.close(); st_y2.close(); st_y.close()
            return r_nc

        wr_d = din("wr", [128, 16, 36]); br_d = din("br", [128, 36])
        wg_d = din("wg_r", [NE, 4, 128, 16, 128]); wu_d = din("wu_r", [NE, 4, 128, 16, 128]); wd_d = din("wd_r", [NE, 4, 128, 4, 512])
        WT = nc.dram_tensor("WT", [NE, TOK], F32, kind="Internal").ap()
        with ExitStack() as ph:
            hn2T = B(ph, "hn2T", [128, 16, TOK], BF16)
            with ExitStack() as p2:
                wr = B(p2, "wr", [128, 16, 36]); br = B(p2, "br", [128, 36])
                LD("sp", wr.t[:], wr_d, "const", [wr], batch=True); LD("sp", br.t[:], br_d, "const", [br], batch=True)
                sq = B(p2, "nsq", [128, TOK]); rs2 = B(p2, "rs2", [128, TOK]); hf = B(p2, "hf", [128, TOK])
                pS = [PB(p2, "nS%d" % i, [128, 512]) for i in range(2)]; pL = [PB(p2, "nL%d" % i, [128, 512]) for i in range(2)]
                pT = PB(p2, "nT", [128, 512]); pW = [PB(p2, "nW%d" % i, [128, 512]) for i in range(2)]
                for fc in range(16):
                    A(lambda e, fc=fc: e.activation(sq.t[:], xT[:, fc, :], AF.Square), [BxT], [sq])
                    for half in range(2):
                        hsl = slice(half * 512, half * 512 + 512)
                        M(lambda e, fc=fc, hsl=hsl, half=half: e.matmul(pS[half].t[:], ones.t[:], sq.t[:, hsl], start=(fc == 0), stop=(fc == 15)), [ones, sq], [pS[half]])
                for half in range(2):
                    hsl = slice(half * 512, half * 512 + 512)
                    V(lambda e, hsl=hsl, half=half: e.tensor_scalar(rs2.t[:, hsl], pS[half].t[:], 1.0 / D, RMS_EPS, ALU.mult, ALU.add), [pS[half]], [rs2])
                A(lambda e: e.activation(rs2.t[:], rs2.t[:], AF.Sqrt), [rs2], [rs2])
                V(lambda e: e.reciprocal(rs2.t[:], rs2.t[:]), [rs2], [rs2])
                for fc in range(16):
                    V(lambda e, fc=fc: e.tensor_tensor(hf.t[:], xT[:, fc, :], rs2.t[:], ALU.mult), [BxT, rs2], [hf])
                    V(lambda e, fc=fc: e.tensor_scalar(hf.t[:], hf.t[:], a2[:, fc:fc + 1], modv[:, 48 + fc:49 + fc], ALU.mult, ALU.add), [hf, Ba2, Bmodv], [hf])
                    A(lambda e, fc=fc: e.copy(hn2T.t[:, fc, :], hf.t[:]), [hf], [hn2T])
                    for half in range(2):
                        hsl = slice(half * 512, half * 512 + 512)
                        M(lambda e, fc=fc, hsl=hsl, half=half: e.matmul(pL[half].t[0:36, :], wr.t[:, fc, :], hf.t[:, hsl], start=(fc == 0), stop=(fc == 15)), [wr, hf], [pL[half]])
                LT = B(p2, "LT", [36, TOK]); L = B(p2, "L", [128, 8, 36])
                for half in range(2):
                    A(lambda e, half=half: e.copy(LT.t[:, half * 512:(half + 1) * 512], pL[half].t[0:36, :]), [pL[half]], [LT])
                for t_ in range(8):
                    M(lambda e, t_=t_: e.matmul(pT.t[:, t_ * 36:(t_ + 1) * 36], LT.t[:, t_ * 128:(t_ + 1) * 128], ident[0:36, 0:36], start=True, stop=True), [LT, Bident], [pT], sig=(t_ == 7))
                V(lambda e: e.tensor_tensor(L.t[:], pT.t[:, 0:288].rearrange("p (t c) -> p t c", c=36), br.t[:].rearrange("p (o c) -> p o c", o=1).to_broadcast([128, 8, 36]), ALU.add), [pT, br], [L])
                gl = L.t[:, :, 0:4]
                el = L.t[:, :, 4:36]
                gmax = B(p2, "gmax", [128, 8]); gmask = B(p2, "gmask", [128, 8, 4]); gex = B(p2, "gex", [128, 8, 4]); pg = B(p2, "pg", [128, 8])
                elm = B(p2, "elm", [128, 8, 32]); m1 = B(p2, "m1", [128, 8]); m2 = B(p2, "m2", [128, 8]); k1 = B(p2, "k1", [128, 8, 32]); k2 = B(p2, "k2", [128, 8, 32])
                w1 = B(p2, "w1", [128, 8]); w2_ = B(p2, "w2_", [128, 8]); wt = B(p2, "wt", [128, 8, 32])
                bc8 = lambda b_, n: b_.t[:].rearrange("p (t o) -> p t o", o=1).to_broadcast([128, 8, n])
                V(lambda e: e.tensor_reduce(gmax.t[:], gl, AX.X, ALU.max), [L], [gmax])
                V(lambda e: e.tensor_tensor(gmask.t[:], gl, bc8(gmax, 4), ALU.is_equal), [L, gmax], [gmask])
                V(lambda e: e.tensor_tensor(gex.t[:], gl, bc8(gmax, 4), ALU.subtract), [L, gmax], [gex])
                A(lambda e: e.activation(gex.t[:], gex.t[:], AF.Exp), [gex], [gex])
                V(lambda e: e.tensor_reduce(pg.t[:], gex.t[:], AX.X, ALU.add), [gex], [pg])
                V(lambda e: e.reciprocal(pg.t[:], pg.t[:]), [pg], [pg])
                V(lambda e: e.tensor_scalar(gmask.t[:], gmask.t[:], 1e30, -1e30, ALU.mult, ALU.add), [gmask], [gmask])
                V(lambda e: e.tensor_tensor(elm.t[:].rearrange("p t (g x) -> p t g x", x=8), el.rearrange("p t (g x) -> p t g x", x=8),
                                            gmask.t[:].rearrange("p t (g o) -> p t g o", o=1).to_broadcast([128, 8, 4, 8]), ALU.add), [L, gmask], [elm])
                V(lambda e: e.tensor_reduce(m1.t[:], elm.t[:], AX.X, ALU.max), [elm], [m1])
                V(lambda e: e.tensor_tensor(k1.t[:], elm.t[:], bc8(m1, 32), ALU.is_equal), [elm, m1], [k1])
                V(lambda e: e.scalar_tensor_tensor(elm.t[:], k1.t[:], -1e30, elm.t[:], ALU.mult, ALU.add), [k1, elm], [elm])
                V(lambda e: e.tensor_reduce(m2.t[:], elm.t[:], AX.X, ALU.max), [elm], [m2])
                V(lambda e: e.tensor_tensor(k2.t[:], elm.t[:], bc8(m2, 32), ALU.is_equal), [elm, m2], [k2])
                V(lambda e: e.tensor_tensor(w1.t[:], m1.t[:], m2.t[:], ALU.subtract), [m1, m2], [w1])
                A(lambda e: e.activation(w2_.t[:], w1.t[:], AF.Sigmoid, scale=-1.0), [w1], [w2_])
                A(lambda e: e.activation(w1.t[:], w1.t[:], AF.Sigmoid), [w1], [w1])
                V(lambda e: e.tensor_tensor(w1.t[:], w1.t[:], pg.t[:], ALU.mult), [w1, pg], [w1])
                V(lambda e: e.tensor_tensor(w2_.t[:], w2_.t[:], pg.t[:], ALU.mult), [w2_, pg], [w2_])
                V(lambda e: e.tensor_tensor(wt.t[:], k1.t[:], bc8(w1, 32), ALU.mult), [k1, w1], [wt])
                V(lambda e: e.tensor_tensor(k2.t[:], k2.t[:], bc8(w2_, 32), ALU.mult), [k2, w2_], [k2])
                V(lambda e: e.tensor_tensor(wt.t[:], wt.t[:], k2.t[:], ALU.add), [wt, k2], [wt])
                wtT = B(p2, "wtT", [32, TOK])
                for t_ in range(8):
                    M(lambda e, t_=t_: e.matmul(pW[t_ // 4].t[0:32, (t_ % 4) * 128:(t_ % 4 + 1) * 128], wt.t[:, t_, :], ident[:], start=True, stop=True), [wt, Bident], [pW[t_ // 4]], sig=(t_ % 4 == 3))
                for half in range(2):
                    A(lambda e, half=half: e.copy(wtT.t[:, half * 512:(half + 1) * 512], pW[half].t[0:32, :]), [pW[half]], [wtT])
                S.dma("sp", WT, wtT.t[:], "wt", reads=[wtT.k])
                S.barrier(); S.emit()
            with ExitStack() as p2:
                gu = [B(p2, "gu%d" % i, [128, 16, 128], BF16) for i in range(4)]
                gu = [Buf(g0.t[:], g0.k) for g0 in gu]
                for ysrc in (yTr, yTc):
                    for j4 in range(4):
                        gu.append(Buf(ysrc.t[:, 2 * j4:2 * j4 + 2, :].rearrange("p a (b c) -> p (a b) c", c=128)))
                NGU = len(gu)
                wdn = [B(p2, "wdn%d" % i, [128, 4, 512], BF16) for i in range(4)]
                act = B(p2, "act", [128, 4, TOK], BF16)
                wb = [B(p2, "wb%d" % i, [128, TOK]) for i in range(2)]
                sl_ = [B(p2, "sl%d" % i, [128, 512]) for i in range(2)]
                pG = [PB(p2, "pG%d" % i, [128, 512]) for i in range(2)]; pU = [PB(p2, "pU%d" % i, [128, 512]) for i in range(2)]
                pD = [PB(p2, "pD%d" % i, [128, 512]) for i in range(4)]
                gi = 0; di = 0; si = 0
                for ex in range(NE):
                    wbe = wb[ex % 2]
                    LD("sp", wbe.t[:], WT[ex:ex + 1, :].to_broadcast([128, TOK]), "wb%d" % (ex % 2), [wbe])
                    for dc in range(4):
                        g_ = gu[gi % NGU]; LD("pool", g_.t, wg_d[ex, dc], "gu%d" % (gi % NGU), [g_]); gi += 1
                        u_ = gu[gi % NGU]; LD("pool", u_.t, wu_d[ex, dc], "gu%d" % (gi % NGU), [u_]); gi += 1
                        for half in range(2):
                            hsl = slice(half * 512, half * 512 + 512)
                            for kk in range(16):
                                M(lambda e, g_=g_, kk=kk, hsl=hsl, half=half: e.matmul(pG[half].t[:], g_.t[:, kk, :], hn2T.t[:, kk, hsl], start=(kk == 0), stop=(kk == 15)), [g_, hn2T], [pG[half]], sig=(kk == 15))
                            for kk in range(16):
                                M(lambda e, u_=u_, kk=kk, hsl=hsl, half=half: e.matmul(pU[half].t[:], u_.t[:, kk, :], hn2T.t[:, kk, hsl], start=(kk == 0), stop=(kk == 15)), [u_, hn2T], [pU[half]], sig=(kk == 15))
                            s_ = sl_[si % 2]; si += 1
                            A(lambda e, s_=s_, half=half: e.activation(s_.t[:], pG[half].t[:], AF.Silu), [pG[half]], [s_])
                            P(lambda e, s_=s_, wbe=wbe, hsl=hsl: e.tensor_tensor(s_.t[:], s_.t[:], wbe.t[:, hsl], ALU.mult), [s_, wbe], [s_])
                            V(lambda e, s_=s_, dc=dc, hsl=hsl, half=half: e.tensor_tensor(act.t[:, dc, hsl], pU[half].t[:], s_.t[:], ALU.mult), [pU[half], s_], [act])
                    for og in range(4):
                        wd_ = wdn[di % 4]; LD("pool", wd_.t[:], wd_d[ex, og], "wd%d" % (di % 4), [wd_]); di += 1
                        for o4 in range(4):
                            oc = og * 4 + o4
                            for half in range(2):
                                hsl = slice(half * 512, half * 512 + 512)
                                pt = pD[(o4 * 2 + half) % 4]
                                for kk in range(4):
                                    M(lambda e, pt=pt, wd_=wd_, kk=kk, o4=o4, hsl=hsl: e.matmul(pt.t[:], wd_.t[:, kk, o4 * 128:(o4 + 1) * 128], act.t[:, kk, hsl], start=(kk == 0), stop=(kk == 3)), [wd_, act], [pt], sig=(kk == 3))
                                V(lambda e, pt=pt, oc=oc, hsl=hsl: e.scalar_tensor_tensor(xT[:, oc, hsl], pt.t[:], modv[:, 80 + oc:81 + oc], xT[:, oc, hsl], ALU.mult, ALU.add), [pt, Bmodv, BxT], [BxT])
                S.barrier(); S.emit()
        if stop <= 6:
            r_nc = _finish(nc, S, st, dram_in, dbg_fn=lambda: S.dma("sp", dbg["xT"], xT[:], "dbg", reads=[t_xT], batch=True, is_out=True) if debug else None)
            st_x.close(); st_y2.close(); st_y.close()
            return r_nc

        with ExitStack() as ph:
            dgf = B(ph, "dgf", [128, 16, 128]); sqs = [B(ph, "fsq%d" % i, [128, 128]) for i in range(2)]
            rt = B(ph, "frt", [128, 8]); ot = [B(ph, "ot%d" % i, [128, D]) for i in range(2)]
            pss = PB(ph, "pss", [128, 8]); pf = [PB(ph, "pf%d" % i, [128, 512]) for i in range(4)]
            for fc in range(16):
                V(lambda e, fc=fc: e.tensor_scalar(dgf.t[:, fc, :], ident[:], gnv[:, 2, fc:fc + 1], None, ALU.mult), [Bident, Bgnv], [dgf])
            qi = 0
            for t_ in range(8):
                for fc in range(16):
                    q_ = sqs[qi % 2]; qi += 1
                    A(lambda e, q_=q_, fc=fc, t_=t_: e.activation(q_.t[:], xT[:, fc, t_ * 128:(t_ + 1) * 128], AF.Square), [BxT], [q_])
                    M(lambda e, q_=q_, fc=fc, t_=t_: e.matmul(pss.t[:, t_:t_ + 1], q_.t[:], ones.t[:, 0:1], start=(fc == 0), stop=(fc == 15)), [q_, ones], [pss])
            V(lambda e: e.tensor_scalar(rt.t[:], pss.t[:], 1.0 / D, RMS_EPS, ALU.mult, ALU.add), [pss], [rt])
            A(lambda e: e.activation(rt.t[:], rt.t[:], AF.Sqrt), [rt], [rt])
            V(lambda e: e.reciprocal(rt.t[:], rt.t[:]), [rt], [rt])
            pi = 0
            for t_ in range(8):
                o_ = ot[t_ % 2]
                for fg in range(4):
                    pt = pf[pi % 4]; pi += 1
                    for j4 in range(4):
                        fc = fg * 4 + j4
                        M(lambda e, pt=pt, fc=fc, j4=j4, t_=t_: e.matmul(pt.t[:, j4 * 128:(j4 + 1) * 128], xT[:, fc, t_ * 128:(t_ + 1) * 128], dgf.t[:, fc, :], start=True, stop=True), [BxT, dgf], [pt], sig=(j4 == 3))
                    if fg % 2 == 0:
                        V(lambda e, pt=pt, o_=o_, fg=fg, t_=t_: e.tensor_scalar(o_.t[:, fg * 512:(fg + 1) * 512], pt.t[:], rt.t[:, t_:t_ + 1], None, ALU.mult), [pt, rt], [o_])
                    else:
                        A(lambda e, pt=pt, o_=o_, fg=fg, t_=t_: e.mul(o_.t[:, fg * 512:(fg + 1) * 512], pt.t[:], rt.t[:, t_:t_ + 1]), [pt, rt], [o_])
                S.dma("sp", out_d[t_ * 128:(t_ + 1) * 128, :], o_.t[:], "out%d" % (t_ % 2), reads=[o_.k], is_out=True)
            r_nc = _finish(nc, S, st, dram_in)
        st_x.close(); st_y2.close(); st_y.close()
        return r_nc


_IN_NAMES = []


def _finish(nc, S, st, dram_in=None, dbg_fn=None):
    _IN_NAMES[:] = list(dram_in.keys())
    if dbg_fn is not None:
        dbg_fn()
    S.barrier()
    S.emit(final=True)
    return nc


def _colvec(v, n):
    return np.ascontiguousarray(np.asarray(v, np.float32).reshape(n, 128).T)


def _kslab(w, nchunk):
    K, nc_ = w.shape
    pad = nchunk * 128 - nc_
    if pad:
        w = np.concatenate([w, np.zeros((K, pad), np.float32)], axis=1)
    return np.ascontiguousarray(w.reshape(K // 128, 128, nchunk, 128).transpose(2, 1, 0, 3))


def prep_shared(inp):
    f = lambda k: np.asarray(inp[k], np.float32)
    sh = {}
    sh["w_ada_r"] = _kslab(f("w_ada")[0], 96)
    sh["b_ada_r"] = _colvec(f("b_ada")[0], 96)
    g = np.stack([f("norm1_g")[0], f("norm2_g")[0], f("normf_g")])
    sh["gnorm"] = np.ascontiguousarray(g.reshape(3, 16, 128).transpose(2, 0, 1))
    sh["w_in_r"] = _kslab(f("w_in")[0], NCC)
    sh["ident"] = np.eye(128, dtype=np.float32)
    idx = np.arange(128)
    bm = lambda L: (idx[:, None] // L) == (idx[None, :] // L)
    cm = np.stack([idx[:, None] < idx[None, :], idx[:, None] <= idx[None, :], idx[:, None] > idx[None, :], idx[:, None] >= idx[None, :],
                   bm(16), bm(32) & ~bm(16), bm(64) & ~bm(32), ~bm(64)], axis=1)
    sh["cmask"] = np.ascontiguousarray(cm.astype(np.float32))
    rst = np.ones((128, 512), np.float32); rst[:, ::128] = 0.0
    sh["rst"] = rst
    sh["blk64"] = np.kron(np.eye(2, dtype=np.float32), np.ones((64, 64), np.float32))
    sh["ones"] = np.ones((128, 128), np.float32)
    sh["ident2"] = np.ascontiguousarray(np.concatenate([np.eye(64, dtype=np.float32)] * 2, axis=0))
    mu = np.zeros((2, NCC * 128), np.float32)
    mu[0, 2048:2048 + 3488] = f("mu_prev")[0]
    mu[1, 2048:2048 + 3488] = f("mu_next")[0]
    sh["mu"] = np.ascontiguousarray(mu.reshape(2, NCC, 128).transpose(2, 0, 1))
    rvn = ["w0_f", "w0_b", "a0_f", "a0_b", "k_k", "k_a", "r_k", "lnx_g", "lnx_b"]
    sh["rv"] = np.ascontiguousarray(np.stack([f(k)[0].reshape(8, 128).T for k in rvn], axis=1))
    sh["w2"] = np.ascontiguousarray(np.concatenate([f("w2_f")[0], f("w2_b")[0]], axis=0))
    sh["a2w"] = np.ascontiguousarray(np.concatenate([f("a2_f")[0], f("a2_b")[0]], axis=0))
    sh["g2a"] = np.ascontiguousarray(f("g2")[0][:128])
    sh["g2b"] = np.ascontiguousarray(f("g2")[0][128:160])
    cw = f("conv_w")[0][:, 0, :]
    sh["convw"] = np.ascontiguousarray(cw.reshape(KW, 8, 128).transpose(2, 1, 0))
    sh["convv"] = np.ascontiguousarray(np.stack([f(k)[0].reshape(8, 128).T for k in ("conv_b", "conv_ln_g", "conv_ln_b")], axis=1))
    sh["w_out_r"] = _kslab(f("w_out")[0], 16)
    wr = np.concatenate([f("w_rg")[0], f("w_re")[0]], axis=1)
    sh["wr"] = np.ascontiguousarray(wr.reshape(16, 128, 36).transpose(1, 0, 2))
    br = np.concatenate([f("b_rg")[0], f("b_re")[0]])
    sh["br"] = np.ascontiguousarray(np.broadcast_to(br[None, :], (128, 36)))
    wg, wu, wd = f("w_gate")[0], f("w_up")[0], f("w_down")[0]
    sh["wg_r"] = np.ascontiguousarray(wg.reshape(NE, 16, 128, 4, 128).transpose(0, 3, 2, 1, 4))
    sh["wu_r"] = np.ascontiguousarray(wu.reshape(NE, 16, 128, 4, 128).transpose(0, 3, 2, 1, 4))
    sh["wd_r"] = np.ascontiguousarray(wd.reshape(NE, 4, 128, 4, 512).transpose(0, 3, 2, 1, 4))
    return sh


def prep_core(inp, i):
    b, q = i // 4, i % 4
    x = np.asarray(inp["x"], np.float32)
    m = {}
    m["xf"] = np.ascontiguousarray(x[b])
    lo = q * TOK - HALO
    xh = np.zeros((TH, D), np.float32)
    hm = np.zeros((TH,), np.float32)
    s0, s1 = max(lo, 0), min(lo + TH, SEQ)
    xh[s0 - lo:s1 - lo] = x[b, s0:s1]
    hm[s0 - lo:s1 - lo] = 1.0
    m["xh"] = xh
    m["hmask"] = np.ascontiguousarray(np.broadcast_to(hm[None, :], (128, TH)))
    qf = np.zeros((128, 4), np.float32)
    qf[:, q] = 1.0
    m["qflag"] = qf
    m["c_col"] = _colvec(np.asarray(inp["c"], np.float32)[b], 16)
    return m


_NC_CACHE = {}


def kernel(**inputs):
    if "nc" not in _NC_CACHE:
        _NC_CACHE["nc"] = build()
    nc = _NC_CACHE["nc"]
    sh = prep_shared(inputs)
    in_maps = []
    for i in range(NCORES):
        m = dict(sh)
        m.update(prep_core(inputs, i))
        in_maps.append({k: m[k] for k in _IN_NAMES})
    res = run_bass_kernel_spmd(nc, in_maps, core_ids=list(range(NCORES)))
    out = np.zeros((2, SEQ, D), np.float32)
    for i in range(NCORES):
        b, q = i // 4, i % 4
        out[b, q * TOK:(q + 1) * TOK] = res.results[i]["out"]
    return out
```
